# Optimizing a Trainium2 kernel written in Bass

```python
import math
import jax
import jax.numpy as jnp
from jax import lax
import numpy as np

D_MODEL = 1024
BATCH = 1
SEQ = 16384
DEPTH = 1

HEAD_DIM = 64
FOX_HEADS = 6
DIL_CONFIGS = ((128, 1), (512, 4), (2048, 16))
DIL_HEADS_PER_GROUP = 2
DIL_GROUPS = len(DIL_CONFIGS)
DIL_HEADS = DIL_HEADS_PER_GROUP * DIL_GROUPS
MEM_HEADS = 4
MEM_LEN = 256
FOX_W = FOX_HEADS * HEAD_DIM
DIL_W = DIL_HEADS * HEAD_DIM
MEM_W = MEM_HEADS * HEAD_DIM
N_BRANCHES = 3
QBLK = 128
DBLK = 128
T5_BUCKETS = 32
T5_MAX_EXACT = T5_BUCKETS // 2
T5_MAX_DISTANCE = 2048
N_EXPERTS = 32
TOP_K = 4
D_FF_EXPERT = D_MODEL
SWIGLU_LIMIT = 7.0
SWIGLU_ALPHA = 1.702
EBLK = 128
LN_EPS = 1e-5
DEEPNORM_ALPHA = (2 * DEPTH) ** 0.25
DEEPNORM_BETA = (8 * DEPTH) ** -0.25

OFF_FOX_QKV = 0
OFF_FOX_F = OFF_FOX_QKV + 3 * FOX_W
OFF_DIL_QKV = OFF_FOX_F + FOX_HEADS
OFF_MEM_Q = OFF_DIL_QKV + 3 * DIL_W
OFF_GATES = OFF_MEM_Q + MEM_W
D_IN = OFF_GATES + N_BRANCHES * D_MODEL

kernel_name = 'hybrid_fox_dilated_mem_moe_block'


def layer_norm(x, g, b):
    xf = x.astype(jnp.float32)
    mu = jnp.mean(xf, axis=-1, keepdims=True)
    var = jnp.mean(jnp.square(xf - mu), axis=-1, keepdims=True)
    y = (xf - mu) * lax.rsqrt(var + LN_EPS) * g.astype(jnp.float32) + b.astype(jnp.float32)
    return y.astype(x.dtype)


def t5_bucket(dist):
    is_small = dist < T5_MAX_EXACT
    nf = jnp.maximum(dist, T5_MAX_EXACT).astype(jnp.float32)
    large = T5_MAX_EXACT + (jnp.log(nf / T5_MAX_EXACT) / math.log(T5_MAX_DISTANCE / T5_MAX_EXACT)
                            * (T5_BUCKETS - T5_MAX_EXACT)).astype(jnp.int32)
    large = jnp.minimum(large, T5_BUCKETS - 1)
    return jnp.where(is_small, dist, large)


def fox_attention(q, k, v, log_f):
    B, S, H, Dh = q.shape
    nblk = S // QBLK
    scale = Dh ** -0.5
    c = jnp.cumsum(log_f, axis=1)
    c_keys = c.transpose(0, 2, 1)
    qb = jnp.moveaxis(q.reshape(B, nblk, QBLK, H, Dh), 1, 0)
    cb = jnp.moveaxis(c_keys.reshape(B, H, nblk, QBLK), 2, 0)
    key_pos = jnp.arange(S)

    def block(args):
        n, qn, cn = args
        s = jnp.einsum('bqhd,bkhd->bhqk', qn, k, preferred_element_type=jnp.float32) * scale
        s = s + (cn[..., :, None] - c_keys[:, :, None, :])
        q_pos = n * QBLK + jnp.arange(QBLK)
        causal = key_pos[None, :] <= q_pos[:, None]
        s = jnp.where(causal, s, -jnp.inf)
        p = jax.nn.softmax(s, axis=-1)
        return jnp.einsum('bhqk,bkhd->bqhd', p.astype(v.dtype), v)

    out = lax.map(block, (jnp.arange(nblk), qb, cb))
    return jnp.moveaxis(out, 0, 1).reshape(B, S, H, Dh)


def dilated_group(q, k, v, table, window, dil):
    B, S, H, Dh = q.shape
    n_back = window // dil
    unit = dil * DBLK
    Sp = -(-S // unit) * unit
    M = Sp // dil
    nb = M // DBLK
    scale = Dh ** -0.5

    def to_blocks(a):
        a = jnp.pad(a, ((0, 0), (0, Sp - S), (0, 0), (0, 0)))
        a = a.reshape(B, M, dil, H, Dh).transpose(0, 2, 1, 3, 4)
        return a.reshape(B, dil, nb, DBLK, H, Dh)

    def with_prev(a):
        prev = jnp.pad(a, ((0, 0), (0, 0), (1, 0), (0, 0), (0, 0), (0, 0)))[:, :, :-1]
        return jnp.concatenate([prev, a], axis=3)

    qb = to_blocks(q)
    kc = with_prev(to_blocks(k))
    vc = with_prev(to_blocks(v))
    s = jnp.einsum('brnqhd,brnkhd->brnhqk', qb, kc, preferred_element_type=jnp.float32) * scale

    i = np.arange(DBLK, dtype=np.int32)[:, None]
    j = np.arange(2 * DBLK, dtype=np.int32)[None, :]
    rel = DBLK + i - j
    in_window = (rel >= 0) & (rel <= n_back)
    bucket = t5_bucket(jnp.asarray(np.clip(rel, 0, None) * dil, dtype=jnp.int32))
    bias = table[bucket].astype(jnp.float32).transpose(2, 0, 1)
    s = s + bias
    has_prev = np.arange(nb)[:, None, None] > 0
    mask = in_window[None] & (has_prev | (j >= DBLK)[None])
    s = jnp.where(mask[None, None, :, None], s, -jnp.inf)

    m = jnp.max(s, axis=-1, keepdims=True)
    e = jnp.exp(s - m)
    den = jnp.sum(e, axis=-1, keepdims=True)
    p = e / den
    lse = (m + jnp.log(den))[..., 0]
    o = jnp.einsum('brnhqk,brnkhd->brnqhd', p.astype(vc.dtype), vc)
    o = o.reshape(B, dil, M, H, Dh).transpose(0, 2, 1, 3, 4).reshape(B, Sp, H, Dh)[:, :S]
    lse = lse.transpose(0, 1, 2, 4, 3).reshape(B, dil, M, H).transpose(0, 2, 1, 3).reshape(B, Sp, H)[:, :S]
    return o, lse


def mem_attention(q, mem, w_kv):
    B, S, H, Dh = q.shape
    M = mem.shape[1]
    kv = jnp.einsum('bmd,de->bme', mem, w_kv).reshape(B, M, 2, H, Dh)
    k, v = kv[:, :, 0], kv[:, :, 1]
    s = jnp.einsum('bshd,bmhd->bhsm', q, k, preferred_element_type=jnp.float32) * (Dh ** -0.5)
    p = jax.nn.softmax(s, axis=-1)
    return jnp.einsum('bhsm,bmhd->bshd', p.astype(v.dtype), v)


def clamped_swiglu(h):
    h_glu, h_lin = jnp.split(h, 2, axis=-1)
    h_glu = jnp.minimum(h_glu, SWIGLU_LIMIT)
    h_lin = jnp.clip(h_lin, -SWIGLU_LIMIT, SWIGLU_LIMIT)
    return h_glu * jax.nn.sigmoid(SWIGLU_ALPHA * h_glu) * (h_lin + 1.0)


def moe_ffn(xt, w_router, b_router, w1, b1, w2, b2):
    T, D = xt.shape
    A = T * TOP_K
    logits = jnp.einsum('td,de->te', xt, w_router, preferred_element_type=jnp.float32) + b_router.astype(jnp.float32)
    top_vals, top_idx = lax.top_k(logits, TOP_K)
    gates = jax.nn.softmax(top_vals, axis=-1)
    flat_e = top_idx.reshape(A)
    flat_tok = jnp.broadcast_to(jnp.arange(T, dtype=jnp.int32)[:, None], (T, TOP_K)).reshape(A)
    flat_g = gates.reshape(A)
    order = jnp.argsort(flat_e)
    sorted_e = flat_e[order]
    counts = jnp.bincount(flat_e, length=N_EXPERTS)
    starts = jnp.cumsum(counts) - counts
    padded = (counts + EBLK - 1) // EBLK * EBLK
    pad_ends = jnp.cumsum(padded)
    pad_starts = pad_ends - padded
    dest = pad_starts[sorted_e] + (jnp.arange(A) - starts[sorted_e])
    n_blocks = -(-(A + N_EXPERTS * EBLK) // EBLK)
    P = n_blocks * EBLK
    tok_buf = jnp.full((P,), T, dtype=jnp.int32).at[dest].set(flat_tok[order])
    gate_buf = jnp.zeros((P,), jnp.float32).at[dest].set(flat_g[order])
    blk_e = jnp.minimum(jnp.searchsorted(pad_ends, jnp.arange(n_blocks) * EBLK, side='right'), N_EXPERTS - 1)
    x_pad = jnp.concatenate([xt, jnp.zeros((1, D), xt.dtype)], axis=0)
    xb = x_pad[tok_buf].reshape(n_blocks, EBLK, D)

    def expert_block(args):
        xe, e = args
        h = xe @ w1[e] + b1[e]
        return clamped_swiglu(h) @ w2[e] + b2[e]

    yb = lax.map(expert_block, (xb, blk_e)).reshape(P, D)
    out = jnp.zeros((T + 1, D), jnp.float32).at[tok_buf].add(yb.astype(jnp.float32) * gate_buf[:, None])
    return out[:T].astype(xt.dtype)


def setup_inputs(seed: int = 0) -> dict:
    key = jax.random.key(seed)
    ks = jax.random.split(key, 32)

    def nrm(k, shape, scale):
        return jax.random.normal(k, shape, jnp.float32) * scale

    sd = D_MODEL ** -0.5
    beta = DEEPNORM_BETA
    x = nrm(ks[0], (BATCH, SEQ, D_MODEL), 1.0)
    mem = nrm(ks[1], (BATCH, MEM_LEN, D_MODEL), 1.0)
    w_in = jnp.concatenate([
        nrm(ks[2], (DEPTH, D_MODEL, 2 * FOX_W), sd),
        nrm(ks[3], (DEPTH, D_MODEL, FOX_W), sd * beta),
        nrm(ks[4], (DEPTH, D_MODEL, FOX_HEADS), 0.5 * sd),
        nrm(ks[5], (DEPTH, D_MODEL, 2 * DIL_W), sd),
        nrm(ks[6], (DEPTH, D_MODEL, DIL_W), sd * beta),
        nrm(ks[7], (DEPTH, D_MODEL, MEM_W), sd),
        nrm(ks[8], (DEPTH, D_MODEL, N_BRANCHES * D_MODEL), sd),
    ], axis=-1)
    b_fgate = jnp.linspace(1.0, 6.0, FOX_HEADS, dtype=jnp.float32)[None, :] + nrm(ks[9], (DEPTH, FOX_HEADS), 0.1)
    t5_bias = nrm(ks[10], (T5_BUCKETS, DIL_HEADS), 0.5)
    w_mem_kv = jnp.concatenate([
        nrm(ks[11], (DEPTH, D_MODEL, MEM_W), sd),
        nrm(ks[12], (DEPTH, D_MODEL, MEM_W), sd * beta),
    ], axis=-1)
    w_br_fox = nrm(ks[13], (DEPTH, FOX_W, D_MODEL), FOX_W ** -0.5 * beta)
    w_br_dil = nrm(ks[14], (DEPTH, DIL_W, D_MODEL), DIL_W ** -0.5 * beta)
    w_br_mem = nrm(ks[15], (DEPTH, MEM_W, D_MODEL), MEM_W ** -0.5 * beta)
    w_out = nrm(ks[16], (DEPTH, D_MODEL, D_MODEL), sd * beta)
    ln1_g = 1.0 + nrm(ks[17], (DEPTH, D_MODEL), 0.02)
    ln1_b = nrm(ks[18], (DEPTH, D_MODEL), 0.02)
    w_router = nrm(ks[19], (DEPTH, D_MODEL, N_EXPERTS), sd)
    b_router = nrm(ks[20], (DEPTH, N_EXPERTS), 0.01)
    w_exp_in = nrm(ks[21], (DEPTH, N_EXPERTS, D_MODEL, 2 * D_FF_EXPERT), sd * beta)
    b_exp_in = nrm(ks[22], (DEPTH, N_EXPERTS, 2 * D_FF_EXPERT), 0.01)
    w_exp_out = nrm(ks[23], (DEPTH, N_EXPERTS, D_FF_EXPERT, D_MODEL), D_FF_EXPERT ** -0.5 * beta)
    b_exp_out = nrm(ks[24], (DEPTH, N_EXPERTS, D_MODEL), 0.01)
    ln2_g = 1.0 + nrm(ks[25], (DEPTH, D_MODEL), 0.02)
    ln2_b = nrm(ks[26], (DEPTH, D_MODEL), 0.02)
    return {'x': x, 'mem': mem, 'w_in': w_in, 'b_fgate': b_fgate, 't5_bias': t5_bias,
            'w_mem_kv': w_mem_kv, 'w_br_fox': w_br_fox, 'w_br_dil': w_br_dil, 'w_br_mem': w_br_mem,
            'w_out': w_out, 'ln1_g': ln1_g, 'ln1_b': ln1_b, 'w_router': w_router, 'b_router': b_router,
            'w_exp_in': w_exp_in, 'b_exp_in': b_exp_in, 'w_exp_out': w_exp_out, 'b_exp_out': b_exp_out,
            'ln2_g': ln2_g, 'ln2_b': ln2_b}


def reference(x, mem, w_in, b_fgate, t5_bias, w_mem_kv, w_br_fox, w_br_dil, w_br_mem, w_out,
              ln1_g, ln1_b, w_router, b_router, w_exp_in, b_exp_in, w_exp_out, b_exp_out, ln2_g, ln2_b):
    B, S, D = x.shape
    h = x
    for l in range(DEPTH):
        proj = jnp.einsum('bsd,de->bse', h, w_in[l])
        fox_qkv = proj[..., OFF_FOX_QKV:OFF_FOX_F].reshape(B, S, 3, FOX_HEADS, HEAD_DIM)
        f_logit = proj[..., OFF_FOX_F:OFF_DIL_QKV] + b_fgate[l]
        log_f = jax.nn.log_sigmoid(f_logit.astype(jnp.float32))
        o_fox = fox_attention(fox_qkv[:, :, 0], fox_qkv[:, :, 1], fox_qkv[:, :, 2], log_f)
        o_fox = o_fox.reshape(B, S, FOX_W)

        dil_qkv = proj[..., OFF_DIL_QKV:OFF_MEM_Q].reshape(B, S, 3, DIL_HEADS, HEAD_DIM)
        outs = []
        lses = []
        for g, (window, dil) in enumerate(DIL_CONFIGS):
            hs = slice(g * DIL_HEADS_PER_GROUP, (g + 1) * DIL_HEADS_PER_GROUP)
            o_g, lse_g = dilated_group(dil_qkv[:, :, 0, hs], dil_qkv[:, :, 1, hs], dil_qkv[:, :, 2, hs],
                                       t5_bias[:, hs], window, dil)
            outs.append(o_g)
            lses.append(lse_g)
        o_stack = jnp.stack(outs, axis=2)
        w_den = jax.nn.softmax(jnp.stack(lses, axis=2), axis=2)
        o_dil = (o_stack * w_den[..., None].astype(o_stack.dtype)).reshape(B, S, DIL_W)

        mem_q = proj[..., OFF_MEM_Q:OFF_GATES].reshape(B, S, MEM_HEADS, HEAD_DIM)
        o_mem = mem_attention(mem_q, mem, w_mem_kv[l]).reshape(B, S, MEM_W)

        gates = jax.nn.sigmoid(proj[..., OFF_GATES:].reshape(B, S, N_BRANCHES, D))
        merged = (gates[:, :, 0] * jnp.einsum('bse,ed->bsd', o_fox, w_br_fox[l])
                  + gates[:, :, 1] * jnp.einsum('bse,ed->bsd', o_dil, w_br_dil[l])
                  + gates[:, :, 2] * jnp.einsum('bse,ed->bsd', o_mem, w_br_mem[l]))
        y = jnp.einsum('bsd,de->bse', merged, w_out[l])
        h = layer_norm(DEEPNORM_ALPHA * h + y, ln1_g[l], ln1_b[l])

        y_moe = moe_ffn(h.reshape(B * S, D), w_router[l], b_router[l], w_exp_in[l], b_exp_in[l],
                        w_exp_out[l], b_exp_out[l]).reshape(B, S, D)
        h = layer_norm(DEEPNORM_ALPHA * h + y_moe, ln2_g[l], ln2_b[l])
    return h
```

```python
import contextlib
import math
import numpy as np
import concourse.bass as bass
import concourse.mybir as mybir
from concourse.bass_utils import run_bass_kernel_spmd

F32 = mybir.dt.float32
BF16 = mybir.dt.bfloat16
I32 = mybir.dt.int32
AF = mybir.ActivationFunctionType
ALU = mybir.AluOpType

NCORES = 8
D = 1024
S_LEN = 16384
NTS = 136
NOWN = 16
NE = 32
CAP = 384
ALPHA = 2.0 ** 0.25
NEG = -30000.0
DIL_CFG = ((128, 1), (512, 4), (2048, 16))
DMAX = (1, 4, 16)
ENGINES = ("tensor", "vector", "scalar", "gpsimd", "sync")
PHASES = ("A", "Q", "B", "C", "Dm", "M1", "M2", "R", "E", "F")


class Sched:
    NDS = 48

    def __init__(self, nc, stack):
        self.nc = nc
        self.esem = {e: stack.enter_context(nc.semaphore(f"s_{e}")) for e in ENGINES}
        self.dsem = [stack.enter_context(nc.semaphore(f"d_{i}")) for i in range(self.NDS)]
        self.ecount = {e: 0 for e in ENGINES}
        self.dcount = [0] * self.NDS
        self.dnext = {"sync": 0, "gpsimd": 0}
        self.reset()

    def reset(self):
        self.ops = []
        self.last_writer = {}
        self.readers = {}

    def add(self, engine, fn, reads=(), writes=(), dma=False):
        deps = set()
        for r in reads:
            if r in self.last_writer:
                deps.add(self.last_writer[r])
        for w in writes:
            if w in self.last_writer:
                deps.add(self.last_writer[w])
            for rd in self.readers.get(w, ()):
                deps.add(rd)
        oid = len(self.ops)
        self.ops.append((engine, fn, deps, dma))
        for r in reads:
            self.readers.setdefault(r, []).append(oid)
        for w in writes:
            self.last_writer[w] = oid
            self.readers[w] = []
        return oid

    def flush(self):
        nc = self.nc
        ops = self.ops
        n = len(ops)
        if n == 0:
            return
        needed = [False] * n
        for (_, _, deps, _) in ops:
            for d in deps:
                needed[d] = True
        last_on = {}
        for i, (e, _, _, dma) in enumerate(ops):
            if not dma:
                last_on[e] = i
        for i in last_on.values():
            needed[i] = True
        comp = [None] * n
        issue_wait = {}
        prev_on_sem = {}
        for i, (eng, fn, deps, dma) in enumerate(ops):
            if dma:
                half = self.NDS // 2
                s = self.dnext[eng] + (half if eng == "gpsimd" else 0)
                self.dnext[eng] = (self.dnext[eng] + 1) % half
                if self.dcount[s] > 0:
                    issue_wait[i] = (s, self.dcount[s])
                self.dcount[s] += 16
                comp[i] = ("d", s, self.dcount[s])
            elif needed[i]:
                self.ecount[eng] += 1
                comp[i] = ("e", eng, self.ecount[eng])
        final_e = dict(self.ecount)
        final_d = list(self.dcount)
        esem, dsem = self.esem, self.dsem

        def run_engine(ename):
            def body(eng):
                waited = {}

                def do_wait(key, semh, val):
                    if waited.get(key, -1) >= val:
                        return
                    eng.wait_ge(semh, val)
                    waited[key] = val

                for i, (e, fn, deps, dma) in enumerate(ops):
                    if e != ename:
                        continue
                    for d in sorted(deps):
                        c = comp[d]
                        if c is None:
                            continue
                        if c[0] == "e":
                            if c[1] == "tensor" and ename == "tensor":
                                continue
                            do_wait(("e", c[1]), esem[c[1]], c[2])
                        else:
                            do_wait(("d", c[1]), dsem[c[1]], c[2])
                    if i in issue_wait:
                        s, v = issue_wait[i]
                        do_wait(("d", s), dsem[s], v)
                    ins = fn(eng)
                    c = comp[i]
                    if c is not None:
                        if c[0] == "e":
                            ins.then_inc(esem[c[1]], 1)
                        else:
                            ins.then_inc(dsem[c[1]], 16)
                for e2 in ENGINES:
                    if final_e[e2] > 0:
                        do_wait(("e", e2), esem[e2], final_e[e2])
                for s in range(self.NDS):
                    if final_d[s] > 0:
                        do_wait(("d", s), dsem[s], final_d[s])
            return body

        with nc.Block() as block:
            block.tensor(run_engine("tensor"))
            block.vector(run_engine("vector"))
            block.scalar(run_engine("scalar"))
            block.gpsimd(run_engine("gpsimd"))
            block.sync(run_engine("sync"))
        self.reset()


class Arena:
    def __init__(self, nc, stack, nbytes):
        self.t = stack.enter_context(nc.sbuf_tensor("arena", [128, nbytes // 4], F32))
        self.nbytes = nbytes
        self.off = 0

    def seek(self, off):
        self.off = off

    def alloc(self, free_shape, dtype):
        n = int(np.prod(free_shape))
        esz = 4 if dtype in (F32, I32) else 2
        nb = (n * esz + 31) // 32 * 32
        assert self.off + nb <= self.nbytes, (self.off, nb, self.nbytes)
        ap = self.t[:, self.off // 4:(self.off + nb) // 4]
        self.off += nb
        if dtype != F32:
            ap = ap.bitcast(dtype)
        ap = ap[:, 0:n]
        if len(free_shape) == 2:
            ap = ap.rearrange("p (a b) -> p a b", b=free_shape[1])
        elif len(free_shape) == 3:
            ap = ap.rearrange("p (a b c) -> p a b c", b=free_shape[1], c=free_shape[2])
        elif len(free_shape) == 4:
            ap = ap.rearrange("p (a b c d) -> p a b c d", b=free_shape[1], c=free_shape[2], d=free_shape[3])
        return ap


def build_program(last_phase="F", debug=()):
    nc = bass.Bass("TRN2", target_bir_lowering=False)
    T = NTS * 128

    def din(name, shape, dt=F32):
        return nc.dram_tensor(name, list(shape), dt, kind="ExternalInput").ap()

    xT = din("xT", [D, T])
    x_own = din("x_own", [NOWN * 128, D])
    memT = din("memT", [D, 256])
    w_in = din("w_in", [D, 5638])
    w_mem_kv = din("w_mem_kv", [D, 512])
    w_br = din("w_br", [D, D])
    w_out = din("w_out", [D, D])
    w_router = din("w_router", [D, NE])
    if PHASES.index(last_phase) >= PHASES.index("E"):
        w_exp_in = din("w_exp_in", [NE, D, 2 * D])
        w_exp_out = din("w_exp_out", [NE, D, D])
    b_exp_out = din("b_exp_out", [NE, D])
    b1T = din("b1T", [128, NE * 16])
    lnrep = din("lnrep", [4, 128, D])
    cst = din("cst", [128, 6 * 128 + 32 + NTS + 24 + 32])
    dilb = din("dilb", [6, 128, 17, 128])
    padrow = din("padrow", [1, NTS * 128])
    out_d = nc.dram_tensor("out", [NOWN * 128, D], F32, kind="ExternalOutput").ap()
    dbg = {}
    for name, shape in debug:
        dbg[name] = nc.dram_tensor(name, list(shape), F32, kind="ExternalOutput").ap()

    KT_scr = nc.dram_tensor("KT_scr", [6, 128, T], BF16, kind="Internal").ap()
    V_scr = nc.dram_tensor("V_scr", [12, 128, NTS, 65], BF16, kind="Internal").ap()
    X_scr = nc.dram_tensor("X_scr", [NE * CAP, D], BF16, kind="Internal").ap()
    Y_scr = nc.dram_tensor("Y_scr", [NE * CAP, D], F32, kind="Internal").ap()
    Gd = nc.dram_tensor("Gd", [6, 3, NTS * 128], BF16, kind="Internal").ap()

    xTv = xT.rearrange("(c p) t -> p c t", p=128)
    xTown = xT.rearrange("(c p) (m j t) -> p c m j t", p=128, j=8, t=128)
    w_inv = w_in.rearrange("(c p) n -> p c n", p=128)

    li = PHASES.index(last_phase)

    def on(ph):
        return PHASES.index(ph) <= li

    with contextlib.ExitStack() as st:
        S = Sched(nc, st)
        A = Arena(nc, st, 206 * 1024)

        def psum(name, shape, dt=F32):
            return st2.enter_context(nc.psum_tensor(name, list(shape), dt))[:]

        A.seek(0)
        cst_sb = A.alloc([6 * 128 + 32 + NTS + 24 + 32], F32)
        ident_f = cst_sb[:, 0:128]
        tri_incl = cst_sb[:, 128:256]
        tri_strict_f = cst_sb[:, 256:384]
        ones_f = cst_sb[:, 384:512]
        e64 = cst_sb[:, 512:640]
        foxtri = cst_sb[:, 640:768]
        iotaC = cst_sb[:, 768:800]
        padb = cst_sb[:, 800:800 + NTS]
        bf_rep4 = cst_sb[:, 800 + NTS:800 + NTS + 24].rearrange("p (a b) -> p a b", b=6)
        br_rep = cst_sb[:, 800 + NTS + 24:800 + NTS + 56]
        ident_b = A.alloc([128], BF16)
        tri_strict_b = A.alloc([128], BF16)
        ones_b = A.alloc([128], BF16)
        foxtri_b = A.alloc([128], BF16)
        G_all = A.alloc([NTS, 6], F32)
        Gref = A.alloc([NOWN, 6], F32)
        dest_i = A.alloc([NOWN, 4], I32)
        gate4 = A.alloc([NOWN, 4], F32)
        assert A.off <= 12 * 1024, A.off
        OFF_QT = 12 * 1024
        OFF_O = OFF_QT + 44 * 1024
        OFF_LOC = OFF_O + 32 * 1024
        OFF_LOC2 = OFF_O + 64 * 1024
        A.seek(OFF_QT)
        QTf = A.alloc([6, NOWN * 128], BF16)
        QT = A.alloc([5, NOWN * 128], BF16)
        A.seek(OFF_QT)
        mergedT = A.alloc([8, NOWN * 128], BF16)
        A.seek(OFF_O)
        o_fox = A.alloc([NOWN, 384], BF16)
        o_dil = A.alloc([NOWN, 384], BF16)
        o_mem = A.alloc([NOWN, 256], BF16)
        A.seek(OFF_O)
        h1 = A.alloc([NOWN, D], F32)

        S.add("sync", lambda e: e.dma_start(out=cst_sb, in_=cst), writes=["cst"], dma=True)
        S.add("vector", lambda e: e.tensor_copy(out=ident_b, in_=ident_f), reads=["cst"], writes=["ident_b"])
        S.add("vector", lambda e: e.tensor_copy(out=tri_strict_b, in_=tri_strict_f), reads=["cst"], writes=["tsb"])
        S.add("vector", lambda e: e.tensor_copy(out=ones_b, in_=ones_f), reads=["cst"], writes=["ones_b"])
        S.add("vector", lambda e: e.tensor_copy(out=foxtri_b, in_=foxtri), reads=["cst"], writes=["foxtri_b"])
        S.flush()

        def evac(i, out, in_, reads, writes, scale=None):
            if i % 2 == 0:
                if scale is None:
                    S.add("scalar", lambda e: e.copy(out=out, in_=in_), reads, writes)
                else:
                    S.add("scalar", lambda e: e.mul(out=out, in_=in_, mul=scale), reads, writes)
            else:
                if scale is None:
                    S.add("vector", lambda e: e.tensor_copy(out=out, in_=in_), reads, writes)
                else:
                    S.add("vector", lambda e: e.tensor_scalar(out=out, in0=in_, scalar1=scale, scalar2=None, op0=ALU.mult), reads, writes)

        def mm(out, lhsT, rhs, start, stop, reads, writes):
            S.add("tensor", lambda e: e.matmul(out, lhsT, rhs, start=start, stop=stop), reads, writes)

        if on("A"):
            with contextlib.ExitStack() as st2:
                A.seek(OFF_LOC)
                Wk = A.alloc([8, 768], BF16)
                Wv = A.alloc([8, 768], BF16)
                Wf = A.alloc([8, 6], BF16)
                xbuf = [A.alloc([8, 512], BF16) for _ in range(2)]
                ktst = [A.alloc([6, 512], BF16) for _ in range(2)]
                vst = [A.alloc([12, 4, 65], BF16) for _ in range(2)]
                Z_all = A.alloc([NTS, 6], F32)
                SP_all = A.alloc([NTS, 6], F32)
                Wsb = A.alloc([NTS, 6], F32)
                Tsb = A.alloc([NTS, 6], F32)
                HA = A.alloc([NTS, 6], F32)
                HB = A.alloc([NTS, 6], F32)
                zero_t = A.alloc([8, D], BF16)
                Gp = [A.alloc([NTS, 6], BF16) for _ in range(3)]
                R1 = A.alloc([NTS, 6], F32)
                R2 = A.alloc([NTS, 6], F32)
                gst = [A.alloc([128], BF16) for _ in range(2)]
                psGT = psum("psGT", [128, 128], BF16)
                psK = [psum(f"psK{i}", [128, 512]) for i in range(2)]
                psV = [psum(f"psV{i}", [128, 384]) for i in range(2)]
                psF = psum("psF", [128, 4, 6])
                psW = [psum(f"psW{i}", [128, 408]) for i in range(2)]

                S.add("vector", lambda e: e.memset(zero_t, 0.0), writes=["zero_t"])
                Xz = X_scr.rearrange("(a b p) d -> a p b d", b=8, p=128)
                for a in range(NE * CAP // 1024):
                    S.add("sync", lambda e, a=a: e.dma_start(out=Xz[a], in_=zero_t), reads=["zero_t"], writes=[("X_scr", a)], dma=True)

                for (dst, lo) in ((Wk[:, :, 0:384], 384), (Wk[:, :, 384:768], 1542), (Wv[:, :, 0:384], 768), (Wv[:, :, 384:768], 1926)):
                    S.add("gpsimd", lambda e, dst=dst, lo=lo: e.dma_start(out=dst, in_=w_inv[:, :, lo:lo + 384]), writes=["WA"], dma=True)
                S.add("gpsimd", lambda e: e.dma_start(out=Wf, in_=w_inv[:, :, 1152:1158]), writes=["WA"], dma=True)
                for b in range(2):
                    S.add("vector", lambda e, b=b: e.memset(vst[b][:, :, :, 64:65], 1.0), writes=[("vst1", b)])

                KTv = KT_scr.rearrange("r p t -> p r t")
                Vv = V_scr.rearrange("h p t c -> p h t c")
                ev = 0
                for tb in range(NTS // 4):
                    xb = xbuf[tb % 2]
                    rx = ("xb", tb % 2)
                    S.add("gpsimd", lambda e, xb=xb, tb=tb: e.dma_start(out=xb, in_=xTv[:, :, tb * 512:(tb + 1) * 512]), writes=[rx], dma=True)
                    kt = ktst[tb % 2]
                    rk = ("ktst", tb % 2)
                    for pr in range(6):
                        ps = psK[pr % 2]
                        rp = ("psK", pr % 2)
                        for c in range(8):
                            mm(ps, Wk[:, c, pr * 128:(pr + 1) * 128], xb[:, c, :], c == 0, c == 7, [rx, "WA"], [rp])
                        evac(ev, kt[:, pr, :], ps, [rp], [rk]); ev += 1
                    S.add("sync", lambda e, kt=kt, tb=tb: e.dma_start(out=KTv[:, :, tb * 512:(tb + 1) * 512], in_=kt), reads=[rk], writes=[("KT_scr", tb)], dma=True)
                    vs = vst[tb % 2]
                    rv = ("vst", tb % 2)
                    for t4 in range(4):
                        for grp in range(2):
                            ps = psV[grp]
                            rp = ("psV", grp)
                            for c in range(8):
                                mm(ps, xb[:, c, t4 * 128:(t4 + 1) * 128], Wv[:, c, grp * 384:(grp + 1) * 384], c == 0, c == 7, [rx, "WA"], [rp])
                            evac(ev, vs[:, grp * 6:(grp + 1) * 6, t4, 0:64], ps.rearrange("p (h d) -> p h d", d=64), [rp], [rv, ("vst1", tb % 2)]); ev += 1
                    S.add("sync", lambda e, vs=vs, tb=tb: e.dma_start(out=Vv[:, :, tb * 4:(tb + 1) * 4, :], in_=vs), reads=[rv, ("vst1", tb % 2)], writes=[("V_scr", tb)], dma=True)
                    for t4 in range(4):
                        for c in range(8):
                            mm(psF[:, t4, :], xb[:, c, t4 * 128:(t4 + 1) * 128], Wf[:, c, :], c == 0, c == 7, [rx, "WA"], ["psF"])
                    S.add("vector", lambda e, tb=tb: e.tensor_tensor(out=Z_all[:, tb * 4:(tb + 1) * 4, :], in0=psF, in1=bf_rep4, op=ALU.add), reads=["psF", "cst"], writes=["Z_all"])

                Zf = Z_all.rearrange("p a b -> p (a b)")
                SPf = SP_all.rearrange("p a b -> p (a b)")
                S.add("scalar", lambda e: e.activation(out=SPf, in_=Zf, func=AF.Exp, scale=-1.0), reads=["Z_all"], writes=["SP"])
                S.add("scalar", lambda e: e.activation(out=SPf, in_=SPf, func=AF.Ln, bias=1.0, scale=1.0), reads=["SP"], writes=["SP"])
                Wf2 = Wsb.rearrange("p a b -> p (a b)")
                Tf2 = Tsb.rearrange("p a b -> p (a b)")
                for hf in range(2):
                    mm(psW[hf], tri_incl, SPf[:, hf * 408:(hf + 1) * 408], True, True, ["SP", "cst"], [("psW", hf)])
                    S.add("vector", lambda e, hf=hf: e.tensor_copy(out=Wf2[:, hf * 408:(hf + 1) * 408], in_=psW[hf]), reads=[("psW", hf)], writes=["Wsb"])
                for hf in range(2):
                    mm(psW[hf], ones_f, SPf[:, hf * 408:(hf + 1) * 408], True, True, ["SP", "cst"], [("psW", hf)])
                    S.add("vector", lambda e, hf=hf: e.tensor_copy(out=Tf2[:, hf * 408:(hf + 1) * 408], in_=psW[hf]), reads=[("psW", hf)], writes=["Tsb"])
                S.add("vector", lambda e: e.tensor_copy(out=HA, in_=Tsb), reads=["Tsb"], writes=["HA"])
                cur, nxt, rc, rn = HA, HB, "HA", "HB"
                sh = 1
                while sh < NTS:
                    S.add("vector", lambda e, cur=cur, nxt=nxt, sh=sh: e.tensor_copy(out=nxt[:, 0:sh, :], in_=cur[:, 0:sh, :]), reads=[rc], writes=[rn])
                    S.add("vector", lambda e, cur=cur, nxt=nxt, sh=sh: e.tensor_tensor(out=nxt[:, sh:NTS, :], in0=cur[:, sh:NTS, :], in1=cur[:, 0:NTS - sh, :], op=ALU.add), reads=[rc], writes=[rn])
                    cur, nxt, rc, rn = nxt, cur, rn, rc
                    sh *= 2
                S.add("vector", lambda e, cur=cur: e.tensor_tensor(out=G_all, in0=cur, in1=Tsb, op=ALU.subtract), reads=[rc, "Tsb"], writes=["G_all"])
                S.add("vector", lambda e: e.tensor_tensor(out=G_all, in0=G_all, in1=Wsb, op=ALU.add), reads=["G_all", "Wsb"], writes=["G_all"])
                Gown = G_all.rearrange("p (m j) h -> p m j h", j=8)[:, 0:NOWN, 7, :]
                S.add("vector", lambda e: e.tensor_copy(out=HA[:, 0:NOWN, :], in_=Gown), reads=["G_all"], writes=["HA", "HB"])
                mm(psW[0][:, 0:NOWN * 6], e64, HA[:, 0:NOWN, :].rearrange("p a b -> p (a b)"), True, True, ["HA", "cst"], [("psW", 0)])
                S.add("vector", lambda e: e.tensor_copy(out=Gref.rearrange("p a b -> p (a b)"), in_=psW[0][:, 0:NOWN * 6]), reads=[("psW", 0)], writes=["Gref"])
                S.add("vector", lambda e: e.tensor_copy(out=Gp[0], in_=G_all), reads=["G_all"], writes=["Gp0"])
                S.add("vector", lambda e: e.tensor_tensor(out=R1, in0=G_all, in1=Gp[0], op=ALU.subtract), reads=["G_all", "Gp0"], writes=["R1"])
                S.add("vector", lambda e: e.tensor_copy(out=Gp[1], in_=R1), reads=["R1"], writes=["Gp1"])
                S.add("vector", lambda e: e.tensor_tensor(out=R2, in0=R1, in1=Gp[1], op=ALU.subtract), reads=["R1", "Gp1"], writes=["R2"])
                S.add("vector", lambda e: e.tensor_copy(out=Gp[2], in_=R2), reads=["R2"], writes=["Gp2"])
                Gdv = Gd.rearrange("h r (s p) -> h r s p", p=128)
                kk = 0
                for h in range(6):
                    for part in range(3):
                        for (s0, ns) in ((0, 128), (128, NTS - 128)):
                            stg = gst[kk % 2]
                            rst = ("gst", kk % 2)
                            S.add("tensor", lambda e, h=h, part=part, s0=s0, ns=ns: e.transpose(psGT[0:ns, :], Gp[part][:, s0:s0 + ns, h], ident_b), reads=[f"Gp{part}", "ident_b"], writes=["psGT"])
                            evac(kk, stg[0:ns, :], psGT[0:ns, :], ["psGT"], [rst])
                            S.add("sync", lambda e, h=h, part=part, s0=s0, ns=ns, stg=stg: e.dma_start(out=Gdv[h, part, s0:s0 + ns, :], in_=stg[0:ns, :]), reads=[rst], writes=[("Gd", h, part, s0)], dma=True)
                            kk += 1
                if "G_all" in dbg:
                    S.add("sync", lambda e: e.dma_start(out=dbg["G_all"], in_=G_all.rearrange("p a b -> p (a b)")), reads=["G_all"], writes=["dbgG"], dma=True)
                S.flush()

        if on("Q"):
            with contextlib.ExitStack() as st2:
                A.seek(OFF_LOC)
                Wq = A.alloc([8, 1024], BF16)
                xo = A.alloc([8, NOWN, 128], BF16)
                psQ = [psum(f"psQ{i}", [128, 512]) for i in range(2)]
                for (lo_dst, lo, n) in ((0, 0, 384), (384, 1158, 384), (768, 2310, 256)):
                    S.add("gpsimd", lambda e, lo_dst=lo_dst, lo=lo, n=n: e.dma_start(out=Wq[:, :, lo_dst:lo_dst + n], in_=w_inv[:, :, lo:lo + n]), writes=["Wq"], dma=True)
                for c in range(8):
                    S.add("gpsimd", lambda e, c=c: e.dma_start(out=xo[:, c, :, :], in_=xTown[:, c, 0:NOWN, 7, :]), writes=["xo"], dma=True)
                xof = xo.rearrange("p c m t -> p c (m t)")
                k = 0
                for b4 in range(4):
                    for h in range(6):
                        ps = psQ[k % 2]
                        for c in range(8):
                            mm(ps[0:64, :], Wq[:, c, h * 64:(h + 1) * 64], xof[:, c, b4 * 512:(b4 + 1) * 512], c == 0, c == 7, ["Wq", "xo"], [("psQ", k % 2)])
                        evac(k, QTf[0:64, h, b4 * 512:(b4 + 1) * 512], ps[0:64, :], [("psQ", k % 2)], ["QTf"], scale=0.125)
                        k += 1
                    for pr in range(5):
                        ps = psQ[k % 2]
                        for c in range(8):
                            mm(ps, Wq[:, c, 384 + pr * 128:384 + (pr + 1) * 128], xof[:, c, b4 * 512:(b4 + 1) * 512], c == 0, c == 7, ["Wq", "xo"], [("psQ", k % 2)])
                        evac(k, QT[:, pr, b4 * 512:(b4 + 1) * 512], ps, [("psQ", k % 2)], ["QT"], scale=0.125)
                        k += 1
                S.add("vector", lambda e: e.memset(QTf[64:69, :, :], 1.0), writes=["QTfa"])
                for h in range(6):
                    for m in range(NOWN):
                        S.add("vector", lambda e, h=h, m=m: e.tensor_scalar(out=QTf[64:65, h, m * 128:(m + 1) * 128], in0=ones_f[64:65, :], scalar1=Gref[64:65, m, h:h + 1], scalar2=-1.0,
                                                                    op0=ALU.mult, op1=ALU.mult), reads=["Gref", "cst", "QTfa"], writes=["QTfa"])
                S.flush()

        def load_kv(KTb, Vb, pr, hp, vh, par, ntiles):
            rk, rv = ("KTb", par), ("Vb", par)
            q4 = (ntiles * 128) // 4
            for q in range(4):
                S.add("sync", lambda e, q=q: e.dma_start(out=KTb[hp:hp + 64, q * q4:(q + 1) * q4], in_=KT_scr[pr, hp:hp + 64, q * q4:(q + 1) * q4]), writes=[rk], dma=True)
            S.add("sync", lambda e: e.dma_start(out=Vb[:, 0:ntiles, :], in_=V_scr[vh, :, 0:ntiles, :]), writes=[rv], dma=True)
            return rk, rv

        class AttnPipe:
            LA = 2

            def __init__(self, psS, Pt, ssb, psO):
                self.psS, self.Pt, self.ssb, self.psO = psS, Pt, ssb, psO
                self.cnt = 0
                self.acc_i = 0
                self.pending = []

            def _drain(self, keep):
                while len(self.pending) > keep:
                    self.pending.pop(0)()

            def run(self, items, finish):
                acc = self.psO[self.acc_i % 2]
                racc = ("psO", self.acc_i % 2)
                self.acc_i += 1
                n = len(items)
                for idx, it in enumerate(items):
                    slot = self.cnt % 8
                    sslot = self.cnt % 4
                    self.cnt += 1
                    ps = self.psS[sslot]
                    rps = ("psS", sslot)
                    P = self.Pt[:, slot, :]
                    rP = ("P", slot)
                    mm(ps, it["lhsT"], it["rhs"], True, True, it["reads"], [rps])
                    src, rsrc = ps, rps
                    if it["mask"] is not None:
                        sb = self.ssb[:, slot % 4, :]
                        rsb = ("ssb", slot % 4)
                        S.add("vector", lambda e, sb=sb, ps=ps, m=it["mask"]: e.tensor_tensor(out=sb, in0=ps, in1=m, op=ALU.add), reads=[rps] + it["mreads"], writes=[rsb])
                        src, rsrc = sb, rsb
                    if it["bias"] is not None:
                        S.add("scalar", lambda e, P=P, src=src, b=it["bias"]: e.activation(out=P, in_=src, func=AF.Exp, bias=b, scale=1.0), reads=[rsrc] + it["breads"], writes=[rP])
                    else:
                        S.add("scalar", lambda e, P=P, src=src: e.activation(out=P, in_=src, func=AF.Exp), reads=[rsrc], writes=[rP])

                    def pv(P=P, rP=rP, it=it, first=(idx == 0), last=(idx == n - 1), acc=acc, racc=racc):
                        mm(acc, P, it["v"], first, last, [rP] + it["vreads"], [racc])
                        if last:
                            finish(acc, racc)
                    self.pending.append(pv)
                    self._drain(self.LA)

            def flush(self):
                self._drain(0)

        if on("B"):
            with contextlib.ExitStack() as st2:
                A.seek(OFF_LOC)
                KTb = [A.alloc([NTS * 128], BF16) for _ in range(2)]
                Vb = [A.alloc([NTS, 65], BF16) for _ in range(2)]
                NPB = 2
                Pt = [A.alloc([1024], BF16) for _ in range(NPB)]
                rec = A.alloc([8], F32)
                psS = [psum(f"psS{i}", [128, 1024]) for i in range(NPB)]
                psO = [psum(f"psO{i}", [128, 65]) for i in range(4)]
                fin_i = [0]
                if "o_fox" in dbg:
                    S.add("vector", lambda e: e.memset(o_fox, 0.0), writes=["o_attn"])
                pending = []
                gcnt = [0]
                acc_i = [0]

                def drain(keep):
                    while len(pending) > keep:
                        pending.pop(0)()

                def finish(acc, racc, dst):
                    r = rec[:, fin_i[0] % 8:fin_i[0] % 8 + 1]
                    rr = ("rec", fin_i[0] % 8)
                    fin_i[0] += 1
                    S.add("vector", lambda e: e.reciprocal(out=r, in_=acc[:, 64:65]), reads=[racc], writes=[rr])
                    S.add("vector", lambda e: e.tensor_scalar(out=dst, in0=acc[:, 0:64], scalar1=r, scalar2=None, op0=ALU.mult), reads=[racc, rr], writes=["o_attn"])

                for h in range(6):
                    par = h % 2
                    hp = (h % 2) * 64
                    pr = h // 2
                    KA = KTb[par]
                    rk, rv = ("KTb", par), ("Vb", par)
                    q4 = (NTS * 128) // 4
                    for q in range(4):
                        S.add("sync", lambda e, q=q, KA=KA, pr=pr, hp=hp: e.dma_start(out=KA[0:64, q * q4:(q + 1) * q4], in_=KT_scr[pr, hp:hp + 64, q * q4:(q + 1) * q4]), writes=[rk], dma=True)
                    S.add("vector", lambda e, KA=KA: e.memset(KA[64:65, :], 1.0), writes=[rk])
                    S.add("sync", lambda e, KA=KA, h=h: e.dma_start(out=KA[65:68, :], in_=Gd[h]), writes=[rk], dma=True)
                    S.add("gpsimd", lambda e, KA=KA: e.dma_start(out=KA[68:69, :], in_=padrow), writes=[rk], dma=True)
                    S.add("sync", lambda e, par=par, h=h: e.dma_start(out=Vb[par], in_=V_scr[h]), writes=[rv], dma=True)
                    for mp_ in range(NOWN // 2):
                        m0, m1 = 2 * mp_, 2 * mp_ + 1
                        J0, J1 = 8 * m0 + 8, 8 * m1 + 8
                        acc0 = psO[acc_i[0] % 4]; racc0 = ("psO", acc_i[0] % 4); acc_i[0] += 1
                        acc1 = psO[acc_i[0] % 4]; racc1 = ("psO", acc_i[0] % 4); acc_i[0] += 1
                        dst0 = o_fox[:, m0, h * 64:(h + 1) * 64]
                        dst1 = o_fox[:, m1, h * 64:(h + 1) * 64]
                        qrd = [rk, "QTf", "QTfa"]
                        for g in range(J0 // 4):
                            b = gcnt[0] % NPB
                            gcnt[0] += 1
                            ps = psS[b].rearrange("p (a q) -> p a q", q=256)
                            rps = ("psS", b)
                            P = Pt[b].rearrange("p (a q) -> p a q", q=256)
                            rP = ("P", b)
                            for jj in range(4):
                                j = 4 * g + jj
                                kt = KA[0:69, j * 128:(j + 1) * 128]
                                if j == J0 - 1:
                                    mm(ps[:, jj, 0:128], kt, QTf[0:69, h, m0 * 128:(m0 + 1) * 128], True, False, qrd, [rps])
                                    mm(ps[:, jj, 0:128], ident_b, foxtri_b, False, True, ["ident_b", "foxtri_b"], [rps])
                                    mm(ps[:, jj, 128:256], kt, QTf[0:69, h, m1 * 128:(m1 + 1) * 128], True, True, qrd, [rps])
                                else:
                                    mm(ps[:, jj, :], kt, QTf[0:69, h, m0 * 128:(m0 + 2) * 128], True, True, qrd, [rps])
                            S.add("scalar", lambda e, b=b: e.activation(out=Pt[b], in_=psS[b], func=AF.Exp), reads=[rps], writes=[rP])

                            def pv(g=g, P=P, rP=rP, acc0=acc0, racc0=racc0, acc1=acc1, racc1=racc1, J0=J0, par=par, rv=rv, dst0=dst0):
                                for jj in range(4):
                                    j = 4 * g + jj
                                    mm(acc0, P[:, jj, 0:128], Vb[par][:, j, :], j == 0, j == J0 - 1, [rP, rv], [racc0])
                                    mm(acc1, P[:, jj, 128:256], Vb[par][:, j, :], j == 0, False, [rP, rv], [racc1])
                                if 4 * g + 3 == J0 - 1:
                                    finish(acc0, racc0, dst0)
                            pending.append(pv)
                            drain(1)
                        for g in range(1):
                            b = gcnt[0] % NPB
                            gcnt[0] += 1
                            ps = psS[b].rearrange("p (a q) -> p a q", q=128)
                            rps = ("psS", b)
                            P = Pt[b].rearrange("p (a q) -> p a q", q=128)
                            rP = ("P", b)
                            for jj in range(8):
                                j = J0 + jj
                                diag = (j == J1 - 1)
                                mm(ps[:, jj, :], KA[0:69, j * 128:(j + 1) * 128], QTf[0:69, h, m1 * 128:(m1 + 1) * 128], True, not diag, qrd, [rps])
                                if diag:
                                    mm(ps[:, jj, :], ident_b, foxtri_b, False, True, ["ident_b", "foxtri_b"], [rps])
                            S.add("scalar", lambda e, b=b: e.activation(out=Pt[b], in_=psS[b], func=AF.Exp), reads=[rps], writes=[rP])

                            def pv2(g=g, P=P, rP=rP, acc1=acc1, racc1=racc1, J0=J0, J1=J1, par=par, rv=rv, dst1=dst1):
                                for jj in range(8):
                                    j = J0 + jj
                                    mm(acc1, P[:, jj, :], Vb[par][:, j, :], False, j == J1 - 1, [rP, rv], [racc1])
                                finish(acc1, racc1, dst1)
                            pending.append(pv2)
                            drain(1)
                drain(0)
                if "o_fox" in dbg:
                    A.seek(OFF_LOC)
                    tmp = A.alloc([NOWN, 384], F32)
                    S.add("vector", lambda e: e.tensor_copy(out=tmp, in_=o_fox), reads=["o_attn", ("KTb", 0), ("KTb", 1)], writes=[("KTb", 0)])
                    S.add("sync", lambda e: e.dma_start(out=dbg["o_fox"].rearrange("(m p) d -> p m d", p=128), in_=tmp), reads=[("KTb", 0)], writes=["dbgo"], dma=True)
                S.flush()

        if on("C"):
            with contextlib.ExitStack() as st2:
                A.seek(OFF_LOC)
                KTb = [A.alloc([NTS * 128], BF16)]
                Vb = [A.alloc([NTS, 65], BF16) for _ in range(2)]
                dbt = [A.alloc([17, 128], F32) for _ in range(2)]
                loc_save = A.off
                A.seek(OFF_QT)
                num = A.alloc([NOWN, 384], F32)
                A.seek(loc_save)
                den = A.alloc([NOWN, 6], F32)
                dsum = A.alloc([NOWN, 2], F32)
                NPB = 3
                Pt = [A.alloc([4, 128], BF16) for _ in range(NPB)]
                ssb = [A.alloc([4, 128], F32) for _ in range(NPB)]
                psS = [psum(f"psSc{i}", [128, 4, 128]) for i in range(NPB)]
                psO = [psum(f"psOc{i}", [128, 65]) for i in range(2)]
                pending = []
                gcnt = [0]
                acc_i = [0]

                def drain_c(keep):
                    while len(pending) > keep:
                        pending.pop(0)()

                def finish_c(acc, racc, m, hd):
                    S.add("vector", lambda e: e.tensor_copy(out=num[:, m, hd * 64:(hd + 1) * 64], in_=acc[:, 0:64]), reads=[racc], writes=["num"])
                    S.add("vector", lambda e: e.tensor_copy(out=den[:, m, hd:hd + 1], in_=acc[:, 64:65]), reads=[racc], writes=["den"])

                def load_v_c(hd_):
                    S.add("sync", lambda e, hd_=hd_: e.dma_start(out=Vb[hd_ % 2], in_=V_scr[6 + hd_]), writes=[("Vbc", hd_ % 2)], dma=True)
                    S.add("sync", lambda e, hd_=hd_: e.dma_start(out=dbt[hd_ % 2], in_=dilb[hd_]), writes=[("dbt", hd_ % 2)], dma=True)

                load_v_c(0)
                for hd in range(6):
                    par = 0
                    dpar = hd % 2
                    hp = (hd % 2) * 64
                    pr = 3 + hd // 2
                    prq = hd // 2
                    rk = ("KTb", 0)
                    q4 = (NTS * 128) // 4
                    for q in range(4):
                        S.add("sync", lambda e, q=q, pr=pr, hp=hp: e.dma_start(out=KTb[0][hp:hp + 64, q * q4:(q + 1) * q4], in_=KT_scr[pr, hp:hp + 64, q * q4:(q + 1) * q4]), writes=[rk], dma=True)
                    if hd + 1 < 6:
                        load_v_c(hd + 1)
                    rv = ("Vbc", dpar)
                    rd = ("dbt", dpar)
                    for m in range(NOWN):
                        sm = 8 * m + 7
                        dls = [dl for dl in range(DMAX[hd // 2] + 1) if sm - dl >= 0]
                        batches = [dls[i:i + 4] for i in range(0, len(dls), 4)]
                        acc = psO[acc_i[0] % 2]
                        racc = ("psO", acc_i[0] % 2)
                        acc_i[0] += 1
                        nbt = len(batches)
                        for bi, bt in enumerate(batches):
                            b = gcnt[0] % NPB
                            gcnt[0] += 1
                            ps, sb, P = psS[b], ssb[b], Pt[b]
                            rps, rsb, rP = ("psS", b), ("ssb", b), ("P", b)
                            nb = len(bt)
                            for t, dl in enumerate(bt):
                                j = sm - dl
                                mm(ps[:, t, :], KTb[par][hp:hp + 64, j * 128:(j + 1) * 128], QT[hp:hp + 64, prq, m * 128:(m + 1) * 128], True, True, [rk, "QT"], [rps])
                            S.add("vector", lambda e, sb=sb, ps=ps, nb=nb, d0=bt[0], dpar=dpar: e.tensor_tensor(out=sb[:, 0:nb, :], in0=ps[:, 0:nb, :], in1=dbt[dpar][:, d0:d0 + nb, :], op=ALU.add),
                                  reads=[rps, rd], writes=[rsb])
                            for t, dl in enumerate(bt):
                                j = sm - dl
                                if j <= 6:
                                    S.add("vector", lambda e, sb=sb, t=t, j=j: e.tensor_scalar(out=sb[:, t, :], in0=sb[:, t, :], scalar1=padb[:, j:j + 1], scalar2=None, op0=ALU.add), reads=[rsb, "cst"], writes=[rsb])
                            S.add("scalar", lambda e, P=P, sb=sb, nb=nb: e.activation(out=P[:, 0:nb, :], in_=sb[:, 0:nb, :], func=AF.Exp), reads=[rsb], writes=[rP])

                            def pv(bt=bt, P=P, rP=rP, acc=acc, racc=racc, bi=bi, nbt=nbt, sm=sm, par=par, dpar=dpar, rv=rv, m=m, hd=hd):
                                for t, dl in enumerate(bt):
                                    j = sm - dl
                                    mm(acc, P[:, t, :], Vb[dpar][:, j, :], (bi == 0 and t == 0), (bi == nbt - 1 and t == len(bt) - 1), [rP, rv], [racc])
                                if bi == nbt - 1:
                                    finish_c(acc, racc, m, hd)
                            pending.append(pv)
                            drain_c(1)
                drain_c(0)
                S.add("vector", lambda e: e.tensor_tensor(out=dsum, in0=den[:, :, 0:2], in1=den[:, :, 2:4], op=ALU.add), reads=["den"], writes=["dsum"])
                S.add("vector", lambda e: e.tensor_tensor(out=dsum, in0=dsum, in1=den[:, :, 4:6], op=ALU.add), reads=["den", "dsum"], writes=["dsum"])
                S.add("vector", lambda e: e.reciprocal(out=dsum, in_=dsum), reads=["dsum"], writes=["dsum"])
                for m in range(NOWN):
                    for hd in range(6):
                        S.add("vector", lambda e, m=m, hd=hd: e.tensor_scalar(out=o_dil[:, m, hd * 64:(hd + 1) * 64], in0=num[:, m, hd * 64:(hd + 1) * 64],
                                                                      scalar1=dsum[:, m, hd % 2:hd % 2 + 1], scalar2=None, op0=ALU.mult), reads=["num", "dsum"], writes=["o_attn"])
                S.flush()

        if on("Dm"):
            with contextlib.ExitStack() as st2:
                A.seek(OFF_LOC)
                Wkv = A.alloc([8, 512], BF16)
                mT = A.alloc([8, 256], BF16)
                KmT = A.alloc([2, 256], BF16)
                Vm = A.alloc([2, 4, 65], BF16)
                Pt = A.alloc([8, 128], BF16)
                ssb = A.alloc([4, 128], F32)
                rec = A.alloc([8], F32)
                psS = [psum(f"psSm{i}", [128, 128]) for i in range(4)]
                psO = [psum(f"psOm{i}", [128, 65]) for i in range(2)]
                psM = [psum(f"psM{i}", [128, 256]) for i in range(2)]
                S.add("gpsimd", lambda e: e.dma_start(out=Wkv, in_=w_mem_kv.rearrange("(c p) n -> p c n", p=128)), writes=["Wkv"], dma=True)
                S.add("gpsimd", lambda e: e.dma_start(out=mT, in_=memT.rearrange("(c p) n -> p c n", p=128)), writes=["mT"], dma=True)
                assert A.off <= OFF_LOC + 54 * 1024
                A.seek(OFF_LOC + 54 * 1024)
                Wg = A.alloc([8, 3072], BF16)
                Wbr = A.alloc([8, D], BF16)
                for b in range(3):
                    S.add("gpsimd", lambda e, b=b: e.dma_start(out=Wg[:, :, b * 1024:(b + 1) * 1024], in_=w_inv[:, :, 2566 + b * 1024:2566 + (b + 1) * 1024]), writes=["Wg"], dma=True)
                S.add("gpsimd", lambda e: e.dma_start(out=Wbr, in_=w_br.rearrange("(c p) n -> p c n", p=128)), writes=["Wbr"], dma=True)
                S.add("vector", lambda e: e.memset(Vm[:, :, :, 64:65], 1.0), writes=["Vm1"])
                for pr in range(2):
                    for c in range(8):
                        mm(psM[pr], Wkv[:, c, pr * 128:(pr + 1) * 128], mT[:, c, :], c == 0, c == 7, ["Wkv", "mT"], [("psM", pr)])
                    evac(pr, KmT[:, pr, :], psM[pr], [("psM", pr)], ["KmT"])
                for t2 in range(2):
                    for c in range(8):
                        mm(psM[t2], mT[:, c, t2 * 128:(t2 + 1) * 128], Wkv[:, c, 256:512], c == 0, c == 7, ["Wkv", "mT"], [("psM", t2)])
                    evac(t2, Vm[:, t2, :, 0:64], psM[t2].rearrange("p (h d) -> p h d", d=64), [("psM", t2)], ["Vm", "Vm1"])
                pipe = AttnPipe(psS, Pt, ssb, psO)
                fin_i = [0]

                def make_finish_m(dst):
                    def finish(acc, racc):
                        r = rec[:, fin_i[0] % 8:fin_i[0] % 8 + 1]
                        rr = ("rec", fin_i[0] % 8)
                        fin_i[0] += 1
                        S.add("vector", lambda e: e.reciprocal(out=r, in_=acc[:, 64:65]), reads=[racc], writes=[rr])
                        S.add("vector", lambda e: e.tensor_scalar(out=dst, in0=acc[:, 0:64], scalar1=r, scalar2=None, op0=ALU.mult), reads=[racc, rr], writes=["o_attn"])
                    return finish

                for hm in range(4):
                    hp = (hm % 2) * 64
                    pr = hm // 2
                    for m in range(NOWN):
                        items = []
                        for t2 in range(2):
                            items.append(dict(
                                lhsT=KmT[hp:hp + 64, pr, t2 * 128:(t2 + 1) * 128], rhs=QT[hp:hp + 64, 3 + pr, m * 128:(m + 1) * 128],
                                reads=["KmT", "QT"], mask=None, mreads=[], bias=None, breads=[], v=Vm[:, t2, hm, :], vreads=["Vm", "Vm1"]))
                        pipe.run(items, make_finish_m(o_mem[:, m, hm * 64:(hm + 1) * 64]))
                pipe.flush()
                S.flush()

        if on("M1"):
            with contextlib.ExitStack() as st2:
                A.seek(OFF_LOC + 54 * 1024)
                Wg = A.alloc([8, 3072], BF16)
                Wbr = A.alloc([8, D], BF16)
                A.seek(OFF_LOC2 + 4 * 1024)
                Wout = A.alloc([8, D], BF16)
                S.add("gpsimd", lambda e: e.dma_start(out=Wout, in_=w_out.rearrange("(c p) n -> p c n", p=128)), writes=["Wout"], dma=True)
                A.seek(OFF_LOC)
                xbb = [A.alloc([8, 4, 128], BF16) for _ in range(2)]
                oT = A.alloc([8, 512], BF16)
                sg = [A.alloc([512], F32) for _ in range(3)]
                t1 = A.alloc([512], F32)
                t2b = A.alloc([512], F32)
                assert A.off <= OFF_LOC2 + 4 * 1024, A.off
                psT = psum("psT", [128, 512], BF16)
                psB = [psum(f"psB{i}", [128, 512]) for i in range(3)]
                psG = [psum(f"psG{i}", [128, 512]) for i in range(2)]
                ev = 0
                gk = 0
                for b4 in range(4):
                    xb = xbb[b4 % 2]
                    rx = ("xbb", b4 % 2)
                    for c in range(8):
                        S.add("gpsimd", lambda e, xb=xb, b4=b4, c=c: e.dma_start(out=xb[:, c, :, :], in_=xTown[:, c, 4 * b4:4 * b4 + 4, 7, :]), writes=[rx], dma=True)
                    xbf = xb.rearrange("p c m t -> p c (m t)")
                    for ch in range(8):
                        for mm_ in range(4):
                            m = 4 * b4 + mm_
                            if ch < 3:
                                src = o_fox[:, m, ch * 128:(ch + 1) * 128]
                            elif ch < 6:
                                src = o_dil[:, m, (ch - 3) * 128:(ch - 2) * 128]
                            else:
                                src = o_mem[:, m, (ch - 6) * 128:(ch - 5) * 128]
                            S.add("tensor", lambda e, src=src, mm_=mm_: e.transpose(psT[:, mm_ * 128:(mm_ + 1) * 128], src, ident_b), reads=["o_attn", "ident_b"], writes=["psT"])
                        evac(ev, oT[:, ch, :], psT, ["psT"], ["oT"]); ev += 1
                    for dc in range(8):
                        for bi, chs in enumerate(((0, 1, 2), (3, 4, 5), (6, 7))):
                            for ci, ch in enumerate(chs):
                                mm(psB[bi], Wbr[:, ch, dc * 128:(dc + 1) * 128], oT[:, ch, :], ci == 0, ci == len(chs) - 1, ["Wbr", "oT"], [("psB", bi)])
                        for b in range(3):
                            pg = psG[gk % 2]
                            rg = ("psG", gk % 2)
                            gk += 1
                            for c in range(8):
                                mm(pg, Wg[:, c, b * 1024 + dc * 128:b * 1024 + (dc + 1) * 128], xbf[:, c, :], c == 0, c == 7, ["Wg", rx], [rg])
                            S.add("scalar", lambda e, b=b, pg=pg: e.activation(out=sg[b], in_=pg, func=AF.Sigmoid), reads=[rg], writes=[("sg", b)])
                        S.add("vector", lambda e: e.tensor_tensor(out=t1, in0=psB[0], in1=sg[0], op=ALU.mult), reads=[("psB", 0), ("sg", 0)], writes=["t1"])
                        S.add("vector", lambda e: e.tensor_tensor(out=t2b, in0=psB[1], in1=sg[1], op=ALU.mult), reads=[("psB", 1), ("sg", 1)], writes=["t2b"])
                        S.add("vector", lambda e: e.tensor_tensor(out=t1, in0=t1, in1=t2b, op=ALU.add), reads=["t1", "t2b"], writes=["t1"])
                        S.add("vector", lambda e: e.tensor_tensor(out=t2b, in0=psB[2], in1=sg[2], op=ALU.mult), reads=[("psB", 2), ("sg", 2)], writes=["t2b"])
                        S.add("vector", lambda e, dc=dc, b4=b4: e.tensor_tensor(out=mergedT[:, dc, b4 * 512:(b4 + 1) * 512], in0=t1, in1=t2b, op=ALU.add), reads=["t1", "t2b"], writes=["mergedT"])
                S.flush()

        def layer_norm(z, rz, dst, rdst, g_rep, b_rep, stats, mv, rstd):
            zz = z.rearrange("p (a b) -> p a b", b=512)
            for a in range(2):
                S.add("vector", lambda e, a=a: e.bn_stats(out=stats[:, a, :], in_=zz[:, a, :]), reads=[rz], writes=["stats"])
            S.add("vector", lambda e: e.bn_aggr(out=mv, in_=stats.rearrange("p a b -> p (a b)")), reads=["stats"], writes=["mv"])
            S.add("vector", lambda e: e.tensor_scalar(out=rstd, in0=mv[:, 1:2], scalar1=1e-5, scalar2=None, op0=ALU.add), reads=["mv"], writes=["rstd"])
            S.add("scalar", lambda e: e.activation(out=rstd, in_=rstd, func=AF.Sqrt), reads=["rstd"], writes=["rstd"])
            S.add("vector", lambda e: e.reciprocal(out=rstd, in_=rstd), reads=["rstd"], writes=["rstd"])
            S.add("vector", lambda e: e.tensor_scalar(out=z, in0=z, scalar1=mv[:, 0:1], scalar2=rstd, op0=ALU.subtract, op1=ALU.mult), reads=[rz, "mv", "rstd"], writes=[rz])
            S.add("gpsimd", lambda e: e.tensor_tensor(out=z, in0=z, in1=g_rep, op=ALU.mult), reads=[rz, "lnp"], writes=[rz])
            S.add("vector", lambda e: e.tensor_tensor(out=dst, in0=z, in1=b_rep, op=ALU.add), reads=[rz, "lnp"], writes=[rdst])

        if on("M2"):
            with contextlib.ExitStack() as st2:
                A.seek(OFF_LOC2 + 4 * 1024)
                Wout = A.alloc([8, D], BF16)
                g_rep = A.alloc([D], F32)
                b_rep = A.alloc([D], F32)
                xown = [A.alloc([D], F32) for _ in range(2)]
                zb = [A.alloc([D], F32) for _ in range(2)]
                stats = A.alloc([2, 6], F32)
                mv = A.alloc([2], F32)
                rstd = A.alloc([1], F32)
                psY = [psum(f"psY{i}", [128, 512]) for i in range(4)]
                S.add("sync", lambda e: e.dma_start(out=g_rep, in_=lnrep[0]), writes=["lnp"], dma=True)
                S.add("sync", lambda e: e.dma_start(out=b_rep, in_=lnrep[1]), writes=["lnp"], dma=True)
                for m in range(NOWN):
                    xo_ = xown[m % 2]
                    rxo = ("xown", m % 2)
                    z = zb[m % 2]
                    rz = ("z", m % 2)
                    S.add("sync", lambda e, xo_=xo_, m=m: e.dma_start(out=xo_, in_=x_own[m * 128:(m + 1) * 128, :]), writes=[rxo], dma=True)
                    for eh in range(2):
                        py = psY[(2 * m + eh) % 4]
                        rpy = ("psY", (2 * m + eh) % 4)
                        for dc in range(8):
                            mm(py, mergedT[:, dc, m * 128:(m + 1) * 128], Wout[:, dc, eh * 512:(eh + 1) * 512], dc == 0, dc == 7, ["mergedT", "Wout"], [rpy])
                        S.add("vector", lambda e, z=z, xo_=xo_, py=py, eh=eh: e.scalar_tensor_tensor(
                            out=z[:, eh * 512:(eh + 1) * 512], in0=xo_[:, eh * 512:(eh + 1) * 512], scalar=ALPHA, in1=py, op0=ALU.mult, op1=ALU.add),
                            reads=[rxo, rpy], writes=[rz])
                    layer_norm(z, rz, h1[:, m, :], "h1", g_rep, b_rep, stats, mv, rstd)
                if "h1" in dbg:
                    S.add("sync", lambda e: e.dma_start(out=dbg["h1"].rearrange("(m p) d -> p m d", p=128), in_=h1), reads=["h1"], writes=["dbgh"], dma=True)
                S.flush()

        if on("R"):
            with contextlib.ExitStack() as st2:
                A.seek(OFF_LOC2)
                Wr = A.alloc([8, NE], F32)
                hT = [A.alloc([8, 128], F32) for _ in range(2)]
                hbf = [A.alloc([D], BF16) for _ in range(2)]
                lg2 = [A.alloc([NE], F32) for _ in range(2)]
                mx82 = [A.alloc([8], F32) for _ in range(2)]
                negm2 = [A.alloc([1], F32) for _ in range(2)]
                ex42 = [A.alloc([4], F32) for _ in range(2)]
                esum2 = [A.alloc([1], F32) for _ in range(2)]
                maskall = A.alloc([NOWN, NE], BF16)
                destf2 = [A.alloc([NE], F32) for _ in range(2)]
                selk2 = [A.alloc([NE], F32) for _ in range(2)]
                junk2 = [A.alloc([NE], F32) for _ in range(2)]
                dk2 = [A.alloc([4], F32) for _ in range(2)]
                psTr = [psum(f"psTr{i}", [128, 4, 128]) for i in range(2)]
                psL2 = [psum(f"psL{i}", [128, NE]) for i in range(2)]
                psP2 = [psum(f"psP{i}", [128, NE]) for i in range(2)]
                S.add("sync", lambda e: e.dma_start(out=Wr, in_=w_router.rearrange("(c p) n -> p c n", p=128)), writes=["Wr"], dma=True)
                for m in range(NOWN):
                    p_ = m % 2
                    lg, mx8, negm, ex4, esum = lg2[p_], mx82[p_], negm2[p_], ex42[p_], esum2[p_]
                    destf, selk, junk, dk, psL, psP = destf2[p_], selk2[p_], junk2[p_], dk2[p_], psL2[p_], psP2[p_]
                    rlg, rmx, rng_, rex, res_ = ("lg", p_), ("mx8", p_), ("negm", p_), ("ex4", p_), ("esum", p_)
                    rdf, rsk, rjk, rdk, rpl_, rpp = ("destf", p_), ("selk", p_), ("junk", p_), ("dk", p_), ("psL", p_), ("psP", p_)
                    hTm = hT[m % 2]
                    rh = ("hT", m % 2)
                    for c in range(8):
                        S.add("tensor", lambda e, m=m, c=c: e.transpose(psTr[c // 4][:, c % 4, :], h1[:, m, c * 128:(c + 1) * 128], ident_f), reads=["h1", "cst"], writes=[("psTr", c // 4)])
                        if c % 4 == 3:
                            evac(c // 4, hTm[:, c - 3:c + 1, :], psTr[c // 4], [("psTr", c // 4)], [rh])
                    for c in range(8):
                        mm(psL, hTm[:, c, :], Wr[:, c, :], c == 0, c == 7, [rh, "Wr"], [rpl_])
                    S.add("vector", lambda e, lg=lg, psL=psL: e.tensor_tensor(out=lg, in0=psL, in1=br_rep, op=ALU.add), reads=[rpl_, "cst"], writes=[rlg])
                    S.add("vector", lambda e, lg=lg, mx8=mx8: e.max(out=mx8, in_=lg), reads=[rlg], writes=[rmx])
                    S.add("vector", lambda e, m=m, lg=lg, mx8=mx8: e.tensor_scalar(out=maskall[:, m, :], in0=lg, scalar1=mx8[:, 3:4], scalar2=None, op0=ALU.is_ge), reads=[rlg, rmx], writes=[("maskall", m)])
                    S.add("vector", lambda e, negm=negm, mx8=mx8: e.tensor_scalar(out=negm, in0=mx8[:, 0:1], scalar1=-1.0, scalar2=None, op0=ALU.mult), reads=[rmx], writes=[rng_])
                    S.add("scalar", lambda e, ex4=ex4, mx8=mx8, negm=negm: e.activation(out=ex4, in_=mx8[:, 0:4], func=AF.Exp, bias=negm, scale=1.0), reads=[rmx, rng_], writes=[rex])
                    S.add("vector", lambda e, esum=esum, ex4=ex4: e.tensor_reduce(out=esum, in_=ex4, axis=mybir.AxisListType.X, op=ALU.add), reads=[rex], writes=[res_])
                    S.add("vector", lambda e, esum=esum: e.reciprocal(out=esum, in_=esum), reads=[res_], writes=[res_])
                    S.add("vector", lambda e, m=m, ex4=ex4, esum=esum: e.tensor_scalar(out=gate4[:, m, :], in0=ex4, scalar1=esum, scalar2=None, op0=ALU.mult), reads=[rex, res_], writes=[("gate4", m)])
                    for m2 in range(m):
                        mm(psP, ones_b, maskall[:, m2, :], m2 == 0, False, [("maskall", m2), "ones_b"], [rpp])
                    mm(psP, tri_strict_b, maskall[:, m, :], m == 0, True, [("maskall", m), "tsb"], [rpp])
                    S.add("vector", lambda e, destf=destf, psP=psP: e.scalar_tensor_tensor(out=destf, in0=psP, scalar=float(CAP - 1), in1=iotaC, op0=ALU.min, op1=ALU.add), reads=[rpp, "cst"], writes=[rdf])
                    for k in range(4):
                        S.add("vector", lambda e, k=k, selk=selk, lg=lg, mx8=mx8: e.tensor_scalar(out=selk, in0=lg, scalar1=mx8[:, k:k + 1], scalar2=None, op0=ALU.is_equal), reads=[rlg, rmx], writes=[rsk])
                        S.add("vector", lambda e, k=k, junk=junk, selk=selk, destf=destf: e.tensor_tensor(out=junk, in0=selk, in1=destf, op=ALU.mult), reads=[rsk, rdf], writes=[rjk])
                        S.add("vector", lambda e, k=k, dk=dk, junk=junk: e.tensor_reduce(out=dk[:, k:k + 1], in_=junk, axis=mybir.AxisListType.X, op=ALU.add), reads=[rjk], writes=[rdk])
                    S.add("vector", lambda e, m=m, dk=dk: e.tensor_copy(out=dest_i[:, m, :], in_=dk), reads=[rdk], writes=[("dest_i", m)])
                    hb = hbf[m % 2]
                    rhb = ("hbf", m % 2)
                    S.add("scalar", lambda e, hb=hb, m=m: e.copy(out=hb, in_=h1[:, m, :]), reads=["h1"], writes=[rhb])
                    for k in range(4):
                        S.add("gpsimd", lambda e, hb=hb, m=m, k=k: e.indirect_dma_start(
                            out=X_scr, out_offset=bass.IndirectOffsetOnAxis(ap=dest_i[:, m, k:k + 1], axis=0), in_=hb, in_offset=None),
                            reads=[rhb, ("dest_i", m)], writes=[("X_scr", m, k)], dma=True)
                if "route" in dbg:
                    A.seek(OFF_LOC2 + 48 * 1024)
                    tmp = A.alloc([NOWN, 8], F32)
                    S.add("vector", lambda e: e.tensor_copy(out=tmp[:, :, 0:4], in_=dest_i), reads=[("dest_i", mm_) for mm_ in range(NOWN)], writes=["tmpr"])
                    S.add("vector", lambda e: e.tensor_copy(out=tmp[:, :, 4:8], in_=gate4), reads=[("gate4", mm_) for mm_ in range(NOWN)], writes=["tmpr"])
                    S.add("sync", lambda e: e.dma_start(out=dbg["route"].rearrange("(m p) d -> p m d", p=128), in_=tmp), reads=["tmpr"], writes=["dbgr"], dma=True)
                S.flush()

        if on("E"):
            with contextlib.ExitStack() as st2:
                A.seek(OFF_QT)
                W2h = [A.alloc([8, 512], BF16) for _ in range(3)]
                b1sb = A.alloc([NE * 16], F32)
                b2rep = [A.alloc([D], F32)]
                Ysb = [A.alloc([D], F32) for _ in range(2)]
                Xe = [A.alloc([3, D], BF16)]
                A.seek(OFF_LOC2)
                Xe.append(A.alloc([3, D], BF16))
                NQ = 5
                W1q = [A.alloc([8, 4, 128], BF16) for _ in range(NQ)]
                XT = [A.alloc([8, CAP], BF16) for _ in range(2)]
                actT = A.alloc([8, CAP], BF16)
                glu = [A.alloc([CAP], F32) for _ in range(2)]
                sig = [A.alloc([CAP], F32) for _ in range(4)]
                lin = [A.alloc([CAP], F32) for _ in range(2)]
                psX = psum("psX", [128, 512], BF16)
                psH = [psum(f"psH{i}", [128, CAP]) for i in range(4)]
                psY2 = [psum(f"psY2{i}", [128, 512]) for i in range(2)]
                S.add("sync", lambda e: e.dma_start(out=b1sb, in_=b1T), writes=["b1sb"], dma=True)
                Xv = X_scr.rearrange("(e sb p) d -> e p sb d", sb=3, p=128)
                Yv = Y_scr.rearrange("(e sb p) d -> e sb p d", sb=3, p=128)
                hk = 0
                yk = 0
                pieces = []
                for ex_ in range(NE):
                    pieces += [(ex_, "w1", q_) for q_ in range(4)] + [(ex_, "w2", d_) for d_ in range(2)]
                pbuf = {}
                pstate = {"next": 0, "qi": 0, "w2i": 0}

                def ensure(upto):
                    while pstate["next"] < min(upto, len(pieces)):
                        i = pstate["next"]
                        pstate["next"] += 1
                        ex_, kind, j = pieces[i]
                        if kind == "w1":
                            w1v_ = w_exp_in[ex_].rearrange("(c p) f -> p c f", p=128)
                            f0 = (0, 1024, 512, 1536)[j]
                            wq = W1q[pstate["qi"] % NQ]
                            rwq = ("W1q", pstate["qi"] % NQ)
                            pstate["qi"] += 1
                            S.add("gpsimd", lambda e, wq=wq, w1v_=w1v_, f0=f0: e.dma_start(out=wq.rearrange("p c r f -> p c (r f)"), in_=w1v_[:, :, f0:f0 + 512]), writes=[rwq], dma=True)
                            pbuf[i] = (wq, rwq)
                        else:
                            w2v_ = w_exp_out[ex_].rearrange("(c p) f -> p c f", p=128)
                            w2 = W2h[pstate["w2i"] % 3]
                            rw2 = ("W2h", pstate["w2i"] % 3)
                            pstate["w2i"] += 1
                            S.add("gpsimd", lambda e, w2=w2, w2v_=w2v_, j=j: e.dma_start(out=w2, in_=w2v_[:, :, j * 512:(j + 1) * 512]), writes=[rw2], dma=True)
                            pbuf[i] = (w2, rw2)

                def load_x(ex_):
                    S.add("sync", lambda e, ex_=ex_: e.dma_start(out=Xe[ex_ % 2], in_=Xv[ex_]), reads=["X_scr"], writes=[("Xe", ex_ % 2)], dma=True)

                def transposes(ex_):
                    xe = Xe[ex_ % 2]
                    rxe = ("Xe", ex_ % 2)
                    for c in range(8):
                        for sb in range(3):
                            S.add("tensor", lambda e, xe=xe, c=c, sb=sb: e.transpose(psX[:, sb * 128:(sb + 1) * 128], xe[:, sb, c * 128:(c + 1) * 128], ident_b), reads=[rxe, "ident_b"], writes=["psX"])
                        S.add("vector", lambda e, c=c, ex_=ex_: e.tensor_copy(out=XT[ex_ % 2][:, c, :], in_=psX[:, 0:CAP]), reads=["psX"], writes=[("XT", ex_ % 2)])

                load_x(0)
                ensure(5)
                transposes(0)
                for ex in range(NE):
                    xt = XT[ex % 2]
                    rxt = ("XT", ex % 2)
                    if ex + 1 < NE:
                        load_x(ex + 1)
                    S.add("sync", lambda e, ex=ex: e.dma_start(out=b2rep[0], in_=b_exp_out[ex:ex + 1, :].partition_broadcast(128)), writes=[("b2rep", 0)], dma=True)
                    for half in range(2):
                        pG = ex * 6 + 2 * half
                        ensure(pG + 5)
                        wq, rwq = pbuf.pop(pG)
                        for r in range(4):
                            fb = 4 * half + r
                            pg = psH[hk % 4]; rpg = ("psH", hk % 4); hk += 1
                            for c in range(8):
                                mm(pg, wq[:, c, r, :], xt[:, c, :], c == 0, c == 7, [rwq, rxt], [rpg])
                            g_ = glu[r % 2]; rg_ = ("glu", r % 2)
                            s_ = sig[r]; rs_ = ("sig", r)
                            cg = ex * 16 + fb
                            S.add("vector", lambda e, g_=g_, pg=pg, cg=cg: e.tensor_scalar(out=g_, in0=pg, scalar1=b1sb[:, cg:cg + 1], scalar2=7.0, op0=ALU.add, op1=ALU.min), reads=[rpg, "b1sb"], writes=[rg_])
                            S.add("scalar", lambda e, g_=g_, s_=s_: e.activation(out=s_, in_=g_, func=AF.Silu, scale=1.702), reads=[rg_], writes=[rs_])
                        ensure(pG + 6)
                        wl, rwl = pbuf.pop(pG + 1)
                        for r in range(4):
                            fb = 4 * half + r
                            pl = psH[hk % 4]; rpl = ("psH", hk % 4); hk += 1
                            for c in range(8):
                                mm(pl, wl[:, c, r, :], xt[:, c, :], c == 0, c == 7, [rwl, rxt], [rpl])
                            l_ = lin[r % 2]; rl_ = ("lin", r % 2)
                            s_ = sig[r]; rs_ = ("sig", r)
                            cl = ex * 16 + 8 + fb
                            S.add("vector", lambda e, l_=l_, pl=pl, cl=cl: e.tensor_scalar(out=l_, in0=pl, scalar1=b1sb[:, cl:cl + 1], scalar2=7.0, op0=ALU.add, op1=ALU.min), reads=[rpl, "b1sb"], writes=[rl_])
                            S.add("vector", lambda e, l_=l_: e.tensor_scalar(out=l_, in0=l_, scalar1=-7.0, scalar2=1.0, op0=ALU.max, op1=ALU.add), reads=[rl_], writes=[rl_])
                            S.add("vector", lambda e, fb=fb, s_=s_, l_=l_: e.scalar_tensor_tensor(out=actT[:, fb, :], in0=s_, scalar=1.0 / 1.702, in1=l_, op0=ALU.mult, op1=ALU.mult), reads=[rs_, rl_], writes=["actT"])
                    if ex + 1 < NE:
                        transposes(ex + 1)
                    ensure(ex * 6 + 4 + 5)
                    w2s = [pbuf.pop(ex * 6 + 4), pbuf.pop(ex * 6 + 5)]
                    for sb in range(3):
                        ys = Ysb[yk % 2]
                        rys = ("Ysb", yk % 2)
                        yk += 1
                        for dh in range(2):
                            w2, rw2 = w2s[dh]
                            py = psY2[dh]
                            rpy = ("psY2", dh)
                            for fc in range(8):
                                mm(py, actT[:, fc, sb * 128:(sb + 1) * 128], w2[:, fc, :], fc == 0, fc == 7, ["actT", rw2], [rpy])
                            S.add("vector", lambda e, ys=ys, py=py, dh=dh: e.tensor_tensor(out=ys[:, dh * 512:(dh + 1) * 512], in0=py, in1=b2rep[0][:, dh * 512:(dh + 1) * 512], op=ALU.add),
                                  reads=[rpy, ("b2rep", 0)], writes=[rys])
                        S.add("sync", lambda e, ys=ys, ex=ex, sb=sb: e.dma_start(out=Yv[ex, sb], in_=ys), reads=[rys], writes=[("Y_scr", ex, sb)], dma=True)
                S.flush()

        if on("F"):
            with contextlib.ExitStack() as st2:
                A.seek(OFF_LOC2)
                g_rep = A.alloc([D], F32)
                b_rep = A.alloc([D], F32)
                Yk = [A.alloc([D], F32) for _ in range(8)]
                zb = [A.alloc([D], F32) for _ in range(2)]
                ob = [A.alloc([D], F32) for _ in range(2)]
                stats = A.alloc([2, 6], F32)
                mv = A.alloc([2], F32)
                rstd = A.alloc([1], F32)
                S.add("sync", lambda e: e.dma_start(out=g_rep, in_=lnrep[2]), writes=["lnp"], dma=True)
                S.add("sync", lambda e: e.dma_start(out=b_rep, in_=lnrep[3]), writes=["lnp"], dma=True)
                for m in range(NOWN):
                    z = zb[m % 2]
                    rz = ("z", m % 2)
                    S.add("vector", lambda e, z=z, m=m: e.tensor_scalar(out=z, in0=h1[:, m, :], scalar1=ALPHA, scalar2=None, op0=ALU.mult), reads=["h1"], writes=[rz])
                    for k in range(4):
                        yk_ = Yk[(4 * m + k) % 8]
                        ryk = ("Yk", (4 * m + k) % 8)
                        S.add("gpsimd", lambda e, yk_=yk_, m=m, k=k: e.indirect_dma_start(
                            out=yk_, out_offset=None, in_=Y_scr, in_offset=bass.IndirectOffsetOnAxis(ap=dest_i[:, m, k:k + 1], axis=0)),
                            reads=["Y_scr", "dest_i"], writes=[ryk], dma=True)
                        S.add("vector", lambda e, z=z, yk_=yk_, m=m, k=k: e.scalar_tensor_tensor(out=z, in0=yk_, scalar=gate4[:, m, k:k + 1], in1=z, op0=ALU.mult, op1=ALU.add), reads=[ryk, "gate4", rz], writes=[rz])
                    o_ = ob[m % 2]
                    ro = ("ob", m % 2)
                    layer_norm(z, rz, o_, ro, g_rep, b_rep, stats, mv, rstd)
                    S.add("sync", lambda e, o_=o_, m=m: e.dma_start(out=out_d[m * 128:(m + 1) * 128, :], in_=o_), reads=[ro], writes=["out"], dma=True)
                S.flush()
    return nc


def _t5_bucket(dist):
    dist = np.asarray(dist, dtype=np.int64)
    nf = np.maximum(dist, 16).astype(np.float32)
    large = 16 + (np.log(nf / np.float32(16)) / np.float32(math.log(2048 / 16)) * np.float32(16)).astype(np.int32)
    large = np.minimum(large, 31)
    return np.where(dist < 16, dist, large).astype(np.int64)


def _host_consts(core, b_fgate, b_router):
    n = 6 * 128 + 32 + NTS + 24 + 32
    c = np.zeros((128, n), np.float32)
    idx = np.arange(128)
    c[:, 0:128] = np.eye(128, dtype=np.float32)
    c[:, 128:256] = (idx[:, None] <= idx[None, :])
    c[:, 256:384] = (idx[:, None] < idx[None, :])
    c[:, 384:512] = 1.0
    c[64, 512:640] = 1.0
    c[:, 640:768] = np.where(idx[:, None] <= idx[None, :], 0.0, NEG)
    c[:, 768:800] = (np.arange(NE) * CAP)[None, :]
    s = np.arange(NTS)
    a = s - 7 + core
    c[:, 800:800 + NTS] = np.where((a >= 0) & (a < 128), 0.0, NEG)[None, :]
    c[:, 800 + NTS:800 + NTS + 24] = np.tile(np.asarray(b_fgate, np.float32).reshape(1, 6), (128, 4))
    c[:, 800 + NTS + 24:800 + NTS + 56] = np.asarray(b_router, np.float32).reshape(1, NE)
    return c


def _dil_bias(t5_bias):
    out = np.full((6, 128, 17, 128), NEG, np.float32)
    k = np.arange(128)[:, None]
    q = np.arange(128)[None, :]
    for hd in range(6):
        window, dil = DIL_CFG[hd // 2]
        for dl in range(DMAX[hd // 2] + 1):
            dist = 128 * dl + q - k
            valid = (dist >= 0) & (dist % dil == 0) & (dist <= window)
            b = _t5_bucket(np.clip(dist, 0, None))
            vals = t5_bias[b, hd]
            out[hd, :, dl, :] = np.where(valid, vals, np.float32(NEG))
    return out


_CACHE = {}


def prepare_inputs(inputs):
    x = np.asarray(inputs["x"], np.float32)[0]
    T = NTS * 128
    shared = {
        "memT": np.ascontiguousarray(np.asarray(inputs["mem"], np.float32)[0].T),
        "w_in": np.ascontiguousarray(np.asarray(inputs["w_in"], np.float32)[0]),
        "w_mem_kv": np.ascontiguousarray(np.asarray(inputs["w_mem_kv"], np.float32)[0]),
        "w_br": np.ascontiguousarray(np.concatenate([np.asarray(inputs["w_br_fox"], np.float32)[0], np.asarray(inputs["w_br_dil"], np.float32)[0],
                                                     np.asarray(inputs["w_br_mem"], np.float32)[0]], axis=0)),
        "w_out": np.ascontiguousarray(np.asarray(inputs["w_out"], np.float32)[0]),
        "w_router": np.ascontiguousarray(np.asarray(inputs["w_router"], np.float32)[0]),
        "w_exp_in": np.ascontiguousarray(np.asarray(inputs["w_exp_in"], np.float32)[0]),
        "w_exp_out": np.ascontiguousarray(np.asarray(inputs["w_exp_out"], np.float32)[0]),
        "b_exp_out": np.ascontiguousarray(np.asarray(inputs["b_exp_out"], np.float32)[0]),
        "b1T": np.ascontiguousarray(np.asarray(inputs["b_exp_in"], np.float32)[0].reshape(NE, 16, 128).transpose(2, 0, 1).reshape(128, NE * 16)),
        "lnrep": np.ascontiguousarray(np.stack([np.broadcast_to(np.asarray(inputs[k], np.float32)[0][None, :], (128, D)) for k in ("ln1_g", "ln1_b", "ln2_g", "ln2_b")])),
        "dilb": _dil_bias(np.asarray(inputs["t5_bias"], np.float32)),
    }
    xt = x.reshape(NOWN, NCORES, 128, D)
    in_maps = []
    for i in range(NCORES):
        xs = np.zeros((T, D), np.float32)
        xs[(7 - i) * 128:(7 - i) * 128 + S_LEN] = x
        mp = dict(shared)
        mp["xT"] = np.ascontiguousarray(xs.T)
        mp["x_own"] = np.ascontiguousarray(xt[:, i].reshape(NOWN * 128, D))
        mp["cst"] = _host_consts(i, inputs["b_fgate"], inputs["b_router"])
        a_tile = np.arange(NTS) - 7 + i
        mp["padrow"] = np.repeat(np.where((a_tile >= 0) & (a_tile < 128), 0.0, NEG).astype(np.float32), 128)[None, :]
        in_maps.append(mp)
    return in_maps


def assemble(results, key="out", width=D):
    out = np.zeros((NOWN, NCORES, 128, width), np.float32)
    for i in range(NCORES):
        out[:, i] = np.asarray(results[i][key], np.float32).reshape(NOWN, 128, width)
    return out.reshape(1, S_LEN, width)


def kernel(**inputs):
    if "nc" not in _CACHE:
        _CACHE["nc"] = build_program()
    in_maps = prepare_inputs(inputs)
    res = run_bass_kernel_spmd(_CACHE["nc"], in_maps, core_ids=list(range(NCORES)))
    return assemble(res.results)
```

```python
import contextlib
import math
import numpy as np
import concourse.bass as bass
import concourse.mybir as mybir
from concourse.bass_utils import run_bass_kernel_spmd

F32 = mybir.dt.float32
BF16 = mybir.dt.bfloat16
I32 = mybir.dt.int32
AF = mybir.ActivationFunctionType
ALU = mybir.AluOpType

NCORES = 8
D = 1024
S_LEN = 16384
NTS = 136
NOWN = 16
NE = 32
CAP = 384
ALPHA = 2.0 ** 0.25
NEG = -30000.0
DIL_CFG = ((128, 1), (512, 4), (2048, 16))
DMAX = (1, 4, 16)
ENGINES = ("tensor", "vector", "scalar", "gpsimd", "sync")
PHASES = ("A", "Q", "B", "C", "Dm", "M1", "M2", "R", "E", "F")


class Sched:
    NDS = 48

    def __init__(self, nc, stack):
        self.nc = nc
        self.esem = {e: stack.enter_context(nc.semaphore(f"s_{e}")) for e in ENGINES}
        self.dsem = [stack.enter_context(nc.semaphore(f"d_{i}")) for i in range(self.NDS)]
        self.ecount = {e: 0 for e in ENGINES}
        self.dcount = [0] * self.NDS
        self.dnext = {"sync": 0, "gpsimd": 0}
        self.reset()

    def reset(self):
        self.ops = []
        self.last_writer = {}
        self.readers = {}

    def add(self, engine, fn, reads=(), writes=(), dma=False):
        deps = set()
        for r in reads:
            if r in self.last_writer:
                deps.add(self.last_writer[r])
        for w in writes:
            if w in self.last_writer:
                deps.add(self.last_writer[w])
            for rd in self.readers.get(w, ()):
                deps.add(rd)
        oid = len(self.ops)
        self.ops.append((engine, fn, deps, dma))
        for r in reads:
            self.readers.setdefault(r, []).append(oid)
        for w in writes:
            self.last_writer[w] = oid
            self.readers[w] = []
        return oid

    def flush(self):
        nc = self.nc
        ops = self.ops
        n = len(ops)
        if n == 0:
            return
        needed = [False] * n
        for (_, _, deps, _) in ops:
            for d in deps:
                needed[d] = True
        last_on = {}
        for i, (e, _, _, dma) in enumerate(ops):
            if not dma:
                last_on[e] = i
        for i in last_on.values():
            needed[i] = True
        comp = [None] * n
        issue_wait = {}
        prev_on_sem = {}
        for i, (eng, fn, deps, dma) in enumerate(ops):
            if dma:
                half = self.NDS // 2
                s = self.dnext[eng] + (half if eng == "gpsimd" else 0)
                self.dnext[eng] = (self.dnext[eng] + 1) % half
                if self.dcount[s] > 0:
                    issue_wait[i] = (s, self.dcount[s])
                self.dcount[s] += 16
                comp[i] = ("d", s, self.dcount[s])
            elif needed[i]:
                self.ecount[eng] += 1
                comp[i] = ("e", eng, self.ecount[eng])
        final_e = dict(self.ecount)
        final_d = list(self.dcount)
        esem, dsem = self.esem, self.dsem

        def run_engine(ename):
            def body(eng):
                waited = {}

                def do_wait(key, semh, val):
                    if waited.get(key, -1) >= val:
                        return
                    eng.wait_ge(semh, val)
                    waited[key] = val

                for i, (e, fn, deps, dma) in enumerate(ops):
                    if e != ename:
                        continue
                    for d in sorted(deps):
                        c = comp[d]
                        if c is None:
                            continue
                        if c[0] == "e":
                            if c[1] == "tensor" and ename == "tensor":
                                continue
                            do_wait(("e", c[1]), esem[c[1]], c[2])
                        else:
                            do_wait(("d", c[1]), dsem[c[1]], c[2])
                    if i in issue_wait:
                        s, v = issue_wait[i]
                        do_wait(("d", s), dsem[s], v)
                    ins = fn(eng)
                    c = comp[i]
                    if c is not None:
                        if c[0] == "e":
                            ins.then_inc(esem[c[1]], 1)
                        else:
                            ins.then_inc(dsem[c[1]], 16)
                for e2 in ENGINES:
                    if final_e[e2] > 0:
                        do_wait(("e", e2), esem[e2], final_e[e2])
                for s in range(self.NDS):
                    if final_d[s] > 0:
                        do_wait(("d", s), dsem[s], final_d[s])
            return body

        with nc.Block() as block:
            block.tensor(run_engine("tensor"))
            block.vector(run_engine("vector"))
            block.scalar(run_engine("scalar"))
            block.gpsimd(run_engine("gpsimd"))
            block.sync(run_engine("sync"))
        self.reset()


class Arena:
    def __init__(self, nc, stack, nbytes):
        self.t = stack.enter_context(nc.sbuf_tensor("arena", [128, nbytes // 4], F32))
        self.nbytes = nbytes
        self.off = 0

    def seek(self, off):
        self.off = off

    def alloc(self, free_shape, dtype):
        n = int(np.prod(free_shape))
        esz = 4 if dtype in (F32, I32) else 2
        nb = (n * esz + 31) // 32 * 32
        assert self.off + nb <= self.nbytes, (self.off, nb, self.nbytes)
        ap = self.t[:, self.off // 4:(self.off + nb) // 4]
        self.off += nb
        if dtype != F32:
            ap = ap.bitcast(dtype)
        ap = ap[:, 0:n]
        if len(free_shape) == 2:
            ap = ap.rearrange("p (a b) -> p a b", b=free_shape[1])
        elif len(free_shape) == 3:
            ap = ap.rearrange("p (a b c) -> p a b c", b=free_shape[1], c=free_shape[2])
        elif len(free_shape) == 4:
            ap = ap.rearrange("p (a b c d) -> p a b c d", b=free_shape[1], c=free_shape[2], d=free_shape[3])
        return ap


def build_program(last_phase="F", debug=()):
    nc = bass.Bass("TRN2", target_bir_lowering=False)
    T = NTS * 128

    def din(name, shape, dt=F32):
        return nc.dram_tensor(name, list(shape), dt, kind="ExternalInput").ap()

    xT = din("xT", [D, T])
    x_own = din("x_own", [NOWN * 128, D])
    memT = din("memT", [D, 256])
    w_in = din("w_in", [D, 5638])
    w_mem_kv = din("w_mem_kv", [D, 512])
    w_br = din("w_br", [D, D])
    w_out = din("w_out", [D, D])
    w_router = din("w_router", [D, NE])
    if PHASES.index(last_phase) >= PHASES.index("E"):
        w_exp_in = din("w_exp_in", [NE, D, 2 * D])
        w_exp_out = din("w_exp_out", [NE, D, D])
    b_exp_out = din("b_exp_out", [NE, D])
    b1T = din("b1T", [128, NE * 16])
    lnrep = din("lnrep", [4, 128, D])
    cst = din("cst", [128, 6 * 128 + 32 + NTS + 24 + 32])
    dilb = din("dilb", [6, 128, 17, 128])
    padrow = din("padrow", [1, NTS * 128])
    out_d = nc.dram_tensor("out", [NOWN * 128, D], F32, kind="ExternalOutput").ap()
    dbg = {}
    for name, shape in debug:
        dbg[name] = nc.dram_tensor(name, list(shape), F32, kind="ExternalOutput").ap()

    KT_scr = nc.dram_tensor("KT_scr", [6, 128, T], BF16, kind="Internal").ap()
    V_scr = nc.dram_tensor("V_scr", [12, 128, NTS, 65], BF16, kind="Internal").ap()
    X_scr = nc.dram_tensor("X_scr", [NE * CAP, D], BF16, kind="Internal").ap()
    Y_scr = nc.dram_tensor("Y_scr", [NE * CAP, D], F32, kind="Internal").ap()
    Gd = nc.dram_tensor("Gd", [6, 3, NTS * 128], BF16, kind="Internal").ap()

    xTv = xT.rearrange("(c p) t -> p c t", p=128)
    xTown = xT.rearrange("(c p) (m j t) -> p c m j t", p=128, j=8, t=128)
    w_inv = w_in.rearrange("(c p) n -> p c n", p=128)

    li = PHASES.index(last_phase)

    def on(ph):
        return PHASES.index(ph) <= li

    with contextlib.ExitStack() as st:
        S = Sched(nc, st)
        A = Arena(nc, st, 206 * 1024)

        def psum(name, shape, dt=F32):
            return st2.enter_context(nc.psum_tensor(name, list(shape), dt))[:]

        A.seek(0)
        cst_sb = A.alloc([6 * 128 + 32 + NTS + 24 + 32], F32)
        ident_f = cst_sb[:, 0:128]
        tri_incl = cst_sb[:, 128:256]
        tri_strict_f = cst_sb[:, 256:384]
        ones_f = cst_sb[:, 384:512]
        e64 = cst_sb[:, 512:640]
        foxtri = cst_sb[:, 640:768]
        iotaC = cst_sb[:, 768:800]
        padb = cst_sb[:, 800:800 + NTS]
        bf_rep4 = cst_sb[:, 800 + NTS:800 + NTS + 24].rearrange("p (a b) -> p a b", b=6)
        br_rep = cst_sb[:, 800 + NTS + 24:800 + NTS + 56]
        ident_b = A.alloc([128], BF16)
        tri_strict_b = A.alloc([128], BF16)
        ones_b = A.alloc([128], BF16)
        foxtri_b = A.alloc([128], BF16)
        G_all = A.alloc([NTS, 6], F32)
        Gref = A.alloc([NOWN, 6], F32)
        dest_i = A.alloc([NOWN, 4], I32)
        gate4 = A.alloc([NOWN, 4], F32)
        assert A.off <= 12 * 1024, A.off
        OFF_QT = 12 * 1024
        OFF_O = OFF_QT + 44 * 1024
        OFF_LOC = OFF_O + 32 * 1024
        OFF_LOC2 = OFF_O + 64 * 1024
        A.seek(OFF_QT)
        QTf = A.alloc([6, NOWN * 128], BF16)
        QT = A.alloc([5, NOWN * 128], BF16)
        A.seek(OFF_QT)
        mergedT = A.alloc([8, NOWN * 128], BF16)
        A.seek(OFF_O)
        o_fox = A.alloc([NOWN, 384], BF16)
        o_dil = A.alloc([NOWN, 384], BF16)
        o_mem = A.alloc([NOWN, 256], BF16)
        A.seek(OFF_O)
        h1 = A.alloc([NOWN, D], F32)

        S.add("sync", lambda e: e.dma_start(out=cst_sb, in_=cst), writes=["cst"], dma=True)
        S.add("vector", lambda e: e.tensor_copy(out=ident_b, in_=ident_f), reads=["cst"], writes=["ident_b"])
        S.add("vector", lambda e: e.tensor_copy(out=tri_strict_b, in_=tri_strict_f), reads=["cst"], writes=["tsb"])
        S.add("vector", lambda e: e.tensor_copy(out=ones_b, in_=ones_f), reads=["cst"], writes=["ones_b"])
        S.add("vector", lambda e: e.tensor_copy(out=foxtri_b, in_=foxtri), reads=["cst"], writes=["foxtri_b"])
        S.flush()

        def evac(i, out, in_, reads, writes, scale=None):
            if i % 2 == 0:
                if scale is None:
                    S.add("scalar", lambda e: e.copy(out=out, in_=in_), reads, writes)
                else:
                    S.add("scalar", lambda e: e.mul(out=out, in_=in_, mul=scale), reads, writes)
            else:
                if scale is None:
                    S.add("vector", lambda e: e.tensor_copy(out=out, in_=in_), reads, writes)
                else:
                    S.add("vector", lambda e: e.tensor_scalar(out=out, in0=in_, scalar1=scale, scalar2=None, op0=ALU.mult), reads, writes)

        def mm(out, lhsT, rhs, start, stop, reads, writes):
            S.add("tensor", lambda e: e.matmul(out, lhsT, rhs, start=start, stop=stop), reads, writes)

        if on("A"):
            with contextlib.ExitStack() as st2:
                A.seek(OFF_LOC)
                Wk = A.alloc([8, 768], BF16)
                Wv = A.alloc([8, 768], BF16)
                Wf = A.alloc([8, 6], BF16)
                xbuf = [A.alloc([8, 512], BF16) for _ in range(2)]
                ktst = [A.alloc([6, 512], BF16) for _ in range(2)]
                vst = [A.alloc([12, 4, 65], BF16) for _ in range(2)]
                Z_all = A.alloc([NTS, 6], F32)
                SP_all = A.alloc([NTS, 6], F32)
                Wsb = A.alloc([NTS, 6], F32)
                Tsb = A.alloc([NTS, 6], F32)
                HA = A.alloc([NTS, 6], F32)
                HB = A.alloc([NTS, 6], F32)
                zero_t = A.alloc([8, D], BF16)
                Gp = [A.alloc([NTS, 6], BF16) for _ in range(3)]
                R1 = A.alloc([NTS, 6], F32)
                R2 = A.alloc([NTS, 6], F32)
                gst = [A.alloc([128], BF16) for _ in range(2)]
                psGT = psum("psGT", [128, 128], BF16)
                psK = [psum(f"psK{i}", [128, 512]) for i in range(2)]
                psV = [psum(f"psV{i}", [128, 384]) for i in range(2)]
                psF = psum("psF", [128, 4, 6])
                psW = [psum(f"psW{i}", [128, 408]) for i in range(2)]

                S.add("vector", lambda e: e.memset(zero_t, 0.0), writes=["zero_t"])
                Xz = X_scr.rearrange("(a b p) d -> a p b d", b=8, p=128)
                for a in range(NE * CAP // 1024):
                    S.add("sync", lambda e, a=a: e.dma_start(out=Xz[a], in_=zero_t), reads=["zero_t"], writes=[("X_scr", a)], dma=True)

                for (dst, lo) in ((Wk[:, :, 0:384], 384), (Wk[:, :, 384:768], 1542), (Wv[:, :, 0:384], 768), (Wv[:, :, 384:768], 1926)):
                    S.add("gpsimd", lambda e, dst=dst, lo=lo: e.dma_start(out=dst, in_=w_inv[:, :, lo:lo + 384]), writes=["WA"], dma=True)
                S.add("gpsimd", lambda e: e.dma_start(out=Wf, in_=w_inv[:, :, 1152:1158]), writes=["WA"], dma=True)
                for b in range(2):
                    S.add("vector", lambda e, b=b: e.memset(vst[b][:, :, :, 64:65], 1.0), writes=[("vst1", b)])

                KTv = KT_scr.rearrange("r p t -> p r t")
                Vv = V_scr.rearrange("h p t c -> p h t c")
                ev = 0
                for tb in range(NTS // 4):
                    xb = xbuf[tb % 2]
                    rx = ("xb", tb % 2)
                    S.add("gpsimd", lambda e, xb=xb, tb=tb: e.dma_start(out=xb, in_=xTv[:, :, tb * 512:(tb + 1) * 512]), writes=[rx], dma=True)
                    kt = ktst[tb % 2]
                    rk = ("ktst", tb % 2)
                    for pr in range(6):
                        ps = psK[pr % 2]
                        rp = ("psK", pr % 2)
                        for c in range(8):
                            mm(ps, Wk[:, c, pr * 128:(pr + 1) * 128], xb[:, c, :], c == 0, c == 7, [rx, "WA"], [rp])
                        evac(ev, kt[:, pr, :], ps, [rp], [rk]); ev += 1
                    S.add("sync", lambda e, kt=kt, tb=tb: e.dma_start(out=KTv[:, :, tb * 512:(tb + 1) * 512], in_=kt), reads=[rk], writes=[("KT_scr", tb)], dma=True)
                    vs = vst[tb % 2]
                    rv = ("vst", tb % 2)
                    for t4 in range(4):
                        for grp in range(2):
                            ps = psV[grp]
                            rp = ("psV", grp)
                            for c in range(8):
                                mm(ps, xb[:, c, t4 * 128:(t4 + 1) * 128], Wv[:, c, grp * 384:(grp + 1) * 384], c == 0, c == 7, [rx, "WA"], [rp])
                            evac(ev, vs[:, grp * 6:(grp + 1) * 6, t4, 0:64], ps.rearrange("p (h d) -> p h d", d=64), [rp], [rv, ("vst1", tb % 2)]); ev += 1
                    S.add("sync", lambda e, vs=vs, tb=tb: e.dma_start(out=Vv[:, :, tb * 4:(tb + 1) * 4, :], in_=vs), reads=[rv, ("vst1", tb % 2)], writes=[("V_scr", tb)], dma=True)
                    for t4 in range(4):
                        for c in range(8):
                            mm(psF[:, t4, :], xb[:, c, t4 * 128:(t4 + 1) * 128], Wf[:, c, :], c == 0, c == 7, [rx, "WA"], ["psF"])
                    S.add("vector", lambda e, tb=tb: e.tensor_tensor(out=Z_all[:, tb * 4:(tb + 1) * 4, :], in0=psF, in1=bf_rep4, op=ALU.add), reads=["psF", "cst"], writes=["Z_all"])

                Zf = Z_all.rearrange("p a b -> p (a b)")
                SPf = SP_all.rearrange("p a b -> p (a b)")
                S.add("scalar", lambda e: e.activation(out=SPf, in_=Zf, func=AF.Exp, scale=-1.0), reads=["Z_all"], writes=["SP"])
                S.add("scalar", lambda e: e.activation(out=SPf, in_=SPf, func=AF.Ln, bias=1.0, scale=1.0), reads=["SP"], writes=["SP"])
                Wf2 = Wsb.rearrange("p a b -> p (a b)")
                Tf2 = Tsb.rearrange("p a b -> p (a b)")
                for hf in range(2):
                    mm(psW[hf], tri_incl, SPf[:, hf * 408:(hf + 1) * 408], True, True, ["SP", "cst"], [("psW", hf)])
                    S.add("vector", lambda e, hf=hf: e.tensor_copy(out=Wf2[:, hf * 408:(hf + 1) * 408], in_=psW[hf]), reads=[("psW", hf)], writes=["Wsb"])
                for hf in range(2):
                    mm(psW[hf], ones_f, SPf[:, hf * 408:(hf + 1) * 408], True, True, ["SP", "cst"], [("psW", hf)])
                    S.add("vector", lambda e, hf=hf: e.tensor_copy(out=Tf2[:, hf * 408:(hf + 1) * 408], in_=psW[hf]), reads=[("psW", hf)], writes=["Tsb"])
                S.add("vector", lambda e: e.tensor_copy(out=HA, in_=Tsb), reads=["Tsb"], writes=["HA"])
                cur, nxt, rc, rn = HA, HB, "HA", "HB"
                sh = 1
                while sh < NTS:
                    S.add("vector", lambda e, cur=cur, nxt=nxt, sh=sh: e.tensor_copy(out=nxt[:, 0:sh, :], in_=cur[:, 0:sh, :]), reads=[rc], writes=[rn])
                    S.add("vector", lambda e, cur=cur, nxt=nxt, sh=sh: e.tensor_tensor(out=nxt[:, sh:NTS, :], in0=cur[:, sh:NTS, :], in1=cur[:, 0:NTS - sh, :], op=ALU.add), reads=[rc], writes=[rn])
                    cur, nxt, rc, rn = nxt, cur, rn, rc
                    sh *= 2
                S.add("vector", lambda e, cur=cur: e.tensor_tensor(out=G_all, in0=cur, in1=Tsb, op=ALU.subtract), reads=[rc, "Tsb"], writes=["G_all"])
                S.add("vector", lambda e: e.tensor_tensor(out=G_all, in0=G_all, in1=Wsb, op=ALU.add), reads=["G_all", "Wsb"], writes=["G_all"])
                Gown = G_all.rearrange("p (m j) h -> p m j h", j=8)[:, 0:NOWN, 7, :]
                S.add("vector", lambda e: e.tensor_copy(out=HA[:, 0:NOWN, :], in_=Gown), reads=["G_all"], writes=["HA", "HB"])
                mm(psW[0][:, 0:NOWN * 6], e64, HA[:, 0:NOWN, :].rearrange("p a b -> p (a b)"), True, True, ["HA", "cst"], [("psW", 0)])
                S.add("vector", lambda e: e.tensor_copy(out=Gref.rearrange("p a b -> p (a b)"), in_=psW[0][:, 0:NOWN * 6]), reads=[("psW", 0)], writes=["Gref"])
                S.add("vector", lambda e: e.tensor_copy(out=Gp[0], in_=G_all), reads=["G_all"], writes=["Gp0"])
                S.add("vector", lambda e: e.tensor_tensor(out=R1, in0=G_all, in1=Gp[0], op=ALU.subtract), reads=["G_all", "Gp0"], writes=["R1"])
                S.add("vector", lambda e: e.tensor_copy(out=Gp[1], in_=R1), reads=["R1"], writes=["Gp1"])
                S.add("vector", lambda e: e.tensor_tensor(out=R2, in0=R1, in1=Gp[1], op=ALU.subtract), reads=["R1", "Gp1"], writes=["R2"])
                S.add("vector", lambda e: e.tensor_copy(out=Gp[2], in_=R2), reads=["R2"], writes=["Gp2"])
                Gdv = Gd.rearrange("h r (s p) -> h r s p", p=128)
                kk = 0
                for h in range(6):
                    for part in range(3):
                        for (s0, ns) in ((0, 128), (128, NTS - 128)):
                            stg = gst[kk % 2]
                            rst = ("gst", kk % 2)
                            S.add("tensor", lambda e, h=h, part=part, s0=s0, ns=ns: e.transpose(psGT[0:ns, :], Gp[part][:, s0:s0 + ns, h], ident_b), reads=[f"Gp{part}", "ident_b"], writes=["psGT"])
                            evac(kk, stg[0:ns, :], psGT[0:ns, :], ["psGT"], [rst])
                            S.add("sync", lambda e, h=h, part=part, s0=s0, ns=ns, stg=stg: e.dma_start(out=Gdv[h, part, s0:s0 + ns, :], in_=stg[0:ns, :]), reads=[rst], writes=[("Gd", h, part, s0)], dma=True)
                            kk += 1
                if "G_all" in dbg:
                    S.add("sync", lambda e: e.dma_start(out=dbg["G_all"], in_=G_all.rearrange("p a b -> p (a b)")), reads=["G_all"], writes=["dbgG"], dma=True)
                S.flush()

        if on("Q"):
            with contextlib.ExitStack() as st2:
                A.seek(OFF_LOC)
                Wq = A.alloc([8, 1024], BF16)
                xo = A.alloc([8, NOWN, 128], BF16)
                psQ = [psum(f"psQ{i}", [128, 512]) for i in range(2)]
                for (lo_dst, lo, n) in ((0, 0, 384), (384, 1158, 384), (768, 2310, 256)):
                    S.add("gpsimd", lambda e, lo_dst=lo_dst, lo=lo, n=n: e.dma_start(out=Wq[:, :, lo_dst:lo_dst + n], in_=w_inv[:, :, lo:lo + n]), writes=["Wq"], dma=True)
                for c in range(8):
                    S.add("gpsimd", lambda e, c=c: e.dma_start(out=xo[:, c, :, :], in_=xTown[:, c, 0:NOWN, 7, :]), writes=["xo"], dma=True)
                xof = xo.rearrange("p c m t -> p c (m t)")
                k = 0
                for b4 in range(4):
                    for h in range(6):
                        ps = psQ[k % 2]
                        for c in range(8):
                            mm(ps[0:64, :], Wq[:, c, h * 64:(h + 1) * 64], xof[:, c, b4 * 512:(b4 + 1) * 512], c == 0, c == 7, ["Wq", "xo"], [("psQ", k % 2)])
                        evac(k, QTf[0:64, h, b4 * 512:(b4 + 1) * 512], ps[0:64, :], [("psQ", k % 2)], ["QTf"], scale=0.125)
                        k += 1
                    for pr in range(5):
                        ps = psQ[k % 2]
                        for c in range(8):
                            mm(ps, Wq[:, c, 384 + pr * 128:384 + (pr + 1) * 128], xof[:, c, b4 * 512:(b4 + 1) * 512], c == 0, c == 7, ["Wq", "xo"], [("psQ", k % 2)])
                        evac(k, QT[:, pr, b4 * 512:(b4 + 1) * 512], ps, [("psQ", k % 2)], ["QT"], scale=0.125)
                        k += 1
                S.add("vector", lambda e: e.memset(QTf[64:69, :, :], 1.0), writes=["QTfa"])
                for h in range(6):
                    for m in range(NOWN):
                        S.add("vector", lambda e, h=h, m=m: e.tensor_scalar(out=QTf[64:65, h, m * 128:(m + 1) * 128], in0=ones_f[64:65, :], scalar1=Gref[64:65, m, h:h + 1], scalar2=-1.0,
                                                                    op0=ALU.mult, op1=ALU.mult), reads=["Gref", "cst", "QTfa"], writes=["QTfa"])
                S.flush()

        def load_kv(KTb, Vb, pr, hp, vh, par, ntiles):
            rk, rv = ("KTb", par), ("Vb", par)
            q4 = (ntiles * 128) // 4
            for q in range(4):
                S.add("sync", lambda e, q=q: e.dma_start(out=KTb[hp:hp + 64, q * q4:(q + 1) * q4], in_=KT_scr[pr, hp:hp + 64, q * q4:(q + 1) * q4]), writes=[rk], dma=True)
            S.add("sync", lambda e: e.dma_start(out=Vb[:, 0:ntiles, :], in_=V_scr[vh, :, 0:ntiles, :]), writes=[rv], dma=True)
            return rk, rv

        class AttnPipe:
            LA = 2

            def __init__(self, psS, Pt, ssb, psO):
                self.psS, self.Pt, self.ssb, self.psO = psS, Pt, ssb, psO
                self.cnt = 0
                self.acc_i = 0
                self.pending = []

            def _drain(self, keep):
                while len(self.pending) > keep:
                    self.pending.pop(0)()

            def run(self, items, finish):
                acc = self.psO[self.acc_i % 2]
                racc = ("psO", self.acc_i % 2)
                self.acc_i += 1
                n = len(items)
                for idx, it in enumerate(items):
                    slot = self.cnt % 8
                    sslot = self.cnt % 4
                    self.cnt += 1
                    ps = self.psS[sslot]
                    rps = ("psS", sslot)
                    P = self.Pt[:, slot, :]
                    rP = ("P", slot)
                    mm(ps, it["lhsT"], it["rhs"], True, True, it["reads"], [rps])
                    src, rsrc = ps, rps
                    if it["mask"] is not None:
                        sb = self.ssb[:, slot % 4, :]
                        rsb = ("ssb", slot % 4)
                        S.add("vector", lambda e, sb=sb, ps=ps, m=it["mask"]: e.tensor_tensor(out=sb, in0=ps, in1=m, op=ALU.add), reads=[rps] + it["mreads"], writes=[rsb])
                        src, rsrc = sb, rsb
                    if it["bias"] is not None:
                        S.add("scalar", lambda e, P=P, src=src, b=it["bias"]: e.activation(out=P, in_=src, func=AF.Exp, bias=b, scale=1.0), reads=[rsrc] + it["breads"], writes=[rP])
                    else:
                        S.add("scalar", lambda e, P=P, src=src: e.activation(out=P, in_=src, func=AF.Exp), reads=[rsrc], writes=[rP])

                    def pv(P=P, rP=rP, it=it, first=(idx == 0), last=(idx == n - 1), acc=acc, racc=racc):
                        mm(acc, P, it["v"], first, last, [rP] + it["vreads"], [racc])
                        if last:
                            finish(acc, racc)
                    self.pending.append(pv)
                    self._drain(self.LA)

            def flush(self):
                self._drain(0)

        if on("B"):
            with contextlib.ExitStack() as st2:
                A.seek(OFF_LOC)
                KTb = [A.alloc([NTS * 128], BF16) for _ in range(2)]
                Vb = [A.alloc([NTS, 65], BF16) for _ in range(2)]
                NPB = 4
                Pt = [A.alloc([512], BF16) for _ in range(NPB)]
                rec = A.alloc([8], F32)
                psS = [psum(f"psS{i}", [128, 512]) for i in range(NPB)]
                psO = [psum(f"psO{i}", [128, 65]) for i in range(4)]
                fin_i = [0]
                if "o_fox" in dbg:
                    S.add("vector", lambda e: e.memset(o_fox, 0.0), writes=["o_attn"])
                pending = []
                gcnt = [0]
                acc_i = [0]

                def drain(keep):
                    while len(pending) > keep:
                        pending.pop(0)()

                def finish(acc, racc, dst):
                    r = rec[:, fin_i[0] % 8:fin_i[0] % 8 + 1]
                    rr = ("rec", fin_i[0] % 8)
                    fin_i[0] += 1
                    S.add("vector", lambda e: e.reciprocal(out=r, in_=acc[:, 64:65]), reads=[racc], writes=[rr])
                    S.add("vector", lambda e: e.tensor_scalar(out=dst, in0=acc[:, 0:64], scalar1=r, scalar2=None, op0=ALU.mult), reads=[racc, rr], writes=["o_attn"])

                for h in range(6):
                    par = h % 2
                    hp = (h % 2) * 64
                    pr = h // 2
                    KA = KTb[par]
                    rk, rv = ("KTb", par), ("Vb", par)
                    q4 = (NTS * 128) // 4
                    for q in range(4):
                        S.add("sync", lambda e, q=q, KA=KA, pr=pr, hp=hp: e.dma_start(out=KA[0:64, q * q4:(q + 1) * q4], in_=KT_scr[pr, hp:hp + 64, q * q4:(q + 1) * q4]), writes=[rk], dma=True)
                    S.add("vector", lambda e, KA=KA: e.memset(KA[64:65, :], 1.0), writes=[rk])
                    S.add("sync", lambda e, KA=KA, h=h: e.dma_start(out=KA[65:68, :], in_=Gd[h]), writes=[rk], dma=True)
                    S.add("gpsimd", lambda e, KA=KA: e.dma_start(out=KA[68:69, :], in_=padrow), writes=[rk], dma=True)
                    S.add("sync", lambda e, par=par, h=h: e.dma_start(out=Vb[par], in_=V_scr[h]), writes=[rv], dma=True)
                    for mp_ in range(NOWN // 2):
                        m0, m1 = 2 * mp_, 2 * mp_ + 1
                        J0, J1 = 8 * m0 + 8, 8 * m1 + 8
                        acc0 = psO[acc_i[0] % 4]; racc0 = ("psO", acc_i[0] % 4); acc_i[0] += 1
                        acc1 = psO[acc_i[0] % 4]; racc1 = ("psO", acc_i[0] % 4); acc_i[0] += 1
                        dst0 = o_fox[:, m0, h * 64:(h + 1) * 64]
                        dst1 = o_fox[:, m1, h * 64:(h + 1) * 64]
                        qrd = [rk, "QTf", "QTfa"]
                        for g in range(J0 // 2):
                            b = gcnt[0] % NPB
                            gcnt[0] += 1
                            ps = psS[b].rearrange("p (a q) -> p a q", q=256)
                            rps = ("psS", b)
                            P = Pt[b].rearrange("p (a q) -> p a q", q=256)
                            rP = ("P", b)
                            for jj in range(2):
                                j = 2 * g + jj
                                kt = KA[0:69, j * 128:(j + 1) * 128]
                                if j == J0 - 1:
                                    mm(ps[:, jj, 0:128], kt, QTf[0:69, h, m0 * 128:(m0 + 1) * 128], True, False, qrd, [rps])
                                    mm(ps[:, jj, 0:128], ident_b, foxtri_b, False, True, ["ident_b", "foxtri_b"], [rps])
                                    mm(ps[:, jj, 128:256], kt, QTf[0:69, h, m1 * 128:(m1 + 1) * 128], True, True, qrd, [rps])
                                else:
                                    mm(ps[:, jj, :], kt, QTf[0:69, h, m0 * 128:(m0 + 2) * 128], True, True, qrd, [rps])
                            S.add("scalar", lambda e, b=b: e.activation(out=Pt[b], in_=psS[b], func=AF.Exp), reads=[rps], writes=[rP])

                            def pv(g=g, P=P, rP=rP, acc0=acc0, racc0=racc0, acc1=acc1, racc1=racc1, J0=J0, par=par, rv=rv, dst0=dst0):
                                for jj in range(2):
                                    j = 2 * g + jj
                                    mm(acc0, P[:, jj, 0:128], Vb[par][:, j, :], j == 0, j == J0 - 1, [rP, rv], [racc0])
                                    mm(acc1, P[:, jj, 128:256], Vb[par][:, j, :], j == 0, False, [rP, rv], [racc1])
                                if 2 * g + 1 == J0 - 1:
                                    finish(acc0, racc0, dst0)
                            pending.append(pv)
                            drain(2)
                        for g in range(2):
                            b = gcnt[0] % NPB
                            gcnt[0] += 1
                            ps = psS[b].rearrange("p (a q) -> p a q", q=128)
                            rps = ("psS", b)
                            P = Pt[b].rearrange("p (a q) -> p a q", q=128)
                            rP = ("P", b)
                            for jj in range(4):
                                j = J0 + 4 * g + jj
                                diag = (j == J1 - 1)
                                mm(ps[:, jj, :], KA[0:69, j * 128:(j + 1) * 128], QTf[0:69, h, m1 * 128:(m1 + 1) * 128], True, not diag, qrd, [rps])
                                if diag:
                                    mm(ps[:, jj, :], ident_b, foxtri_b, False, True, ["ident_b", "foxtri_b"], [rps])
                            S.add("scalar", lambda e, b=b: e.activation(out=Pt[b], in_=psS[b], func=AF.Exp), reads=[rps], writes=[rP])

                            def pv2(g=g, P=P, rP=rP, acc1=acc1, racc1=racc1, J0=J0, J1=J1, par=par, rv=rv, dst1=dst1):
                                for jj in range(4):
                                    j = J0 + 4 * g + jj
                                    mm(acc1, P[:, jj, :], Vb[par][:, j, :], False, j == J1 - 1, [rP, rv], [racc1])
                                if g == 1:
                                    finish(acc1, racc1, dst1)
                            pending.append(pv2)
                            drain(2)
                drain(0)
                if "o_fox" in dbg:
                    A.seek(OFF_LOC)
                    tmp = A.alloc([NOWN, 384], F32)
                    S.add("vector", lambda e: e.tensor_copy(out=tmp, in_=o_fox), reads=["o_attn", ("KTb", 0), ("KTb", 1)], writes=[("KTb", 0)])
                    S.add("sync", lambda e: e.dma_start(out=dbg["o_fox"].rearrange("(m p) d -> p m d", p=128), in_=tmp), reads=[("KTb", 0)], writes=["dbgo"], dma=True)
                S.flush()

        if on("C"):
            with contextlib.ExitStack() as st2:
                A.seek(OFF_LOC)
                KTb = [A.alloc([NTS * 128], BF16)]
                Vb = [A.alloc([NTS, 65], BF16) for _ in range(2)]
                dbt = [A.alloc([17, 128], F32) for _ in range(2)]
                loc_save = A.off
                A.seek(OFF_QT)
                num = A.alloc([NOWN, 384], F32)
                A.seek(loc_save)
                den = A.alloc([NOWN, 6], F32)
                dsum = A.alloc([NOWN, 2], F32)
                NPB = 3
                Pt = [A.alloc([4, 128], BF16) for _ in range(NPB)]
                ssb = [A.alloc([4, 128], F32) for _ in range(NPB)]
                psS = [psum(f"psSc{i}", [128, 4, 128]) for i in range(NPB)]
                psO = [psum(f"psOc{i}", [128, 65]) for i in range(2)]
                pending = []
                gcnt = [0]
                acc_i = [0]

                def drain_c(keep):
                    while len(pending) > keep:
                        pending.pop(0)()

                def finish_c(acc, racc, m, hd):
                    S.add("vector", lambda e: e.tensor_copy(out=num[:, m, hd * 64:(hd + 1) * 64], in_=acc[:, 0:64]), reads=[racc], writes=["num"])
                    S.add("vector", lambda e: e.tensor_copy(out=den[:, m, hd:hd + 1], in_=acc[:, 64:65]), reads=[racc], writes=["den"])

                def load_v_c(hd_):
                    S.add("sync", lambda e, hd_=hd_: e.dma_start(out=Vb[hd_ % 2], in_=V_scr[6 + hd_]), writes=[("Vbc", hd_ % 2)], dma=True)
                    S.add("sync", lambda e, hd_=hd_: e.dma_start(out=dbt[hd_ % 2], in_=dilb[hd_]), writes=[("dbt", hd_ % 2)], dma=True)

                load_v_c(0)
                for hd in range(6):
                    par = 0
                    dpar = hd % 2
                    hp = (hd % 2) * 64
                    pr = 3 + hd // 2
                    prq = hd // 2
                    rk = ("KTb", 0)
                    q4 = (NTS * 128) // 4
                    for q in range(4):
                        S.add("sync", lambda e, q=q, pr=pr, hp=hp: e.dma_start(out=KTb[0][hp:hp + 64, q * q4:(q + 1) * q4], in_=KT_scr[pr, hp:hp + 64, q * q4:(q + 1) * q4]), writes=[rk], dma=True)
                    if hd + 1 < 6:
                        load_v_c(hd + 1)
                    rv = ("Vbc", dpar)
                    rd = ("dbt", dpar)
                    for m in range(NOWN):
                        sm = 8 * m + 7
                        dls = [dl for dl in range(DMAX[hd // 2] + 1) if sm - dl >= 0]
                        batches = [dls[i:i + 4] for i in range(0, len(dls), 4)]
                        acc = psO[acc_i[0] % 2]
                        racc = ("psO", acc_i[0] % 2)
                        acc_i[0] += 1
                        nbt = len(batches)
                        for bi, bt in enumerate(batches):
                            b = gcnt[0] % NPB
                            gcnt[0] += 1
                            ps, sb, P = psS[b], ssb[b], Pt[b]
                            rps, rsb, rP = ("psS", b), ("ssb", b), ("P", b)
                            nb = len(bt)
                            for t, dl in enumerate(bt):
                                j = sm - dl
                                mm(ps[:, t, :], KTb[par][hp:hp + 64, j * 128:(j + 1) * 128], QT[hp:hp + 64, prq, m * 128:(m + 1) * 128], True, True, [rk, "QT"], [rps])
                            S.add("vector", lambda e, sb=sb, ps=ps, nb=nb, d0=bt[0], dpar=dpar: e.tensor_tensor(out=sb[:, 0:nb, :], in0=ps[:, 0:nb, :], in1=dbt[dpar][:, d0:d0 + nb, :], op=ALU.add),
                                  reads=[rps, rd], writes=[rsb])
                            for t, dl in enumerate(bt):
                                j = sm - dl
                                if j <= 6:
                                    S.add("vector", lambda e, sb=sb, t=t, j=j: e.tensor_scalar(out=sb[:, t, :], in0=sb[:, t, :], scalar1=padb[:, j:j + 1], scalar2=None, op0=ALU.add), reads=[rsb, "cst"], writes=[rsb])
                            S.add("scalar", lambda e, P=P, sb=sb, nb=nb: e.activation(out=P[:, 0:nb, :], in_=sb[:, 0:nb, :], func=AF.Exp), reads=[rsb], writes=[rP])

                            def pv(bt=bt, P=P, rP=rP, acc=acc, racc=racc, bi=bi, nbt=nbt, sm=sm, par=par, dpar=dpar, rv=rv, m=m, hd=hd):
                                for t, dl in enumerate(bt):
                                    j = sm - dl
                                    mm(acc, P[:, t, :], Vb[dpar][:, j, :], (bi == 0 and t == 0), (bi == nbt - 1 and t == len(bt) - 1), [rP, rv], [racc])
                                if bi == nbt - 1:
                                    finish_c(acc, racc, m, hd)
                            pending.append(pv)
                            drain_c(1)
                drain_c(0)
                S.add("vector", lambda e: e.tensor_tensor(out=dsum, in0=den[:, :, 0:2], in1=den[:, :, 2:4], op=ALU.add), reads=["den"], writes=["dsum"])
                S.add("vector", lambda e: e.tensor_tensor(out=dsum, in0=dsum, in1=den[:, :, 4:6], op=ALU.add), reads=["den", "dsum"], writes=["dsum"])
                S.add("vector", lambda e: e.reciprocal(out=dsum, in_=dsum), reads=["dsum"], writes=["dsum"])
                for m in range(NOWN):
                    for hd in range(6):
                        S.add("vector", lambda e, m=m, hd=hd: e.tensor_scalar(out=o_dil[:, m, hd * 64:(hd + 1) * 64], in0=num[:, m, hd * 64:(hd + 1) * 64],
                                                                      scalar1=dsum[:, m, hd % 2:hd % 2 + 1], scalar2=None, op0=ALU.mult), reads=["num", "dsum"], writes=["o_attn"])
                S.flush()

        if on("Dm"):
            with contextlib.ExitStack() as st2:
                A.seek(OFF_LOC)
                Wkv = A.alloc([8, 512], BF16)
                mT = A.alloc([8, 256], BF16)
                KmT = A.alloc([2, 256], BF16)
                Vm = A.alloc([2, 4, 65], BF16)
                Pt = A.alloc([8, 128], BF16)
                ssb = A.alloc([4, 128], F32)
                rec = A.alloc([8], F32)
                psS = [psum(f"psSm{i}", [128, 128]) for i in range(4)]
                psO = [psum(f"psOm{i}", [128, 65]) for i in range(2)]
                psM = [psum(f"psM{i}", [128, 256]) for i in range(2)]
                S.add("gpsimd", lambda e: e.dma_start(out=Wkv, in_=w_mem_kv.rearrange("(c p) n -> p c n", p=128)), writes=["Wkv"], dma=True)
                S.add("gpsimd", lambda e: e.dma_start(out=mT, in_=memT.rearrange("(c p) n -> p c n", p=128)), writes=["mT"], dma=True)
                assert A.off <= OFF_LOC + 54 * 1024
                A.seek(OFF_LOC + 54 * 1024)
                Wg = A.alloc([8, 3072], BF16)
                Wbr = A.alloc([8, D], BF16)
                for b in range(3):
                    S.add("gpsimd", lambda e, b=b: e.dma_start(out=Wg[:, :, b * 1024:(b + 1) * 1024], in_=w_inv[:, :, 2566 + b * 1024:2566 + (b + 1) * 1024]), writes=["Wg"], dma=True)
                S.add("gpsimd", lambda e: e.dma_start(out=Wbr, in_=w_br.rearrange("(c p) n -> p c n", p=128)), writes=["Wbr"], dma=True)
                S.add("vector", lambda e: e.memset(Vm[:, :, :, 64:65], 1.0), writes=["Vm1"])
                for pr in range(2):
                    for c in range(8):
                        mm(psM[pr], Wkv[:, c, pr * 128:(pr + 1) * 128], mT[:, c, :], c == 0, c == 7, ["Wkv", "mT"], [("psM", pr)])
                    evac(pr, KmT[:, pr, :], psM[pr], [("psM", pr)], ["KmT"])
                for t2 in range(2):
                    for c in range(8):
                        mm(psM[t2], mT[:, c, t2 * 128:(t2 + 1) * 128], Wkv[:, c, 256:512], c == 0, c == 7, ["Wkv", "mT"], [("psM", t2)])
                    evac(t2, Vm[:, t2, :, 0:64], psM[t2].rearrange("p (h d) -> p h d", d=64), [("psM", t2)], ["Vm", "Vm1"])
                pipe = AttnPipe(psS, Pt, ssb, psO)
                fin_i = [0]

                def make_finish_m(dst):
                    def finish(acc, racc):
                        r = rec[:, fin_i[0] % 8:fin_i[0] % 8 + 1]
                        rr = ("rec", fin_i[0] % 8)
                        fin_i[0] += 1
                        S.add("vector", lambda e: e.reciprocal(out=r, in_=acc[:, 64:65]), reads=[racc], writes=[rr])
                        S.add("vector", lambda e: e.tensor_scalar(out=dst, in0=acc[:, 0:64], scalar1=r, scalar2=None, op0=ALU.mult), reads=[racc, rr], writes=["o_attn"])
                    return finish

                for hm in range(4):
                    hp = (hm % 2) * 64
                    pr = hm // 2
                    for m in range(NOWN):
                        items = []
                        for t2 in range(2):
                            items.append(dict(
                                lhsT=KmT[hp:hp + 64, pr, t2 * 128:(t2 + 1) * 128], rhs=QT[hp:hp + 64, 3 + pr, m * 128:(m + 1) * 128],
                                reads=["KmT", "QT"], mask=None, mreads=[], bias=None, breads=[], v=Vm[:, t2, hm, :], vreads=["Vm", "Vm1"]))
                        pipe.run(items, make_finish_m(o_mem[:, m, hm * 64:(hm + 1) * 64]))
                pipe.flush()
                S.flush()

        if on("M1"):
            with contextlib.ExitStack() as st2:
                A.seek(OFF_LOC + 54 * 1024)
                Wg = A.alloc([8, 3072], BF16)
                Wbr = A.alloc([8, D], BF16)
                A.seek(OFF_LOC2 + 4 * 1024)
                Wout = A.alloc([8, D], BF16)
                S.add("gpsimd", lambda e: e.dma_start(out=Wout, in_=w_out.rearrange("(c p) n -> p c n", p=128)), writes=["Wout"], dma=True)
                A.seek(OFF_LOC)
                xbb = [A.alloc([8, 4, 128], BF16) for _ in range(2)]
                oT = A.alloc([8, 512], BF16)
                sg = [A.alloc([512], F32) for _ in range(3)]
                t1 = A.alloc([512], F32)
                t2b = A.alloc([512], F32)
                assert A.off <= OFF_LOC2 + 4 * 1024, A.off
                psT = psum("psT", [128, 512], BF16)
                psB = [psum(f"psB{i}", [128, 512]) for i in range(3)]
                psG = [psum(f"psG{i}", [128, 512]) for i in range(2)]
                ev = 0
                gk = 0
                for b4 in range(4):
                    xb = xbb[b4 % 2]
                    rx = ("xbb", b4 % 2)
                    for c in range(8):
                        S.add("gpsimd", lambda e, xb=xb, b4=b4, c=c: e.dma_start(out=xb[:, c, :, :], in_=xTown[:, c, 4 * b4:4 * b4 + 4, 7, :]), writes=[rx], dma=True)
                    xbf = xb.rearrange("p c m t -> p c (m t)")
                    for ch in range(8):
                        for mm_ in range(4):
                            m = 4 * b4 + mm_
                            if ch < 3:
                                src = o_fox[:, m, ch * 128:(ch + 1) * 128]
                            elif ch < 6:
                                src = o_dil[:, m, (ch - 3) * 128:(ch - 2) * 128]
                            else:
                                src = o_mem[:, m, (ch - 6) * 128:(ch - 5) * 128]
                            S.add("tensor", lambda e, src=src, mm_=mm_: e.transpose(psT[:, mm_ * 128:(mm_ + 1) * 128], src, ident_b), reads=["o_attn", "ident_b"], writes=["psT"])
                        evac(ev, oT[:, ch, :], psT, ["psT"], ["oT"]); ev += 1
                    for dc in range(8):
                        for bi, chs in enumerate(((0, 1, 2), (3, 4, 5), (6, 7))):
                            for ci, ch in enumerate(chs):
                                mm(psB[bi], Wbr[:, ch, dc * 128:(dc + 1) * 128], oT[:, ch, :], ci == 0, ci == len(chs) - 1, ["Wbr", "oT"], [("psB", bi)])
                        for b in range(3):
                            pg = psG[gk % 2]
                            rg = ("psG", gk % 2)
                            gk += 1
                            for c in range(8):
                                mm(pg, Wg[:, c, b * 1024 + dc * 128:b * 1024 + (dc + 1) * 128], xbf[:, c, :], c == 0, c == 7, ["Wg", rx], [rg])
                            S.add("scalar", lambda e, b=b, pg=pg: e.activation(out=sg[b], in_=pg, func=AF.Sigmoid), reads=[rg], writes=[("sg", b)])
                        S.add("vector", lambda e: e.tensor_tensor(out=t1, in0=psB[0], in1=sg[0], op=ALU.mult), reads=[("psB", 0), ("sg", 0)], writes=["t1"])
                        S.add("vector", lambda e: e.tensor_tensor(out=t2b, in0=psB[1], in1=sg[1], op=ALU.mult), reads=[("psB", 1), ("sg", 1)], writes=["t2b"])
                        S.add("vector", lambda e: e.tensor_tensor(out=t1, in0=t1, in1=t2b, op=ALU.add), reads=["t1", "t2b"], writes=["t1"])
                        S.add("vector", lambda e: e.tensor_tensor(out=t2b, in0=psB[2], in1=sg[2], op=ALU.mult), reads=[("psB", 2), ("sg", 2)], writes=["t2b"])
                        S.add("vector", lambda e, dc=dc, b4=b4: e.tensor_tensor(out=mergedT[:, dc, b4 * 512:(b4 + 1) * 512], in0=t1, in1=t2b, op=ALU.add), reads=["t1", "t2b"], writes=["mergedT"])
                S.flush()

        def layer_norm(z, rz, dst, rdst, g_rep, b_rep, stats, mv, rstd):
            zz = z.rearrange("p (a b) -> p a b", b=512)
            for a in range(2):
                S.add("vector", lambda e, a=a: e.bn_stats(out=stats[:, a, :], in_=zz[:, a, :]), reads=[rz], writes=["stats"])
            S.add("vector", lambda e: e.bn_aggr(out=mv, in_=stats.rearrange("p a b -> p (a b)")), reads=["stats"], writes=["mv"])
            S.add("vector", lambda e: e.tensor_scalar(out=rstd, in0=mv[:, 1:2], scalar1=1e-5, scalar2=None, op0=ALU.add), reads=["mv"], writes=["rstd"])
            S.add("scalar", lambda e: e.activation(out=rstd, in_=rstd, func=AF.Sqrt), reads=["rstd"], writes=["rstd"])
            S.add("vector", lambda e: e.reciprocal(out=rstd, in_=rstd), reads=["rstd"], writes=["rstd"])
            S.add("vector", lambda e: e.tensor_scalar(out=z, in0=z, scalar1=mv[:, 0:1], scalar2=rstd, op0=ALU.subtract, op1=ALU.mult), reads=[rz, "mv", "rstd"], writes=[rz])
            S.add("gpsimd", lambda e: e.tensor_tensor(out=z, in0=z, in1=g_rep, op=ALU.mult), reads=[rz, "lnp"], writes=[rz])
            S.add("vector", lambda e: e.tensor_tensor(out=dst, in0=z, in1=b_rep, op=ALU.add), reads=[rz, "lnp"], writes=[rdst])

        if on("M2"):
            with contextlib.ExitStack() as st2:
                A.seek(OFF_LOC2 + 4 * 1024)
                Wout = A.alloc([8, D], BF16)
                g_rep = A.alloc([D], F32)
                b_rep = A.alloc([D], F32)
                xown = [A.alloc([D], F32) for _ in range(2)]
                zb = [A.alloc([D], F32) for _ in range(2)]
                stats = A.alloc([2, 6], F32)
                mv = A.alloc([2], F32)
                rstd = A.alloc([1], F32)
                psY = [psum(f"psY{i}", [128, 512]) for i in range(4)]
                S.add("sync", lambda e: e.dma_start(out=g_rep, in_=lnrep[0]), writes=["lnp"], dma=True)
                S.add("sync", lambda e: e.dma_start(out=b_rep, in_=lnrep[1]), writes=["lnp"], dma=True)
                for m in range(NOWN):
                    xo_ = xown[m % 2]
                    rxo = ("xown", m % 2)
                    z = zb[m % 2]
                    rz = ("z", m % 2)
                    S.add("sync", lambda e, xo_=xo_, m=m: e.dma_start(out=xo_, in_=x_own[m * 128:(m + 1) * 128, :]), writes=[rxo], dma=True)
                    for eh in range(2):
                        py = psY[(2 * m + eh) % 4]
                        rpy = ("psY", (2 * m + eh) % 4)
                        for dc in range(8):
                            mm(py, mergedT[:, dc, m * 128:(m + 1) * 128], Wout[:, dc, eh * 512:(eh + 1) * 512], dc == 0, dc == 7, ["mergedT", "Wout"], [rpy])
                        S.add("vector", lambda e, z=z, xo_=xo_, py=py, eh=eh: e.scalar_tensor_tensor(
                            out=z[:, eh * 512:(eh + 1) * 512], in0=xo_[:, eh * 512:(eh + 1) * 512], scalar=ALPHA, in1=py, op0=ALU.mult, op1=ALU.add),
                            reads=[rxo, rpy], writes=[rz])
                    layer_norm(z, rz, h1[:, m, :], "h1", g_rep, b_rep, stats, mv, rstd)
                if "h1" in dbg:
                    S.add("sync", lambda e: e.dma_start(out=dbg["h1"].rearrange("(m p) d -> p m d", p=128), in_=h1), reads=["h1"], writes=["dbgh"], dma=True)
                S.flush()

        if on("R"):
            with contextlib.ExitStack() as st2:
                A.seek(OFF_LOC2)
                Wr = A.alloc([8, NE], F32)
                hT = [A.alloc([8, 128], F32) for _ in range(2)]
                hbf = [A.alloc([D], BF16) for _ in range(2)]
                lg2 = [A.alloc([NE], F32) for _ in range(2)]
                mx82 = [A.alloc([8], F32) for _ in range(2)]
                negm2 = [A.alloc([1], F32) for _ in range(2)]
                ex42 = [A.alloc([4], F32) for _ in range(2)]
                esum2 = [A.alloc([1], F32) for _ in range(2)]
                maskall = A.alloc([NOWN, NE], BF16)
                destf2 = [A.alloc([NE], F32) for _ in range(2)]
                selk2 = [A.alloc([NE], F32) for _ in range(2)]
                junk2 = [A.alloc([NE], F32) for _ in range(2)]
                dk2 = [A.alloc([4], F32) for _ in range(2)]
                psTr = [psum(f"psTr{i}", [128, 4, 128]) for i in range(2)]
                psL2 = [psum(f"psL{i}", [128, NE]) for i in range(2)]
                psP2 = [psum(f"psP{i}", [128, NE]) for i in range(2)]
                S.add("sync", lambda e: e.dma_start(out=Wr, in_=w_router.rearrange("(c p) n -> p c n", p=128)), writes=["Wr"], dma=True)
                for m in range(NOWN):
                    p_ = m % 2
                    lg, mx8, negm, ex4, esum = lg2[p_], mx82[p_], negm2[p_], ex42[p_], esum2[p_]
                    destf, selk, junk, dk, psL, psP = destf2[p_], selk2[p_], junk2[p_], dk2[p_], psL2[p_], psP2[p_]
                    rlg, rmx, rng_, rex, res_ = ("lg", p_), ("mx8", p_), ("negm", p_), ("ex4", p_), ("esum", p_)
                    rdf, rsk, rjk, rdk, rpl_, rpp = ("destf", p_), ("selk", p_), ("junk", p_), ("dk", p_), ("psL", p_), ("psP", p_)
                    hTm = hT[m % 2]
                    rh = ("hT", m % 2)
                    for c in range(8):
                        S.add("tensor", lambda e, m=m, c=c: e.transpose(psTr[c // 4][:, c % 4, :], h1[:, m, c * 128:(c + 1) * 128], ident_f), reads=["h1", "cst"], writes=[("psTr", c // 4)])
                        if c % 4 == 3:
                            evac(c // 4, hTm[:, c - 3:c + 1, :], psTr[c // 4], [("psTr", c // 4)], [rh])
                    for c in range(8):
                        mm(psL, hTm[:, c, :], Wr[:, c, :], c == 0, c == 7, [rh, "Wr"], [rpl_])
                    S.add("vector", lambda e, lg=lg, psL=psL: e.tensor_tensor(out=lg, in0=psL, in1=br_rep, op=ALU.add), reads=[rpl_, "cst"], writes=[rlg])
                    S.add("vector", lambda e, lg=lg, mx8=mx8: e.max(out=mx8, in_=lg), reads=[rlg], writes=[rmx])
                    S.add("vector", lambda e, m=m, lg=lg, mx8=mx8: e.tensor_scalar(out=maskall[:, m, :], in0=lg, scalar1=mx8[:, 3:4], scalar2=None, op0=ALU.is_ge), reads=[rlg, rmx], writes=[("maskall", m)])
                    S.add("vector", lambda e, negm=negm, mx8=mx8: e.tensor_scalar(out=negm, in0=mx8[:, 0:1], scalar1=-1.0, scalar2=None, op0=ALU.mult), reads=[rmx], writes=[rng_])
                    S.add("scalar", lambda e, ex4=ex4, mx8=mx8, negm=negm: e.activation(out=ex4, in_=mx8[:, 0:4], func=AF.Exp, bias=negm, scale=1.0), reads=[rmx, rng_], writes=[rex])
                    S.add("vector", lambda e, esum=esum, ex4=ex4: e.tensor_reduce(out=esum, in_=ex4, axis=mybir.AxisListType.X, op=ALU.add), reads=[rex], writes=[res_])
                    S.add("vector", lambda e, esum=esum: e.reciprocal(out=esum, in_=esum), reads=[res_], writes=[res_])
                    S.add("vector", lambda e, m=m, ex4=ex4, esum=esum: e.tensor_scalar(out=gate4[:, m, :], in0=ex4, scalar1=esum, scalar2=None, op0=ALU.mult), reads=[rex, res_], writes=[("gate4", m)])
                    for m2 in range(m):
                        mm(psP, ones_b, maskall[:, m2, :], m2 == 0, False, [("maskall", m2), "ones_b"], [rpp])
                    mm(psP, tri_strict_b, maskall[:, m, :], m == 0, True, [("maskall", m), "tsb"], [rpp])
                    S.add("vector", lambda e, destf=destf, psP=psP: e.scalar_tensor_tensor(out=destf, in0=psP, scalar=float(CAP - 1), in1=iotaC, op0=ALU.min, op1=ALU.add), reads=[rpp, "cst"], writes=[rdf])
                    for k in range(4):
                        S.add("vector", lambda e, k=k, selk=selk, lg=lg, mx8=mx8: e.tensor_scalar(out=selk, in0=lg, scalar1=mx8[:, k:k + 1], scalar2=None, op0=ALU.is_equal), reads=[rlg, rmx], writes=[rsk])
                        S.add("vector", lambda e, k=k, junk=junk, selk=selk, destf=destf: e.tensor_tensor(out=junk, in0=selk, in1=destf, op=ALU.mult), reads=[rsk, rdf], writes=[rjk])
                        S.add("vector", lambda e, k=k, dk=dk, junk=junk: e.tensor_reduce(out=dk[:, k:k + 1], in_=junk, axis=mybir.AxisListType.X, op=ALU.add), reads=[rjk], writes=[rdk])
                    S.add("vector", lambda e, m=m, dk=dk: e.tensor_copy(out=dest_i[:, m, :], in_=dk), reads=[rdk], writes=[("dest_i", m)])
                    hb = hbf[m % 2]
                    rhb = ("hbf", m % 2)
                    S.add("scalar", lambda e, hb=hb, m=m: e.copy(out=hb, in_=h1[:, m, :]), reads=["h1"], writes=[rhb])
                    for k in range(4):
                        S.add("gpsimd", lambda e, hb=hb, m=m, k=k: e.indirect_dma_start(
                            out=X_scr, out_offset=bass.IndirectOffsetOnAxis(ap=dest_i[:, m, k:k + 1], axis=0), in_=hb, in_offset=None),
                            reads=[rhb, ("dest_i", m)], writes=[("X_scr", m, k)], dma=True)
                if "route" in dbg:
                    A.seek(OFF_LOC2 + 48 * 1024)
                    tmp = A.alloc([NOWN, 8], F32)
                    S.add("vector", lambda e: e.tensor_copy(out=tmp[:, :, 0:4], in_=dest_i), reads=[("dest_i", mm_) for mm_ in range(NOWN)], writes=["tmpr"])
                    S.add("vector", lambda e: e.tensor_copy(out=tmp[:, :, 4:8], in_=gate4), reads=[("gate4", mm_) for mm_ in range(NOWN)], writes=["tmpr"])
                    S.add("sync", lambda e: e.dma_start(out=dbg["route"].rearrange("(m p) d -> p m d", p=128), in_=tmp), reads=["tmpr"], writes=["dbgr"], dma=True)
                S.flush()

        if on("E"):
            with contextlib.ExitStack() as st2:
                A.seek(OFF_QT)
                W2h = [A.alloc([8, 512], BF16) for _ in range(3)]
                b1sb = A.alloc([NE * 16], F32)
                b2rep = [A.alloc([D], F32)]
                Ysb = [A.alloc([D], F32) for _ in range(2)]
                Xe = [A.alloc([3, D], BF16)]
                A.seek(OFF_LOC2)
                Xe.append(A.alloc([3, D], BF16))
                NQ = 5
                W1q = [A.alloc([8, 4, 128], BF16) for _ in range(NQ)]
                XT = [A.alloc([8, CAP], BF16) for _ in range(2)]
                actT = A.alloc([8, CAP], BF16)
                glu = [A.alloc([CAP], F32) for _ in range(2)]
                sig = [A.alloc([CAP], F32) for _ in range(4)]
                lin = [A.alloc([CAP], F32) for _ in range(2)]
                psX = psum("psX", [128, 512], BF16)
                psH = [psum(f"psH{i}", [128, CAP]) for i in range(4)]
                psY2 = [psum(f"psY2{i}", [128, 512]) for i in range(2)]
                S.add("sync", lambda e: e.dma_start(out=b1sb, in_=b1T), writes=["b1sb"], dma=True)
                Xv = X_scr.rearrange("(e sb p) d -> e p sb d", sb=3, p=128)
                Yv = Y_scr.rearrange("(e sb p) d -> e sb p d", sb=3, p=128)
                hk = 0
                yk = 0
                pieces = []
                for ex_ in range(NE):
                    pieces += [(ex_, "w1", q_) for q_ in range(4)] + [(ex_, "w2", d_) for d_ in range(2)]
                pbuf = {}
                pstate = {"next": 0, "qi": 0, "w2i": 0}

                def ensure(upto):
                    while pstate["next"] < min(upto, len(pieces)):
                        i = pstate["next"]
                        pstate["next"] += 1
                        ex_, kind, j = pieces[i]
                        if kind == "w1":
                            w1v_ = w_exp_in[ex_].rearrange("(c p) f -> p c f", p=128)
                            f0 = (0, 1024, 512, 1536)[j]
                            wq = W1q[pstate["qi"] % NQ]
                            rwq = ("W1q", pstate["qi"] % NQ)
                            pstate["qi"] += 1
                            S.add("gpsimd", lambda e, wq=wq, w1v_=w1v_, f0=f0: e.dma_start(out=wq.rearrange("p c r f -> p c (r f)"), in_=w1v_[:, :, f0:f0 + 512]), writes=[rwq], dma=True)
                            pbuf[i] = (wq, rwq)
                        else:
                            w2v_ = w_exp_out[ex_].rearrange("(c p) f -> p c f", p=128)
                            w2 = W2h[pstate["w2i"] % 3]
                            rw2 = ("W2h", pstate["w2i"] % 3)
                            pstate["w2i"] += 1
                            S.add("gpsimd", lambda e, w2=w2, w2v_=w2v_, j=j: e.dma_start(out=w2, in_=w2v_[:, :, j * 512:(j + 1) * 512]), writes=[rw2], dma=True)
                            pbuf[i] = (w2, rw2)

                def load_x(ex_):
                    S.add("sync", lambda e, ex_=ex_: e.dma_start(out=Xe[ex_ % 2], in_=Xv[ex_]), reads=["X_scr"], writes=[("Xe", ex_ % 2)], dma=True)

                def transposes(ex_):
                    xe = Xe[ex_ % 2]
                    rxe = ("Xe", ex_ % 2)
                    for c in range(8):
                        for sb in range(3):
                            S.add("tensor", lambda e, xe=xe, c=c, sb=sb: e.transpose(psX[:, sb * 128:(sb + 1) * 128], xe[:, sb, c * 128:(c + 1) * 128], ident_b), reads=[rxe, "ident_b"], writes=["psX"])
                        S.add("vector", lambda e, c=c, ex_=ex_: e.tensor_copy(out=XT[ex_ % 2][:, c, :], in_=psX[:, 0:CAP]), reads=["psX"], writes=[("XT", ex_ % 2)])

                load_x(0)
                ensure(5)
                transposes(0)
                for ex in range(NE):
                    xt = XT[ex % 2]
                    rxt = ("XT", ex % 2)
                    if ex + 1 < NE:
                        load_x(ex + 1)
                    S.add("sync", lambda e, ex=ex: e.dma_start(out=b2rep[0], in_=b_exp_out[ex:ex + 1, :].partition_broadcast(128)), writes=[("b2rep", 0)], dma=True)
                    for half in range(2):
                        pG = ex * 6 + 2 * half
                        ensure(pG + 5)
                        wq, rwq = pbuf.pop(pG)
                        for r in range(4):
                            fb = 4 * half + r
                            pg = psH[hk % 4]; rpg = ("psH", hk % 4); hk += 1
                            for c in range(8):
                                mm(pg, wq[:, c, r, :], xt[:, c, :], c == 0, c == 7, [rwq, rxt], [rpg])
                            g_ = glu[r % 2]; rg_ = ("glu", r % 2)
                            s_ = sig[r]; rs_ = ("sig", r)
                            cg = ex * 16 + fb
                            S.add("vector", lambda e, g_=g_, pg=pg, cg=cg: e.tensor_scalar(out=g_, in0=pg, scalar1=b1sb[:, cg:cg + 1], scalar2=7.0, op0=ALU.add, op1=ALU.min), reads=[rpg, "b1sb"], writes=[rg_])
                            S.add("scalar", lambda e, g_=g_, s_=s_: e.activation(out=s_, in_=g_, func=AF.Silu, scale=1.702), reads=[rg_], writes=[rs_])
                        ensure(pG + 6)
                        wl, rwl = pbuf.pop(pG + 1)
                        for r in range(4):
                            fb = 4 * half + r
                            pl = psH[hk % 4]; rpl = ("psH", hk % 4); hk += 1
                            for c in range(8):
                                mm(pl, wl[:, c, r, :], xt[:, c, :], c == 0, c == 7, [rwl, rxt], [rpl])
                            l_ = lin[r % 2]; rl_ = ("lin", r % 2)
                            s_ = sig[r]; rs_ = ("sig", r)
                            cl = ex * 16 + 8 + fb
                            S.add("vector", lambda e, l_=l_, pl=pl, cl=cl: e.tensor_scalar(out=l_, in0=pl, scalar1=b1sb[:, cl:cl + 1], scalar2=7.0, op0=ALU.add, op1=ALU.min), reads=[rpl, "b1sb"], writes=[rl_])
                            S.add("vector", lambda e, l_=l_: e.tensor_scalar(out=l_, in0=l_, scalar1=-7.0, scalar2=1.0, op0=ALU.max, op1=ALU.add), reads=[rl_], writes=[rl_])
                            S.add("vector", lambda e, fb=fb, s_=s_, l_=l_: e.scalar_tensor_tensor(out=actT[:, fb, :], in0=s_, scalar=1.0 / 1.702, in1=l_, op0=ALU.mult, op1=ALU.mult), reads=[rs_, rl_], writes=["actT"])
                    if ex + 1 < NE:
                        transposes(ex + 1)
                    ensure(ex * 6 + 4 + 5)
                    w2s = [pbuf.pop(ex * 6 + 4), pbuf.pop(ex * 6 + 5)]
                    for sb in range(3):
                        ys = Ysb[yk % 2]
                        rys = ("Ysb", yk % 2)
                        yk += 1
                        for dh in range(2):
                            w2, rw2 = w2s[dh]
                            py = psY2[dh]
                            rpy = ("psY2", dh)
                            for fc in range(8):
                                mm(py, actT[:, fc, sb * 128:(sb + 1) * 128], w2[:, fc, :], fc == 0, fc == 7, ["actT", rw2], [rpy])
                            S.add("vector", lambda e, ys=ys, py=py, dh=dh: e.tensor_tensor(out=ys[:, dh * 512:(dh + 1) * 512], in0=py, in1=b2rep[0][:, dh * 512:(dh + 1) * 512], op=ALU.add),
                                  reads=[rpy, ("b2rep", 0)], writes=[rys])
                        S.add("sync", lambda e, ys=ys, ex=ex, sb=sb: e.dma_start(out=Yv[ex, sb], in_=ys), reads=[rys], writes=["Y_scr"], dma=True)
                S.flush()

        if on("F"):
            with contextlib.ExitStack() as st2:
                A.seek(OFF_LOC2)
                g_rep = A.alloc([D], F32)
                b_rep = A.alloc([D], F32)
                Yk = [A.alloc([D], F32) for _ in range(8)]
                zb = [A.alloc([D], F32) for _ in range(2)]
                ob = [A.alloc([D], F32) for _ in range(2)]
                stats = A.alloc([2, 6], F32)
                mv = A.alloc([2], F32)
                rstd = A.alloc([1], F32)
                S.add("sync", lambda e: e.dma_start(out=g_rep, in_=lnrep[2]), writes=["lnp"], dma=True)
                S.add("sync", lambda e: e.dma_start(out=b_rep, in_=lnrep[3]), writes=["lnp"], dma=True)
                for m in range(NOWN):
                    z = zb[m % 2]
                    rz = ("z", m % 2)
                    S.add("vector", lambda e, z=z, m=m: e.tensor_scalar(out=z, in0=h1[:, m, :], scalar1=ALPHA, scalar2=None, op0=ALU.mult), reads=["h1"], writes=[rz])
                    for k in range(4):
                        yk_ = Yk[(4 * m + k) % 8]
                        ryk = ("Yk", (4 * m + k) % 8)
                        S.add("gpsimd", lambda e, yk_=yk_, m=m, k=k: e.indirect_dma_start(
                            out=yk_, out_offset=None, in_=Y_scr, in_offset=bass.IndirectOffsetOnAxis(ap=dest_i[:, m, k:k + 1], axis=0)),
                            reads=["Y_scr", "dest_i"], writes=[ryk], dma=True)
                        S.add("vector", lambda e, z=z, yk_=yk_, m=m, k=k: e.scalar_tensor_tensor(out=z, in0=yk_, scalar=gate4[:, m, k:k + 1], in1=z, op0=ALU.mult, op1=ALU.add), reads=[ryk, "gate4", rz], writes=[rz])
                    o_ = ob[m % 2]
                    ro = ("ob", m % 2)
                    layer_norm(z, rz, o_, ro, g_rep, b_rep, stats, mv, rstd)
                    S.add("sync", lambda e, o_=o_, m=m: e.dma_start(out=out_d[m * 128:(m + 1) * 128, :], in_=o_), reads=[ro], writes=["out"], dma=True)
                S.flush()
    return nc


def _t5_bucket(dist):
    dist = np.asarray(dist, dtype=np.int64)
    nf = np.maximum(dist, 16).astype(np.float32)
    large = 16 + (np.log(nf / np.float32(16)) / np.float32(math.log(2048 / 16)) * np.float32(16)).astype(np.int32)
    large = np.minimum(large, 31)
    return np.where(dist < 16, dist, large).astype(np.int64)


def _host_consts(core, b_fgate, b_router):
    n = 6 * 128 + 32 + NTS + 24 + 32
    c = np.zeros((128, n), np.float32)
    idx = np.arange(128)
    c[:, 0:128] = np.eye(128, dtype=np.float32)
    c[:, 128:256] = (idx[:, None] <= idx[None, :])
    c[:, 256:384] = (idx[:, None] < idx[None, :])
    c[:, 384:512] = 1.0
    c[64, 512:640] = 1.0
    c[:, 640:768] = np.where(idx[:, None] <= idx[None, :], 0.0, NEG)
    c[:, 768:800] = (np.arange(NE) * CAP)[None, :]
    s = np.arange(NTS)
    a = s - 7 + core
    c[:, 800:800 + NTS] = np.where((a >= 0) & (a < 128), 0.0, NEG)[None, :]
    c[:, 800 + NTS:800 + NTS + 24] = np.tile(np.asarray(b_fgate, np.float32).reshape(1, 6), (128, 4))
    c[:, 800 + NTS + 24:800 + NTS + 56] = np.asarray(b_router, np.float32).reshape(1, NE)
    return c


def _dil_bias(t5_bias):
    out = np.full((6, 128, 17, 128), NEG, np.float32)
    k = np.arange(128)[:, None]
    q = np.arange(128)[None, :]
    for hd in range(6):
        window, dil = DIL_CFG[hd // 2]
        for dl in range(DMAX[hd // 2] + 1):
            dist = 128 * dl + q - k
            valid = (dist >= 0) & (dist % dil == 0) & (dist <= window)
            b = _t5_bucket(np.clip(dist, 0, None))
            vals = t5_bias[b, hd]
            out[hd, :, dl, :] = np.where(valid, vals, np.float32(NEG))
    return out


_CACHE = {}


def prepare_inputs(inputs):
    x = np.asarray(inputs["x"], np.float32)[0]
    T = NTS * 128
    shared = {
        "memT": np.ascontiguousarray(np.asarray(inputs["mem"], np.float32)[0].T),
        "w_in": np.ascontiguousarray(np.asarray(inputs["w_in"], np.float32)[0]),
        "w_mem_kv": np.ascontiguousarray(np.asarray(inputs["w_mem_kv"], np.float32)[0]),
        "w_br": np.ascontiguousarray(np.concatenate([np.asarray(inputs["w_br_fox"], np.float32)[0], np.asarray(inputs["w_br_dil"], np.float32)[0],
                                                     np.asarray(inputs["w_br_mem"], np.float32)[0]], axis=0)),
        "w_out": np.ascontiguousarray(np.asarray(inputs["w_out"], np.float32)[0]),
        "w_router": np.ascontiguousarray(np.asarray(inputs["w_router"], np.float32)[0]),
        "w_exp_in": np.ascontiguousarray(np.asarray(inputs["w_exp_in"], np.float32)[0]),
        "w_exp_out": np.ascontiguousarray(np.asarray(inputs["w_exp_out"], np.float32)[0]),
        "b_exp_out": np.ascontiguousarray(np.asarray(inputs["b_exp_out"], np.float32)[0]),
        "b1T": np.ascontiguousarray(np.asarray(inputs["b_exp_in"], np.float32)[0].reshape(NE, 16, 128).transpose(2, 0, 1).reshape(128, NE * 16)),
        "lnrep": np.ascontiguousarray(np.stack([np.broadcast_to(np.asarray(inputs[k], np.float32)[0][None, :], (128, D)) for k in ("ln1_g", "ln1_b", "ln2_g", "ln2_b")])),
        "dilb": _dil_bias(np.asarray(inputs["t5_bias"], np.float32)),
    }
    xt = x.reshape(NOWN, NCORES, 128, D)
    in_maps = []
    for i in range(NCORES):
        xs = np.zeros((T, D), np.float32)
        xs[(7 - i) * 128:(7 - i) * 128 + S_LEN] = x
        mp = dict(shared)
        mp["xT"] = np.ascontiguousarray(xs.T)
        mp["x_own"] = np.ascontiguousarray(xt[:, i].reshape(NOWN * 128, D))
        mp["cst"] = _host_consts(i, inputs["b_fgate"], inputs["b_router"])
        a_tile = np.arange(NTS) - 7 + i
        mp["padrow"] = np.repeat(np.where((a_tile >= 0) & (a_tile < 128), 0.0, NEG).astype(np.float32), 128)[None, :]
        in_maps.append(mp)
    return in_maps


def assemble(results, key="out", width=D):
    out = np.zeros((NOWN, NCORES, 128, width), np.float32)
    for i in range(NCORES):
        out[:, i] = np.asarray(results[i][key], np.float32).reshape(NOWN, 128, width)
    return out.reshape(1, S_LEN, width)


def kernel(**inputs):
    if "nc" not in _CACHE:
        _CACHE["nc"] = build_program()
    in_maps = prepare_inputs(inputs)
    res = run_bass_kernel_spmd(_CACHE["nc"], in_maps, core_ids=list(range(NCORES)))
    return assemble(res.results)
```

```python
import contextlib
import math
import numpy as np
import concourse.bass as bass
import concourse.mybir as mybir
from concourse.bass_utils import run_bass_kernel_spmd

F32 = mybir.dt.float32
BF16 = mybir.dt.bfloat16
I32 = mybir.dt.int32
AF = mybir.ActivationFunctionType
ALU = mybir.AluOpType

NCORES = 8
D = 1024
S_LEN = 16384
NTS = 136
NOWN = 16
NE = 32
CAP = 384
ALPHA = 2.0 ** 0.25
NEG = -30000.0
DIL_CFG = ((128, 1), (512, 4), (2048, 16))
DMAX = (1, 4, 16)
ENGINES = ("tensor", "vector", "scalar", "gpsimd", "sync")
PHASES = ("A", "Q", "B", "C", "Dm", "M1", "M2", "R", "E", "F")


class Sched:
    NDS = 48

    def __init__(self, nc, stack):
        self.nc = nc
        self.esem = {e: stack.enter_context(nc.semaphore(f"s_{e}")) for e in ENGINES}
        self.dsem = [stack.enter_context(nc.semaphore(f"d_{i}")) for i in range(self.NDS)]
        self.ecount = {e: 0 for e in ENGINES}
        self.dcount = [0] * self.NDS
        self.dnext = {"sync": 0, "gpsimd": 0}
        self.reset()

    def reset(self):
        self.ops = []
        self.last_writer = {}
        self.readers = {}

    def add(self, engine, fn, reads=(), writes=(), dma=False):
        deps = set()
        for r in reads:
            if r in self.last_writer:
                deps.add(self.last_writer[r])
        for w in writes:
            if w in self.last_writer:
                deps.add(self.last_writer[w])
            for rd in self.readers.get(w, ()):
                deps.add(rd)
        oid = len(self.ops)
        self.ops.append((engine, fn, deps, dma))
        for r in reads:
            self.readers.setdefault(r, []).append(oid)
        for w in writes:
            self.last_writer[w] = oid
            self.readers[w] = []
        return oid

    def flush(self):
        nc = self.nc
        ops = self.ops
        n = len(ops)
        if n == 0:
            return
        needed = [False] * n
        for (_, _, deps, _) in ops:
            for d in deps:
                needed[d] = True
        last_on = {}
        for i, (e, _, _, dma) in enumerate(ops):
            if not dma:
                last_on[e] = i
        for i in last_on.values():
            needed[i] = True
        comp = [None] * n
        issue_wait = {}
        prev_on_sem = {}
        for i, (eng, fn, deps, dma) in enumerate(ops):
            if dma:
                half = self.NDS // 2
                s = self.dnext[eng] + (half if eng == "gpsimd" else 0)
                self.dnext[eng] = (self.dnext[eng] + 1) % half
                if self.dcount[s] > 0:
                    issue_wait[i] = (s, self.dcount[s])
                self.dcount[s] += 16
                comp[i] = ("d", s, self.dcount[s])
            elif needed[i]:
                self.ecount[eng] += 1
                comp[i] = ("e", eng, self.ecount[eng])
        final_e = dict(self.ecount)
        final_d = list(self.dcount)
        esem, dsem = self.esem, self.dsem

        def run_engine(ename):
            def body(eng):
                waited = {}

                def do_wait(key, semh, val):
                    if waited.get(key, -1) >= val:
                        return
                    eng.wait_ge(semh, val)
                    waited[key] = val

                for i, (e, fn, deps, dma) in enumerate(ops):
                    if e != ename:
                        continue
                    for d in sorted(deps):
                        c = comp[d]
                        if c is None:
                            continue
                        if c[0] == "e":
                            if c[1] == "tensor" and ename == "tensor":
                                continue
                            do_wait(("e", c[1]), esem[c[1]], c[2])
                        else:
                            do_wait(("d", c[1]), dsem[c[1]], c[2])
                    if i in issue_wait:
                        s, v = issue_wait[i]
                        do_wait(("d", s), dsem[s], v)
                    ins = fn(eng)
                    c = comp[i]
                    if c is not None:
                        if c[0] == "e":
                            ins.then_inc(esem[c[1]], 1)
                        else:
                            ins.then_inc(dsem[c[1]], 16)
                for e2 in ENGINES:
                    if final_e[e2] > 0:
                        do_wait(("e", e2), esem[e2], final_e[e2])
                for s in range(self.NDS):
                    if final_d[s] > 0:
                        do_wait(("d", s), dsem[s], final_d[s])
            return body

        with nc.Block() as block:
            block.tensor(run_engine("tensor"))
            block.vector(run_engine("vector"))
            block.scalar(run_engine("scalar"))
            block.gpsimd(run_engine("gpsimd"))
            block.sync(run_engine("sync"))
        self.reset()


class Arena:
    def __init__(self, nc, stack, nbytes):
        self.t = stack.enter_context(nc.sbuf_tensor("arena", [128, nbytes // 4], F32))
        self.nbytes = nbytes
        self.off = 0

    def seek(self, off):
        self.off = off

    def alloc(self, free_shape, dtype):
        n = int(np.prod(free_shape))
        esz = 4 if dtype in (F32, I32) else 2
        nb = (n * esz + 31) // 32 * 32
        assert self.off + nb <= self.nbytes, (self.off, nb, self.nbytes)
        ap = self.t[:, self.off // 4:(self.off + nb) // 4]
        self.off += nb
        if dtype != F32:
            ap = ap.bitcast(dtype)
        ap = ap[:, 0:n]
        if len(free_shape) == 2:
            ap = ap.rearrange("p (a b) -> p a b", b=free_shape[1])
        elif len(free_shape) == 3:
            ap = ap.rearrange("p (a b c) -> p a b c", b=free_shape[1], c=free_shape[2])
        elif len(free_shape) == 4:
            ap = ap.rearrange("p (a b c d) -> p a b c d", b=free_shape[1], c=free_shape[2], d=free_shape[3])
        return ap


def build_program(last_phase="F", debug=()):
    nc = bass.Bass("TRN2", target_bir_lowering=False)
    T = NTS * 128

    def din(name, shape, dt=F32):
        return nc.dram_tensor(name, list(shape), dt, kind="ExternalInput").ap()

    xT = din("xT", [D, T])
    x_own = din("x_own", [NOWN * 128, D])
    memT = din("memT", [D, 256])
    w_in = din("w_in", [D, 5638])
    w_mem_kv = din("w_mem_kv", [D, 512])
    w_br = din("w_br", [D, D])
    w_out = din("w_out", [D, D])
    w_router = din("w_router", [D, NE])
    if PHASES.index(last_phase) >= PHASES.index("E"):
        w_exp_in = din("w_exp_in", [NE, D, 2 * D])
        w_exp_out = din("w_exp_out", [NE, D, D])
    b_exp_out = din("b_exp_out", [NE, D])
    b1T = din("b1T", [128, NE * 16])
    lnrep = din("lnrep", [4, 128, D])
    cst = din("cst", [128, 6 * 128 + 32 + NTS + 24 + 32])
    dilb = din("dilb", [6, 128, 17, 128])
    padrow = din("padrow", [1, NTS * 128])
    out_d = nc.dram_tensor("out", [NOWN * 128, D], F32, kind="ExternalOutput").ap()
    dbg = {}
    for name, shape in debug:
        dbg[name] = nc.dram_tensor(name, list(shape), F32, kind="ExternalOutput").ap()

    KT_scr = nc.dram_tensor("KT_scr", [6, 128, T], BF16, kind="Internal").ap()
    V_scr = nc.dram_tensor("V_scr", [12, 128, NTS, 65], BF16, kind="Internal").ap()
    X_scr = nc.dram_tensor("X_scr", [NE * CAP, D], BF16, kind="Internal").ap()
    Y_scr = nc.dram_tensor("Y_scr", [NE * CAP, D], F32, kind="Internal").ap()
    Gd = nc.dram_tensor("Gd", [6, 3, NTS * 128], BF16, kind="Internal").ap()

    xTv = xT.rearrange("(c p) t -> p c t", p=128)
    xTown = xT.rearrange("(c p) (m j t) -> p c m j t", p=128, j=8, t=128)
    w_inv = w_in.rearrange("(c p) n -> p c n", p=128)

    li = PHASES.index(last_phase)

    def on(ph):
        return PHASES.index(ph) <= li

    with contextlib.ExitStack() as st:
        S = Sched(nc, st)
        A = Arena(nc, st, 206 * 1024)

        def psum(name, shape, dt=F32):
            return st2.enter_context(nc.psum_tensor(name, list(shape), dt))[:]

        A.seek(0)
        cst_sb = A.alloc([6 * 128 + 32 + NTS + 24 + 32], F32)
        ident_f = cst_sb[:, 0:128]
        tri_incl = cst_sb[:, 128:256]
        tri_strict_f = cst_sb[:, 256:384]
        ones_f = cst_sb[:, 384:512]
        e64 = cst_sb[:, 512:640]
        foxtri = cst_sb[:, 640:768]
        iotaC = cst_sb[:, 768:800]
        padb = cst_sb[:, 800:800 + NTS]
        bf_rep4 = cst_sb[:, 800 + NTS:800 + NTS + 24].rearrange("p (a b) -> p a b", b=6)
        br_rep = cst_sb[:, 800 + NTS + 24:800 + NTS + 56]
        ident_b = A.alloc([128], BF16)
        tri_strict_b = A.alloc([128], BF16)
        ones_b = A.alloc([128], BF16)
        foxtri_b = A.alloc([128], BF16)
        G_all = A.alloc([NTS, 6], F32)
        Gref = A.alloc([NOWN, 6], F32)
        dest_i = A.alloc([NOWN, 4], I32)
        gate4 = A.alloc([NOWN, 4], F32)
        assert A.off <= 12 * 1024, A.off
        OFF_QT = 12 * 1024
        OFF_O = OFF_QT + 44 * 1024
        OFF_LOC = OFF_O + 32 * 1024
        OFF_LOC2 = OFF_O + 64 * 1024
        A.seek(OFF_QT)
        QTf = A.alloc([6, NOWN * 128], BF16)
        QT = A.alloc([5, NOWN * 128], BF16)
        A.seek(OFF_QT)
        mergedT = A.alloc([8, NOWN * 128], BF16)
        A.seek(OFF_O)
        o_fox = A.alloc([NOWN, 384], BF16)
        o_dil = A.alloc([NOWN, 384], BF16)
        o_mem = A.alloc([NOWN, 256], BF16)
        A.seek(OFF_O)
        h1 = A.alloc([NOWN, D], F32)

        S.add("sync", lambda e: e.dma_start(out=cst_sb, in_=cst), writes=["cst"], dma=True)
        S.add("vector", lambda e: e.tensor_copy(out=ident_b, in_=ident_f), reads=["cst"], writes=["ident_b"])
        S.add("vector", lambda e: e.tensor_copy(out=tri_strict_b, in_=tri_strict_f), reads=["cst"], writes=["tsb"])
        S.add("vector", lambda e: e.tensor_copy(out=ones_b, in_=ones_f), reads=["cst"], writes=["ones_b"])
        S.add("vector", lambda e: e.tensor_copy(out=foxtri_b, in_=foxtri), reads=["cst"], writes=["foxtri_b"])
        S.flush()

        def evac(i, out, in_, reads, writes, scale=None):
            if i % 2 == 0:
                if scale is None:
                    S.add("scalar", lambda e: e.copy(out=out, in_=in_), reads, writes)
                else:
                    S.add("scalar", lambda e: e.mul(out=out, in_=in_, mul=scale), reads, writes)
            else:
                if scale is None:
                    S.add("vector", lambda e: e.tensor_copy(out=out, in_=in_), reads, writes)
                else:
                    S.add("vector", lambda e: e.tensor_scalar(out=out, in0=in_, scalar1=scale, scalar2=None, op0=ALU.mult), reads, writes)

        def mm(out, lhsT, rhs, start, stop, reads, writes):
            S.add("tensor", lambda e: e.matmul(out, lhsT, rhs, start=start, stop=stop), reads, writes)

        if on("A"):
            with contextlib.ExitStack() as st2:
                A.seek(OFF_LOC)
                Wk = A.alloc([8, 768], BF16)
                Wv = A.alloc([8, 768], BF16)
                Wf = A.alloc([8, 6], BF16)
                xbuf = [A.alloc([8, 512], BF16) for _ in range(2)]
                ktst = [A.alloc([6, 512], BF16) for _ in range(2)]
                vst = [A.alloc([12, 4, 65], BF16) for _ in range(2)]
                Z_all = A.alloc([NTS, 6], F32)
                SP_all = A.alloc([NTS, 6], F32)
                Wsb = A.alloc([NTS, 6], F32)
                Tsb = A.alloc([NTS, 6], F32)
                HA = A.alloc([NTS, 6], F32)
                HB = A.alloc([NTS, 6], F32)
                zero_t = A.alloc([8, D], BF16)
                Gp = [A.alloc([NTS, 6], BF16) for _ in range(3)]
                R1 = A.alloc([NTS, 6], F32)
                R2 = A.alloc([NTS, 6], F32)
                gst = [A.alloc([128], BF16) for _ in range(2)]
                psGT = psum("psGT", [128, 128], BF16)
                psK = [psum(f"psK{i}", [128, 512]) for i in range(2)]
                psV = [psum(f"psV{i}", [128, 384]) for i in range(2)]
                psF = psum("psF", [128, 4, 6])
                psW = [psum(f"psW{i}", [128, 408]) for i in range(2)]

                S.add("vector", lambda e: e.memset(zero_t, 0.0), writes=["zero_t"])
                Xz = X_scr.rearrange("(a b p) d -> a p b d", b=8, p=128)
                for a in range(NE * CAP // 1024):
                    S.add("sync", lambda e, a=a: e.dma_start(out=Xz[a], in_=zero_t), reads=["zero_t"], writes=[("X_scr", a)], dma=True)

                for (dst, lo) in ((Wk[:, :, 0:384], 384), (Wk[:, :, 384:768], 1542), (Wv[:, :, 0:384], 768), (Wv[:, :, 384:768], 1926)):
                    S.add("gpsimd", lambda e, dst=dst, lo=lo: e.dma_start(out=dst, in_=w_inv[:, :, lo:lo + 384]), writes=["WA"], dma=True)
                S.add("gpsimd", lambda e: e.dma_start(out=Wf, in_=w_inv[:, :, 1152:1158]), writes=["WA"], dma=True)
                for b in range(2):
                    S.add("vector", lambda e, b=b: e.memset(vst[b][:, :, :, 64:65], 1.0), writes=[("vst1", b)])

                KTv = KT_scr.rearrange("r p t -> p r t")
                Vv = V_scr.rearrange("h p t c -> p h t c")
                ev = 0
                for tb in range(NTS // 4):
                    xb = xbuf[tb % 2]
                    rx = ("xb", tb % 2)
                    S.add("gpsimd", lambda e, xb=xb, tb=tb: e.dma_start(out=xb, in_=xTv[:, :, tb * 512:(tb + 1) * 512]), writes=[rx], dma=True)
                    kt = ktst[tb % 2]
                    rk = ("ktst", tb % 2)
                    for pr in range(6):
                        ps = psK[pr % 2]
                        rp = ("psK", pr % 2)
                        for c in range(8):
                            mm(ps, Wk[:, c, pr * 128:(pr + 1) * 128], xb[:, c, :], c == 0, c == 7, [rx, "WA"], [rp])
                        evac(ev, kt[:, pr, :], ps, [rp], [rk]); ev += 1
                    S.add("sync", lambda e, kt=kt, tb=tb: e.dma_start(out=KTv[:, :, tb * 512:(tb + 1) * 512], in_=kt), reads=[rk], writes=[("KT_scr", tb)], dma=True)
                    vs = vst[tb % 2]
                    rv = ("vst", tb % 2)
                    for t4 in range(4):
                        for grp in range(2):
                            ps = psV[grp]
                            rp = ("psV", grp)
                            for c in range(8):
                                mm(ps, xb[:, c, t4 * 128:(t4 + 1) * 128], Wv[:, c, grp * 384:(grp + 1) * 384], c == 0, c == 7, [rx, "WA"], [rp])
                            evac(ev, vs[:, grp * 6:(grp + 1) * 6, t4, 0:64], ps.rearrange("p (h d) -> p h d", d=64), [rp], [rv, ("vst1", tb % 2)]); ev += 1
                    S.add("sync", lambda e, vs=vs, tb=tb: e.dma_start(out=Vv[:, :, tb * 4:(tb + 1) * 4, :], in_=vs), reads=[rv, ("vst1", tb % 2)], writes=[("V_scr", tb)], dma=True)
                    for t4 in range(4):
                        for c in range(8):
                            mm(psF[:, t4, :], xb[:, c, t4 * 128:(t4 + 1) * 128], Wf[:, c, :], c == 0, c == 7, [rx, "WA"], ["psF"])
                    S.add("vector", lambda e, tb=tb: e.tensor_tensor(out=Z_all[:, tb * 4:(tb + 1) * 4, :], in0=psF, in1=bf_rep4, op=ALU.add), reads=["psF", "cst"], writes=["Z_all"])

                Zf = Z_all.rearrange("p a b -> p (a b)")
                SPf = SP_all.rearrange("p a b -> p (a b)")
                S.add("scalar", lambda e: e.activation(out=SPf, in_=Zf, func=AF.Exp, scale=-1.0), reads=["Z_all"], writes=["SP"])
                S.add("scalar", lambda e: e.activation(out=SPf, in_=SPf, func=AF.Ln, bias=1.0, scale=1.0), reads=["SP"], writes=["SP"])
                Wf2 = Wsb.rearrange("p a b -> p (a b)")
                Tf2 = Tsb.rearrange("p a b -> p (a b)")
                for hf in range(2):
                    mm(psW[hf], tri_incl, SPf[:, hf * 408:(hf + 1) * 408], True, True, ["SP", "cst"], [("psW", hf)])
                    S.add("vector", lambda e, hf=hf: e.tensor_copy(out=Wf2[:, hf * 408:(hf + 1) * 408], in_=psW[hf]), reads=[("psW", hf)], writes=["Wsb"])
                for hf in range(2):
                    mm(psW[hf], ones_f, SPf[:, hf * 408:(hf + 1) * 408], True, True, ["SP", "cst"], [("psW", hf)])
                    S.add("vector", lambda e, hf=hf: e.tensor_copy(out=Tf2[:, hf * 408:(hf + 1) * 408], in_=psW[hf]), reads=[("psW", hf)], writes=["Tsb"])
                S.add("vector", lambda e: e.tensor_copy(out=HA, in_=Tsb), reads=["Tsb"], writes=["HA"])
                cur, nxt, rc, rn = HA, HB, "HA", "HB"
                sh = 1
                while sh < NTS:
                    S.add("vector", lambda e, cur=cur, nxt=nxt, sh=sh: e.tensor_copy(out=nxt[:, 0:sh, :], in_=cur[:, 0:sh, :]), reads=[rc], writes=[rn])
                    S.add("vector", lambda e, cur=cur, nxt=nxt, sh=sh: e.tensor_tensor(out=nxt[:, sh:NTS, :], in0=cur[:, sh:NTS, :], in1=cur[:, 0:NTS - sh, :], op=ALU.add), reads=[rc], writes=[rn])
                    cur, nxt, rc, rn = nxt, cur, rn, rc
                    sh *= 2
                S.add("vector", lambda e, cur=cur: e.tensor_tensor(out=G_all, in0=cur, in1=Tsb, op=ALU.subtract), reads=[rc, "Tsb"], writes=["G_all"])
                S.add("vector", lambda e: e.tensor_tensor(out=G_all, in0=G_all, in1=Wsb, op=ALU.add), reads=["G_all", "Wsb"], writes=["G_all"])
                Gown = G_all.rearrange("p (m j) h -> p m j h", j=8)[:, 0:NOWN, 7, :]
                S.add("vector", lambda e: e.tensor_copy(out=HA[:, 0:NOWN, :], in_=Gown), reads=["G_all"], writes=["HA", "HB"])
                mm(psW[0][:, 0:NOWN * 6], e64, HA[:, 0:NOWN, :].rearrange("p a b -> p (a b)"), True, True, ["HA", "cst"], [("psW", 0)])
                S.add("vector", lambda e: e.tensor_copy(out=Gref.rearrange("p a b -> p (a b)"), in_=psW[0][:, 0:NOWN * 6]), reads=[("psW", 0)], writes=["Gref"])
                S.add("vector", lambda e: e.tensor_copy(out=Gp[0], in_=G_all), reads=["G_all"], writes=["Gp0"])
                S.add("vector", lambda e: e.tensor_tensor(out=R1, in0=G_all, in1=Gp[0], op=ALU.subtract), reads=["G_all", "Gp0"], writes=["R1"])
                S.add("vector", lambda e: e.tensor_copy(out=Gp[1], in_=R1), reads=["R1"], writes=["Gp1"])
                S.add("vector", lambda e: e.tensor_tensor(out=R2, in0=R1, in1=Gp[1], op=ALU.subtract), reads=["R1", "Gp1"], writes=["R2"])
                S.add("vector", lambda e: e.tensor_copy(out=Gp[2], in_=R2), reads=["R2"], writes=["Gp2"])
                Gdv = Gd.rearrange("h r (s p) -> h r s p", p=128)
                kk = 0
                for h in range(6):
                    for part in range(3):
                        for (s0, ns) in ((0, 128), (128, NTS - 128)):
                            stg = gst[kk % 2]
                            rst = ("gst", kk % 2)
                            S.add("tensor", lambda e, h=h, part=part, s0=s0, ns=ns: e.transpose(psGT[0:ns, :], Gp[part][:, s0:s0 + ns, h], ident_b), reads=[f"Gp{part}", "ident_b"], writes=["psGT"])
                            evac(kk, stg[0:ns, :], psGT[0:ns, :], ["psGT"], [rst])
                            S.add("sync", lambda e, h=h, part=part, s0=s0, ns=ns, stg=stg: e.dma_start(out=Gdv[h, part, s0:s0 + ns, :], in_=stg[0:ns, :]), reads=[rst], writes=[("Gd", h, part, s0)], dma=True)
                            kk += 1
                if "G_all" in dbg:
                    S.add("sync", lambda e: e.dma_start(out=dbg["G_all"], in_=G_all.rearrange("p a b -> p (a b)")), reads=["G_all"], writes=["dbgG"], dma=True)
                S.flush()

        if on("Q"):
            with contextlib.ExitStack() as st2:
                A.seek(OFF_LOC)
                Wq = A.alloc([8, 1024], BF16)
                xo = A.alloc([8, NOWN, 128], BF16)
                psQ = [psum(f"psQ{i}", [128, 512]) for i in range(2)]
                for (lo_dst, lo, n) in ((0, 0, 384), (384, 1158, 384), (768, 2310, 256)):
                    S.add("gpsimd", lambda e, lo_dst=lo_dst, lo=lo, n=n: e.dma_start(out=Wq[:, :, lo_dst:lo_dst + n], in_=w_inv[:, :, lo:lo + n]), writes=["Wq"], dma=True)
                for c in range(8):
                    S.add("gpsimd", lambda e, c=c: e.dma_start(out=xo[:, c, :, :], in_=xTown[:, c, 0:NOWN, 7, :]), writes=["xo"], dma=True)
                xof = xo.rearrange("p c m t -> p c (m t)")
                k = 0
                for b4 in range(4):
                    for h in range(6):
                        ps = psQ[k % 2]
                        for c in range(8):
                            mm(ps[0:64, :], Wq[:, c, h * 64:(h + 1) * 64], xof[:, c, b4 * 512:(b4 + 1) * 512], c == 0, c == 7, ["Wq", "xo"], [("psQ", k % 2)])
                        evac(k, QTf[0:64, h, b4 * 512:(b4 + 1) * 512], ps[0:64, :], [("psQ", k % 2)], ["QTf"], scale=0.125)
                        k += 1
                    for pr in range(5):
                        ps = psQ[k % 2]
                        for c in range(8):
                            mm(ps, Wq[:, c, 384 + pr * 128:384 + (pr + 1) * 128], xof[:, c, b4 * 512:(b4 + 1) * 512], c == 0, c == 7, ["Wq", "xo"], [("psQ", k % 2)])
                        evac(k, QT[:, pr, b4 * 512:(b4 + 1) * 512], ps, [("psQ", k % 2)], ["QT"], scale=0.125)
                        k += 1
                S.add("vector", lambda e: e.memset(QTf[64:69, :, :], 1.0), writes=["QTfa"])
                for h in range(6):
                    for m in range(NOWN):
                        S.add("vector", lambda e, h=h, m=m: e.tensor_scalar(out=QTf[64:65, h, m * 128:(m + 1) * 128], in0=ones_f[64:65, :], scalar1=Gref[64:65, m, h:h + 1], scalar2=-1.0,
                                                                    op0=ALU.mult, op1=ALU.mult), reads=["Gref", "cst", "QTfa"], writes=["QTfa"])
                S.flush()

        def load_kv(KTb, Vb, pr, hp, vh, par, ntiles):
            rk, rv = ("KTb", par), ("Vb", par)
            q4 = (ntiles * 128) // 4
            for q in range(4):
                S.add("sync", lambda e, q=q: e.dma_start(out=KTb[hp:hp + 64, q * q4:(q + 1) * q4], in_=KT_scr[pr, hp:hp + 64, q * q4:(q + 1) * q4]), writes=[rk], dma=True)
            S.add("sync", lambda e: e.dma_start(out=Vb[:, 0:ntiles, :], in_=V_scr[vh, :, 0:ntiles, :]), writes=[rv], dma=True)
            return rk, rv

        class AttnPipe:
            LA = 2

            def __init__(self, psS, Pt, ssb, psO):
                self.psS, self.Pt, self.ssb, self.psO = psS, Pt, ssb, psO
                self.cnt = 0
                self.acc_i = 0
                self.pending = []

            def _drain(self, keep):
                while len(self.pending) > keep:
                    self.pending.pop(0)()

            def run(self, items, finish):
                acc = self.psO[self.acc_i % 2]
                racc = ("psO", self.acc_i % 2)
                self.acc_i += 1
                n = len(items)
                for idx, it in enumerate(items):
                    slot = self.cnt % 8
                    sslot = self.cnt % 4
                    self.cnt += 1
                    ps = self.psS[sslot]
                    rps = ("psS", sslot)
                    P = self.Pt[:, slot, :]
                    rP = ("P", slot)
                    mm(ps, it["lhsT"], it["rhs"], True, True, it["reads"], [rps])
                    src, rsrc = ps, rps
                    if it["mask"] is not None:
                        sb = self.ssb[:, slot % 4, :]
                        rsb = ("ssb", slot % 4)
                        S.add("vector", lambda e, sb=sb, ps=ps, m=it["mask"]: e.tensor_tensor(out=sb, in0=ps, in1=m, op=ALU.add), reads=[rps] + it["mreads"], writes=[rsb])
                        src, rsrc = sb, rsb
                    if it["bias"] is not None:
                        S.add("scalar", lambda e, P=P, src=src, b=it["bias"]: e.activation(out=P, in_=src, func=AF.Exp, bias=b, scale=1.0), reads=[rsrc] + it["breads"], writes=[rP])
                    else:
                        S.add("scalar", lambda e, P=P, src=src: e.activation(out=P, in_=src, func=AF.Exp), reads=[rsrc], writes=[rP])

                    def pv(P=P, rP=rP, it=it, first=(idx == 0), last=(idx == n - 1), acc=acc, racc=racc):
                        mm(acc, P, it["v"], first, last, [rP] + it["vreads"], [racc])
                        if last:
                            finish(acc, racc)
                    self.pending.append(pv)
                    self._drain(self.LA)

            def flush(self):
                self._drain(0)

        if on("B"):
            with contextlib.ExitStack() as st2:
                A.seek(OFF_LOC)
                KTb = [A.alloc([NTS * 128], BF16) for _ in range(2)]
                Vb = [A.alloc([NTS, 65], BF16) for _ in range(2)]
                NPB = 4
                Pt = [A.alloc([512], BF16) for _ in range(NPB)]
                rec = A.alloc([8], F32)
                psS = [psum(f"psS{i}", [128, 512]) for i in range(NPB)]
                psO = [psum(f"psO{i}", [128, 65]) for i in range(4)]
                fin_i = [0]
                if "o_fox" in dbg:
                    S.add("vector", lambda e: e.memset(o_fox, 0.0), writes=["o_attn"])
                pending = []
                gcnt = [0]
                acc_i = [0]

                def drain(keep):
                    while len(pending) > keep:
                        pending.pop(0)()

                def finish(acc, racc, dst):
                    r = rec[:, fin_i[0] % 8:fin_i[0] % 8 + 1]
                    rr = ("rec", fin_i[0] % 8)
                    fin_i[0] += 1
                    S.add("vector", lambda e: e.reciprocal(out=r, in_=acc[:, 64:65]), reads=[racc], writes=[rr])
                    S.add("vector", lambda e: e.tensor_scalar(out=dst, in0=acc[:, 0:64], scalar1=r, scalar2=None, op0=ALU.mult), reads=[racc, rr], writes=["o_attn"])

                for h in range(6):
                    par = h % 2
                    hp = (h % 2) * 64
                    pr = h // 2
                    KA = KTb[par]
                    rk, rv = ("KTb", par), ("Vb", par)
                    q4 = (NTS * 128) // 4
                    for q in range(4):
                        S.add("sync", lambda e, q=q, KA=KA, pr=pr, hp=hp: e.dma_start(out=KA[0:64, q * q4:(q + 1) * q4], in_=KT_scr[pr, hp:hp + 64, q * q4:(q + 1) * q4]), writes=[rk], dma=True)
                    S.add("vector", lambda e, KA=KA: e.memset(KA[64:65, :], 1.0), writes=[rk])
                    S.add("sync", lambda e, KA=KA, h=h: e.dma_start(out=KA[65:68, :], in_=Gd[h]), writes=[rk], dma=True)
                    S.add("gpsimd", lambda e, KA=KA: e.dma_start(out=KA[68:69, :], in_=padrow), writes=[rk], dma=True)
                    S.add("sync", lambda e, par=par, h=h: e.dma_start(out=Vb[par], in_=V_scr[h]), writes=[rv], dma=True)
                    for mp_ in range(NOWN // 2):
                        m0, m1 = 2 * mp_, 2 * mp_ + 1
                        J0, J1 = 8 * m0 + 8, 8 * m1 + 8
                        acc0 = psO[acc_i[0] % 4]; racc0 = ("psO", acc_i[0] % 4); acc_i[0] += 1
                        acc1 = psO[acc_i[0] % 4]; racc1 = ("psO", acc_i[0] % 4); acc_i[0] += 1
                        dst0 = o_fox[:, m0, h * 64:(h + 1) * 64]
                        dst1 = o_fox[:, m1, h * 64:(h + 1) * 64]
                        qrd = [rk, "QTf", "QTfa"]
                        for g in range(J0 // 2):
                            b = gcnt[0] % NPB
                            gcnt[0] += 1
                            ps = psS[b].rearrange("p (a q) -> p a q", q=256)
                            rps = ("psS", b)
                            P = Pt[b].rearrange("p (a q) -> p a q", q=256)
                            rP = ("P", b)
                            for jj in range(2):
                                j = 2 * g + jj
                                kt = KA[0:69, j * 128:(j + 1) * 128]
                                if j == J0 - 1:
                                    mm(ps[:, jj, 0:128], kt, QTf[0:69, h, m0 * 128:(m0 + 1) * 128], True, False, qrd, [rps])
                                    mm(ps[:, jj, 0:128], ident_b, foxtri_b, False, True, ["ident_b", "foxtri_b"], [rps])
                                    mm(ps[:, jj, 128:256], kt, QTf[0:69, h, m1 * 128:(m1 + 1) * 128], True, True, qrd, [rps])
                                else:
                                    mm(ps[:, jj, :], kt, QTf[0:69, h, m0 * 128:(m0 + 2) * 128], True, True, qrd, [rps])
                            S.add("scalar", lambda e, b=b: e.activation(out=Pt[b], in_=psS[b], func=AF.Exp), reads=[rps], writes=[rP])

                            def pv(g=g, P=P, rP=rP, acc0=acc0, racc0=racc0, acc1=acc1, racc1=racc1, J0=J0, par=par, rv=rv, dst0=dst0):
                                for jj in range(2):
                                    j = 2 * g + jj
                                    mm(acc0, P[:, jj, 0:128], Vb[par][:, j, :], j == 0, j == J0 - 1, [rP, rv], [racc0])
                                    mm(acc1, P[:, jj, 128:256], Vb[par][:, j, :], j == 0, False, [rP, rv], [racc1])
                                if 2 * g + 1 == J0 - 1:
                                    finish(acc0, racc0, dst0)
                            pending.append(pv)
                            drain(3)
                        for g in range(2):
                            b = gcnt[0] % NPB
                            gcnt[0] += 1
                            ps = psS[b].rearrange("p (a q) -> p a q", q=128)
                            rps = ("psS", b)
                            P = Pt[b].rearrange("p (a q) -> p a q", q=128)
                            rP = ("P", b)
                            for jj in range(4):
                                j = J0 + 4 * g + jj
                                diag = (j == J1 - 1)
                                mm(ps[:, jj, :], KA[0:69, j * 128:(j + 1) * 128], QTf[0:69, h, m1 * 128:(m1 + 1) * 128], True, not diag, qrd, [rps])
                                if diag:
                                    mm(ps[:, jj, :], ident_b, foxtri_b, False, True, ["ident_b", "foxtri_b"], [rps])
                            S.add("scalar", lambda e, b=b: e.activation(out=Pt[b], in_=psS[b], func=AF.Exp), reads=[rps], writes=[rP])

                            def pv2(g=g, P=P, rP=rP, acc1=acc1, racc1=racc1, J0=J0, J1=J1, par=par, rv=rv, dst1=dst1):
                                for jj in range(4):
                                    j = J0 + 4 * g + jj
                                    mm(acc1, P[:, jj, :], Vb[par][:, j, :], False, j == J1 - 1, [rP, rv], [racc1])
                                if g == 1:
                                    finish(acc1, racc1, dst1)
                            pending.append(pv2)
                            drain(3)
                drain(0)
                if "o_fox" in dbg:
                    A.seek(OFF_LOC)
                    tmp = A.alloc([NOWN, 384], F32)
                    S.add("vector", lambda e: e.tensor_copy(out=tmp, in_=o_fox), reads=["o_attn", ("KTb", 0), ("KTb", 1)], writes=[("KTb", 0)])
                    S.add("sync", lambda e: e.dma_start(out=dbg["o_fox"].rearrange("(m p) d -> p m d", p=128), in_=tmp), reads=[("KTb", 0)], writes=["dbgo"], dma=True)
                S.flush()

        if on("C"):
            with contextlib.ExitStack() as st2:
                A.seek(OFF_LOC)
                KTb = [A.alloc([NTS * 128], BF16)]
                Vb = [A.alloc([NTS, 65], BF16) for _ in range(2)]
                dbt = [A.alloc([17, 128], F32) for _ in range(2)]
                loc_save = A.off
                A.seek(OFF_QT)
                num = A.alloc([NOWN, 384], F32)
                A.seek(loc_save)
                den = A.alloc([NOWN, 6], F32)
                dsum = A.alloc([NOWN, 2], F32)
                NPB = 4
                Pt = [A.alloc([4, 128], BF16) for _ in range(NPB)]
                ssb = [A.alloc([4, 128], F32) for _ in range(NPB)]
                psS = [psum(f"psSc{i}", [128, 4, 128]) for i in range(NPB)]
                psO = [psum(f"psOc{i}", [128, 65]) for i in range(2)]
                pending = []
                gcnt = [0]
                acc_i = [0]

                def drain_c(keep):
                    while len(pending) > keep:
                        pending.pop(0)()

                def finish_c(acc, racc, m, hd):
                    S.add("vector", lambda e: e.tensor_copy(out=num[:, m, hd * 64:(hd + 1) * 64], in_=acc[:, 0:64]), reads=[racc], writes=["num"])
                    S.add("vector", lambda e: e.tensor_copy(out=den[:, m, hd:hd + 1], in_=acc[:, 64:65]), reads=[racc], writes=["den"])

                def load_v_c(hd_):
                    S.add("sync", lambda e, hd_=hd_: e.dma_start(out=Vb[hd_ % 2], in_=V_scr[6 + hd_]), writes=[("Vbc", hd_ % 2)], dma=True)
                    S.add("sync", lambda e, hd_=hd_: e.dma_start(out=dbt[hd_ % 2], in_=dilb[hd_]), writes=[("dbt", hd_ % 2)], dma=True)

                load_v_c(0)
                for hd in range(6):
                    par = 0
                    dpar = hd % 2
                    hp = (hd % 2) * 64
                    pr = 3 + hd // 2
                    prq = hd // 2
                    rk = ("KTb", 0)
                    q4 = (NTS * 128) // 4
                    for q in range(4):
                        S.add("sync", lambda e, q=q, pr=pr, hp=hp: e.dma_start(out=KTb[0][hp:hp + 64, q * q4:(q + 1) * q4], in_=KT_scr[pr, hp:hp + 64, q * q4:(q + 1) * q4]), writes=[rk], dma=True)
                    if hd + 1 < 6:
                        load_v_c(hd + 1)
                    rv = ("Vbc", dpar)
                    rd = ("dbt", dpar)
                    for m in range(NOWN):
                        sm = 8 * m + 7
                        dls = [dl for dl in range(DMAX[hd // 2] + 1) if sm - dl >= 0]
                        batches = [dls[i:i + 4] for i in range(0, len(dls), 4)]
                        acc = psO[acc_i[0] % 2]
                        racc = ("psO", acc_i[0] % 2)
                        acc_i[0] += 1
                        nbt = len(batches)
                        for bi, bt in enumerate(batches):
                            b = gcnt[0] % NPB
                            gcnt[0] += 1
                            ps, sb, P = psS[b], ssb[b], Pt[b]
                            rps, rsb, rP = ("psS", b), ("ssb", b), ("P", b)
                            nb = len(bt)
                            for t, dl in enumerate(bt):
                                j = sm - dl
                                mm(ps[:, t, :], KTb[par][hp:hp + 64, j * 128:(j + 1) * 128], QT[hp:hp + 64, prq, m * 128:(m + 1) * 128], True, True, [rk, "QT"], [rps])
                            S.add("vector", lambda e, sb=sb, ps=ps, nb=nb, d0=bt[0], dpar=dpar: e.tensor_tensor(out=sb[:, 0:nb, :], in0=ps[:, 0:nb, :], in1=dbt[dpar][:, d0:d0 + nb, :], op=ALU.add),
                                  reads=[rps, rd], writes=[rsb])
                            for t, dl in enumerate(bt):
                                j = sm - dl
                                if j <= 6:
                                    S.add("vector", lambda e, sb=sb, t=t, j=j: e.tensor_scalar(out=sb[:, t, :], in0=sb[:, t, :], scalar1=padb[:, j:j + 1], scalar2=None, op0=ALU.add), reads=[rsb, "cst"], writes=[rsb])
                            S.add("scalar", lambda e, P=P, sb=sb, nb=nb: e.activation(out=P[:, 0:nb, :], in_=sb[:, 0:nb, :], func=AF.Exp), reads=[rsb], writes=[rP])

                            def pv(bt=bt, P=P, rP=rP, acc=acc, racc=racc, bi=bi, nbt=nbt, sm=sm, par=par, dpar=dpar, rv=rv, m=m, hd=hd):
                                for t, dl in enumerate(bt):
                                    j = sm - dl
                                    mm(acc, P[:, t, :], Vb[dpar][:, j, :], (bi == 0 and t == 0), (bi == nbt - 1 and t == len(bt) - 1), [rP, rv], [racc])
                                if bi == nbt - 1:
                                    finish_c(acc, racc, m, hd)
                            pending.append(pv)
                            drain_c(2)
                drain_c(0)
                S.add("vector", lambda e: e.tensor_tensor(out=dsum, in0=den[:, :, 0:2], in1=den[:, :, 2:4], op=ALU.add), reads=["den"], writes=["dsum"])
                S.add("vector", lambda e: e.tensor_tensor(out=dsum, in0=dsum, in1=den[:, :, 4:6], op=ALU.add), reads=["den", "dsum"], writes=["dsum"])
                S.add("vector", lambda e: e.reciprocal(out=dsum, in_=dsum), reads=["dsum"], writes=["dsum"])
                for m in range(NOWN):
                    for hd in range(6):
                        S.add("vector", lambda e, m=m, hd=hd: e.tensor_scalar(out=o_dil[:, m, hd * 64:(hd + 1) * 64], in0=num[:, m, hd * 64:(hd + 1) * 64],
                                                                      scalar1=dsum[:, m, hd % 2:hd % 2 + 1], scalar2=None, op0=ALU.mult), reads=["num", "dsum"], writes=["o_attn"])
                S.flush()

        if on("Dm"):
            with contextlib.ExitStack() as st2:
                A.seek(OFF_LOC)
                Wkv = A.alloc([8, 512], BF16)
                mT = A.alloc([8, 256], BF16)
                KmT = A.alloc([2, 256], BF16)
                Vm = A.alloc([2, 4, 65], BF16)
                Pt = A.alloc([8, 128], BF16)
                ssb = A.alloc([4, 128], F32)
                rec = A.alloc([8], F32)
                psS = [psum(f"psSm{i}", [128, 128]) for i in range(4)]
                psO = [psum(f"psOm{i}", [128, 65]) for i in range(2)]
                psM = [psum(f"psM{i}", [128, 256]) for i in range(2)]
                S.add("gpsimd", lambda e: e.dma_start(out=Wkv, in_=w_mem_kv.rearrange("(c p) n -> p c n", p=128)), writes=["Wkv"], dma=True)
                S.add("gpsimd", lambda e: e.dma_start(out=mT, in_=memT.rearrange("(c p) n -> p c n", p=128)), writes=["mT"], dma=True)
                assert A.off <= OFF_LOC + 54 * 1024
                A.seek(OFF_LOC + 54 * 1024)
                Wg = A.alloc([8, 3072], BF16)
                Wbr = A.alloc([8, D], BF16)
                for b in range(3):
                    S.add("gpsimd", lambda e, b=b: e.dma_start(out=Wg[:, :, b * 1024:(b + 1) * 1024], in_=w_inv[:, :, 2566 + b * 1024:2566 + (b + 1) * 1024]), writes=["Wg"], dma=True)
                S.add("gpsimd", lambda e: e.dma_start(out=Wbr, in_=w_br.rearrange("(c p) n -> p c n", p=128)), writes=["Wbr"], dma=True)
                S.add("vector", lambda e: e.memset(Vm[:, :, :, 64:65], 1.0), writes=["Vm1"])
                for pr in range(2):
                    for c in range(8):
                        mm(psM[pr], Wkv[:, c, pr * 128:(pr + 1) * 128], mT[:, c, :], c == 0, c == 7, ["Wkv", "mT"], [("psM", pr)])
                    evac(pr, KmT[:, pr, :], psM[pr], [("psM", pr)], ["KmT"])
                for t2 in range(2):
                    for c in range(8):
                        mm(psM[t2], mT[:, c, t2 * 128:(t2 + 1) * 128], Wkv[:, c, 256:512], c == 0, c == 7, ["Wkv", "mT"], [("psM", t2)])
                    evac(t2, Vm[:, t2, :, 0:64], psM[t2].rearrange("p (h d) -> p h d", d=64), [("psM", t2)], ["Vm", "Vm1"])
                pipe = AttnPipe(psS, Pt, ssb, psO)
                fin_i = [0]

                def make_finish_m(dst):
                    def finish(acc, racc):
                        r = rec[:, fin_i[0] % 8:fin_i[0] % 8 + 1]
                        rr = ("rec", fin_i[0] % 8)
                        fin_i[0] += 1
                        S.add("vector", lambda e: e.reciprocal(out=r, in_=acc[:, 64:65]), reads=[racc], writes=[rr])
                        S.add("vector", lambda e: e.tensor_scalar(out=dst, in0=acc[:, 0:64], scalar1=r, scalar2=None, op0=ALU.mult), reads=[racc, rr], writes=["o_attn"])
                    return finish

                for hm in range(4):
                    hp = (hm % 2) * 64
                    pr = hm // 2
                    for m in range(NOWN):
                        items = []
                        for t2 in range(2):
                            items.append(dict(
                                lhsT=KmT[hp:hp + 64, pr, t2 * 128:(t2 + 1) * 128], rhs=QT[hp:hp + 64, 3 + pr, m * 128:(m + 1) * 128],
                                reads=["KmT", "QT"], mask=None, mreads=[], bias=None, breads=[], v=Vm[:, t2, hm, :], vreads=["Vm", "Vm1"]))
                        pipe.run(items, make_finish_m(o_mem[:, m, hm * 64:(hm + 1) * 64]))
                pipe.flush()
                S.flush()

        if on("M1"):
            with contextlib.ExitStack() as st2:
                A.seek(OFF_LOC + 54 * 1024)
                Wg = A.alloc([8, 3072], BF16)
                Wbr = A.alloc([8, D], BF16)
                A.seek(OFF_LOC2 + 4 * 1024)
                Wout = A.alloc([8, D], BF16)
                S.add("gpsimd", lambda e: e.dma_start(out=Wout, in_=w_out.rearrange("(c p) n -> p c n", p=128)), writes=["Wout"], dma=True)
                A.seek(OFF_LOC)
                xbb = [A.alloc([8, 4, 128], BF16) for _ in range(2)]
                oT = A.alloc([8, 512], BF16)
                sg = [A.alloc([512], F32) for _ in range(3)]
                t1 = A.alloc([512], F32)
                t2b = A.alloc([512], F32)
                assert A.off <= OFF_LOC2 + 4 * 1024, A.off
                psT = psum("psT", [128, 512], BF16)
                psB = [psum(f"psB{i}", [128, 512]) for i in range(3)]
                psG = [psum(f"psG{i}", [128, 512]) for i in range(2)]
                ev = 0
                gk = 0
                for b4 in range(4):
                    xb = xbb[b4 % 2]
                    rx = ("xbb", b4 % 2)
                    for c in range(8):
                        S.add("gpsimd", lambda e, xb=xb, b4=b4, c=c: e.dma_start(out=xb[:, c, :, :], in_=xTown[:, c, 4 * b4:4 * b4 + 4, 7, :]), writes=[rx], dma=True)
                    xbf = xb.rearrange("p c m t -> p c (m t)")
                    for ch in range(8):
                        for mm_ in range(4):
                            m = 4 * b4 + mm_
                            if ch < 3:
                                src = o_fox[:, m, ch * 128:(ch + 1) * 128]
                            elif ch < 6:
                                src = o_dil[:, m, (ch - 3) * 128:(ch - 2) * 128]
                            else:
                                src = o_mem[:, m, (ch - 6) * 128:(ch - 5) * 128]
                            S.add("tensor", lambda e, src=src, mm_=mm_: e.transpose(psT[:, mm_ * 128:(mm_ + 1) * 128], src, ident_b), reads=["o_attn", "ident_b"], writes=["psT"])
                        evac(ev, oT[:, ch, :], psT, ["psT"], ["oT"]); ev += 1
                    for dc in range(8):
                        for bi, chs in enumerate(((0, 1, 2), (3, 4, 5), (6, 7))):
                            for ci, ch in enumerate(chs):
                                mm(psB[bi], Wbr[:, ch, dc * 128:(dc + 1) * 128], oT[:, ch, :], ci == 0, ci == len(chs) - 1, ["Wbr", "oT"], [("psB", bi)])
                        for b in range(3):
                            pg = psG[gk % 2]
                            rg = ("psG", gk % 2)
                            gk += 1
                            for c in range(8):
                                mm(pg, Wg[:, c, b * 1024 + dc * 128:b * 1024 + (dc + 1) * 128], xbf[:, c, :], c == 0, c == 7, ["Wg", rx], [rg])
                            S.add("scalar", lambda e, b=b, pg=pg: e.activation(out=sg[b], in_=pg, func=AF.Sigmoid), reads=[rg], writes=[("sg", b)])
                        S.add("vector", lambda e: e.tensor_tensor(out=t1, in0=psB[0], in1=sg[0], op=ALU.mult), reads=[("psB", 0), ("sg", 0)], writes=["t1"])
                        S.add("vector", lambda e: e.tensor_tensor(out=t2b, in0=psB[1], in1=sg[1], op=ALU.mult), reads=[("psB", 1), ("sg", 1)], writes=["t2b"])
                        S.add("vector", lambda e: e.tensor_tensor(out=t1, in0=t1, in1=t2b, op=ALU.add), reads=["t1", "t2b"], writes=["t1"])
                        S.add("vector", lambda e: e.tensor_tensor(out=t2b, in0=psB[2], in1=sg[2], op=ALU.mult), reads=[("psB", 2), ("sg", 2)], writes=["t2b"])
                        S.add("vector", lambda e, dc=dc, b4=b4: e.tensor_tensor(out=mergedT[:, dc, b4 * 512:(b4 + 1) * 512], in0=t1, in1=t2b, op=ALU.add), reads=["t1", "t2b"], writes=["mergedT"])
                S.flush()

        def layer_norm(z, rz, dst, rdst, g_rep, b_rep, stats, mv, rstd):
            zz = z.rearrange("p (a b) -> p a b", b=512)
            for a in range(2):
                S.add("vector", lambda e, a=a: e.bn_stats(out=stats[:, a, :], in_=zz[:, a, :]), reads=[rz], writes=["stats"])
            S.add("vector", lambda e: e.bn_aggr(out=mv, in_=stats.rearrange("p a b -> p (a b)")), reads=["stats"], writes=["mv"])
            S.add("vector", lambda e: e.tensor_scalar(out=rstd, in0=mv[:, 1:2], scalar1=1e-5, scalar2=None, op0=ALU.add), reads=["mv"], writes=["rstd"])
            S.add("scalar", lambda e: e.activation(out=rstd, in_=rstd, func=AF.Sqrt), reads=["rstd"], writes=["rstd"])
            S.add("vector", lambda e: e.reciprocal(out=rstd, in_=rstd), reads=["rstd"], writes=["rstd"])
            S.add("vector", lambda e: e.tensor_scalar(out=z, in0=z, scalar1=mv[:, 0:1], scalar2=rstd, op0=ALU.subtract, op1=ALU.mult), reads=[rz, "mv", "rstd"], writes=[rz])
            S.add("gpsimd", lambda e: e.tensor_tensor(out=z, in0=z, in1=g_rep, op=ALU.mult), reads=[rz, "lnp"], writes=[rz])
            S.add("vector", lambda e: e.tensor_tensor(out=dst, in0=z, in1=b_rep, op=ALU.add), reads=[rz, "lnp"], writes=[rdst])

        if on("M2"):
            with contextlib.ExitStack() as st2:
                A.seek(OFF_LOC2 + 4 * 1024)
                Wout = A.alloc([8, D], BF16)
                g_rep = A.alloc([D], F32)
                b_rep = A.alloc([D], F32)
                xown = [A.alloc([D], F32) for _ in range(2)]
                zb = [A.alloc([D], F32) for _ in range(2)]
                stats = A.alloc([2, 6], F32)
                mv = A.alloc([2], F32)
                rstd = A.alloc([1], F32)
                psY = [psum(f"psY{i}", [128, 512]) for i in range(4)]
                S.add("sync", lambda e: e.dma_start(out=g_rep, in_=lnrep[0]), writes=["lnp"], dma=True)
                S.add("sync", lambda e: e.dma_start(out=b_rep, in_=lnrep[1]), writes=["lnp"], dma=True)
                for m in range(NOWN):
                    xo_ = xown[m % 2]
                    rxo = ("xown", m % 2)
                    z = zb[m % 2]
                    rz = ("z", m % 2)
                    S.add("sync", lambda e, xo_=xo_, m=m: e.dma_start(out=xo_, in_=x_own[m * 128:(m + 1) * 128, :]), writes=[rxo], dma=True)
                    for eh in range(2):
                        py = psY[(2 * m + eh) % 4]
                        rpy = ("psY", (2 * m + eh) % 4)
                        for dc in range(8):
                            mm(py, mergedT[:, dc, m * 128:(m + 1) * 128], Wout[:, dc, eh * 512:(eh + 1) * 512], dc == 0, dc == 7, ["mergedT", "Wout"], [rpy])
                        S.add("vector", lambda e, z=z, xo_=xo_, py=py, eh=eh: e.scalar_tensor_tensor(
                            out=z[:, eh * 512:(eh + 1) * 512], in0=xo_[:, eh * 512:(eh + 1) * 512], scalar=ALPHA, in1=py, op0=ALU.mult, op1=ALU.add),
                            reads=[rxo, rpy], writes=[rz])
                    layer_norm(z, rz, h1[:, m, :], "h1", g_rep, b_rep, stats, mv, rstd)
                if "h1" in dbg:
                    S.add("sync", lambda e: e.dma_start(out=dbg["h1"].rearrange("(m p) d -> p m d", p=128), in_=h1), reads=["h1"], writes=["dbgh"], dma=True)
                S.flush()

        if on("R"):
            with contextlib.ExitStack() as st2:
                A.seek(OFF_LOC2)
                Wr = A.alloc([8, NE], F32)
                hT = [A.alloc([8, 128], F32) for _ in range(2)]
                hbf = [A.alloc([D], BF16) for _ in range(2)]
                lg2 = [A.alloc([NE], F32) for _ in range(2)]
                mx82 = [A.alloc([8], F32) for _ in range(2)]
                negm2 = [A.alloc([1], F32) for _ in range(2)]
                ex42 = [A.alloc([4], F32) for _ in range(2)]
                esum2 = [A.alloc([1], F32) for _ in range(2)]
                maskall = A.alloc([NOWN, NE], BF16)
                destf2 = [A.alloc([NE], F32) for _ in range(2)]
                selk2 = [A.alloc([NE], F32) for _ in range(2)]
                junk2 = [A.alloc([NE], F32) for _ in range(2)]
                dk2 = [A.alloc([4], F32) for _ in range(2)]
                psTr = [psum(f"psTr{i}", [128, 4, 128]) for i in range(2)]
                psL2 = [psum(f"psL{i}", [128, NE]) for i in range(2)]
                psP2 = [psum(f"psP{i}", [128, NE]) for i in range(2)]
                S.add("sync", lambda e: e.dma_start(out=Wr, in_=w_router.rearrange("(c p) n -> p c n", p=128)), writes=["Wr"], dma=True)
                for m in range(NOWN):
                    p_ = m % 2
                    lg, mx8, negm, ex4, esum = lg2[p_], mx82[p_], negm2[p_], ex42[p_], esum2[p_]
                    destf, selk, junk, dk, psL, psP = destf2[p_], selk2[p_], junk2[p_], dk2[p_], psL2[p_], psP2[p_]
                    rlg, rmx, rng_, rex, res_ = ("lg", p_), ("mx8", p_), ("negm", p_), ("ex4", p_), ("esum", p_)
                    rdf, rsk, rjk, rdk, rpl_, rpp = ("destf", p_), ("selk", p_), ("junk", p_), ("dk", p_), ("psL", p_), ("psP", p_)
                    hTm = hT[m % 2]
                    rh = ("hT", m % 2)
                    for c in range(8):
                        S.add("tensor", lambda e, m=m, c=c: e.transpose(psTr[c // 4][:, c % 4, :], h1[:, m, c * 128:(c + 1) * 128], ident_f), reads=["h1", "cst"], writes=[("psTr", c // 4)])
                        if c % 4 == 3:
                            evac(c // 4, hTm[:, c - 3:c + 1, :], psTr[c // 4], [("psTr", c // 4)], [rh])
                    for c in range(8):
                        mm(psL, hTm[:, c, :], Wr[:, c, :], c == 0, c == 7, [rh, "Wr"], [rpl_])
                    S.add("vector", lambda e, lg=lg, psL=psL: e.tensor_tensor(out=lg, in0=psL, in1=br_rep, op=ALU.add), reads=[rpl_, "cst"], writes=[rlg])
                    S.add("vector", lambda e, lg=lg, mx8=mx8: e.max(out=mx8, in_=lg), reads=[rlg], writes=[rmx])
                    S.add("vector", lambda e, m=m, lg=lg, mx8=mx8: e.tensor_scalar(out=maskall[:, m, :], in0=lg, scalar1=mx8[:, 3:4], scalar2=None, op0=ALU.is_ge), reads=[rlg, rmx], writes=[("maskall", m)])
                    S.add("vector", lambda e, negm=negm, mx8=mx8: e.tensor_scalar(out=negm, in0=mx8[:, 0:1], scalar1=-1.0, scalar2=None, op0=ALU.mult), reads=[rmx], writes=[rng_])
                    S.add("scalar", lambda e, ex4=ex4, mx8=mx8, negm=negm: e.activation(out=ex4, in_=mx8[:, 0:4], func=AF.Exp, bias=negm, scale=1.0), reads=[rmx, rng_], writes=[rex])
                    S.add("vector", lambda e, esum=esum, ex4=ex4: e.tensor_reduce(out=esum, in_=ex4, axis=mybir.AxisListType.X, op=ALU.add), reads=[rex], writes=[res_])
                    S.add("vector", lambda e, esum=esum: e.reciprocal(out=esum, in_=esum), reads=[res_], writes=[res_])
                    S.add("vector", lambda e, m=m, ex4=ex4, esum=esum: e.tensor_scalar(out=gate4[:, m, :], in0=ex4, scalar1=esum, scalar2=None, op0=ALU.mult), reads=[rex, res_], writes=[("gate4", m)])
                    for m2 in range(m):
                        mm(psP, ones_b, maskall[:, m2, :], m2 == 0, False, [("maskall", m2), "ones_b"], [rpp])
                    mm(psP, tri_strict_b, maskall[:, m, :], m == 0, True, [("maskall", m), "tsb"], [rpp])
                    S.add("vector", lambda e, destf=destf, psP=psP: e.scalar_tensor_tensor(out=destf, in0=psP, scalar=float(CAP - 1), in1=iotaC, op0=ALU.min, op1=ALU.add), reads=[rpp, "cst"], writes=[rdf])
                    for k in range(4):
                        S.add("vector", lambda e, k=k, selk=selk, lg=lg, mx8=mx8: e.tensor_scalar(out=selk, in0=lg, scalar1=mx8[:, k:k + 1], scalar2=None, op0=ALU.is_equal), reads=[rlg, rmx], writes=[rsk])
                        S.add("vector", lambda e, k=k, junk=junk, selk=selk, destf=destf: e.tensor_tensor(out=junk, in0=selk, in1=destf, op=ALU.mult), reads=[rsk, rdf], writes=[rjk])
                        S.add("vector", lambda e, k=k, dk=dk, junk=junk: e.tensor_reduce(out=dk[:, k:k + 1], in_=junk, axis=mybir.AxisListType.X, op=ALU.add), reads=[rjk], writes=[rdk])
                    S.add("vector", lambda e, m=m, dk=dk: e.tensor_copy(out=dest_i[:, m, :], in_=dk), reads=[rdk], writes=[("dest_i", m)])
                    hb = hbf[m % 2]
                    rhb = ("hbf", m % 2)
                    S.add("scalar", lambda e, hb=hb, m=m: e.copy(out=hb, in_=h1[:, m, :]), reads=["h1"], writes=[rhb])
                    for k in range(4):
                        S.add("gpsimd", lambda e, hb=hb, m=m, k=k: e.indirect_dma_start(
                            out=X_scr, out_offset=bass.IndirectOffsetOnAxis(ap=dest_i[:, m, k:k + 1], axis=0), in_=hb, in_offset=None),
                            reads=[rhb, ("dest_i", m)], writes=[("X_scr", m, k)], dma=True)
                if "route" in dbg:
                    A.seek(OFF_LOC2 + 48 * 1024)
                    tmp = A.alloc([NOWN, 8], F32)
                    S.add("vector", lambda e: e.tensor_copy(out=tmp[:, :, 0:4], in_=dest_i), reads=[("dest_i", mm_) for mm_ in range(NOWN)], writes=["tmpr"])
                    S.add("vector", lambda e: e.tensor_copy(out=tmp[:, :, 4:8], in_=gate4), reads=[("gate4", mm_) for mm_ in range(NOWN)], writes=["tmpr"])
                    S.add("sync", lambda e: e.dma_start(out=dbg["route"].rearrange("(m p) d -> p m d", p=128), in_=tmp), reads=["tmpr"], writes=["dbgr"], dma=True)
                S.flush()

        if on("E"):
            with contextlib.ExitStack() as st2:
                A.seek(OFF_QT)
                W2h = [A.alloc([8, 512], BF16) for _ in range(3)]
                b1sb = A.alloc([NE * 16], F32)
                b2rep = [A.alloc([D], F32)]
                Ysb = [A.alloc([D], F32) for _ in range(2)]
                Xe = [A.alloc([3, D], BF16)]
                A.seek(OFF_LOC2)
                Xe.append(A.alloc([3, D], BF16))
                NQ = 5
                W1q = [A.alloc([8, 4, 128], BF16) for _ in range(NQ)]
                XT = [A.alloc([8, CAP], BF16) for _ in range(2)]
                actT = A.alloc([8, CAP], BF16)
                glu = [A.alloc([CAP], F32) for _ in range(2)]
                sig = [A.alloc([CAP], F32) for _ in range(4)]
                lin = [A.alloc([CAP], F32) for _ in range(2)]
                psX = psum("psX", [128, 512], BF16)
                psH = [psum(f"psH{i}", [128, CAP]) for i in range(4)]
                psY2 = [psum(f"psY2{i}", [128, 512]) for i in range(2)]
                S.add("sync", lambda e: e.dma_start(out=b1sb, in_=b1T), writes=["b1sb"], dma=True)
                Xv = X_scr.rearrange("(e sb p) d -> e p sb d", sb=3, p=128)
                Yv = Y_scr.rearrange("(e sb p) d -> e sb p d", sb=3, p=128)
                hk = 0
                yk = 0
                pieces = []
                for ex_ in range(NE):
                    pieces += [(ex_, "w1", q_) for q_ in range(4)] + [(ex_, "w2", d_) for d_ in range(2)]
                pbuf = {}
                pstate = {"next": 0, "qi": 0, "w2i": 0}

                def ensure(upto):
                    while pstate["next"] < min(upto, len(pieces)):
                        i = pstate["next"]
                        pstate["next"] += 1
                        ex_, kind, j = pieces[i]
                        if kind == "w1":
                            w1v_ = w_exp_in[ex_].rearrange("(c p) f -> p c f", p=128)
                            f0 = (0, 1024, 512, 1536)[j]
                            wq = W1q[pstate["qi"] % NQ]
                            rwq = ("W1q", pstate["qi"] % NQ)
                            pstate["qi"] += 1
                            S.add("gpsimd", lambda e, wq=wq, w1v_=w1v_, f0=f0: e.dma_start(out=wq.rearrange("p c r f -> p c (r f)"), in_=w1v_[:, :, f0:f0 + 512]), writes=[rwq], dma=True)
                            pbuf[i] = (wq, rwq)
                        else:
                            w2v_ = w_exp_out[ex_].rearrange("(c p) f -> p c f", p=128)
                            w2 = W2h[pstate["w2i"] % 3]
                            rw2 = ("W2h", pstate["w2i"] % 3)
                            pstate["w2i"] += 1
                            S.add("gpsimd", lambda e, w2=w2, w2v_=w2v_, j=j: e.dma_start(out=w2, in_=w2v_[:, :, j * 512:(j + 1) * 512]), writes=[rw2], dma=True)
                            pbuf[i] = (w2, rw2)

                def load_x(ex_):
                    S.add("sync", lambda e, ex_=ex_: e.dma_start(out=Xe[ex_ % 2], in_=Xv[ex_]), reads=["X_scr"], writes=[("Xe", ex_ % 2)], dma=True)

                def transposes(ex_):
                    xe = Xe[ex_ % 2]
                    rxe = ("Xe", ex_ % 2)
                    for c in range(8):
                        for sb in range(3):
                            S.add("tensor", lambda e, xe=xe, c=c, sb=sb: e.transpose(psX[:, sb * 128:(sb + 1) * 128], xe[:, sb, c * 128:(c + 1) * 128], ident_b), reads=[rxe, "ident_b"], writes=["psX"])
                        S.add("vector", lambda e, c=c, ex_=ex_: e.tensor_copy(out=XT[ex_ % 2][:, c, :], in_=psX[:, 0:CAP]), reads=["psX"], writes=[("XT", ex_ % 2)])

                load_x(0)
                ensure(5)
                transposes(0)
                for ex in range(NE):
                    xt = XT[ex % 2]
                    rxt = ("XT", ex % 2)
                    if ex + 1 < NE:
                        load_x(ex + 1)
                    S.add("sync", lambda e, ex=ex: e.dma_start(out=b2rep[0], in_=b_exp_out[ex:ex + 1, :].partition_broadcast(128)), writes=[("b2rep", 0)], dma=True)
                    for half in range(2):
                        pG = ex * 6 + 2 * half
                        ensure(pG + 5)
                        wq, rwq = pbuf.pop(pG)
                        for r in range(4):
                            fb = 4 * half + r
                            pg = psH[hk % 4]; rpg = ("psH", hk % 4); hk += 1
                            for c in range(8):
                                mm(pg, wq[:, c, r, :], xt[:, c, :], c == 0, c == 7, [rwq, rxt], [rpg])
                            g_ = glu[r % 2]; rg_ = ("glu", r % 2)
                            s_ = sig[r]; rs_ = ("sig", r)
                            cg = ex * 16 + fb
                            S.add("vector", lambda e, g_=g_, pg=pg, cg=cg: e.tensor_scalar(out=g_, in0=pg, scalar1=b1sb[:, cg:cg + 1], scalar2=7.0, op0=ALU.add, op1=ALU.min), reads=[rpg, "b1sb"], writes=[rg_])
                            S.add("scalar", lambda e, g_=g_, s_=s_: e.activation(out=s_, in_=g_, func=AF.Silu, scale=1.702), reads=[rg_], writes=[rs_])
                        ensure(pG + 6)
                        wl, rwl = pbuf.pop(pG + 1)
                        for r in range(4):
                            fb = 4 * half + r
                            pl = psH[hk % 4]; rpl = ("psH", hk % 4); hk += 1
                            for c in range(8):
                                mm(pl, wl[:, c, r, :], xt[:, c, :], c == 0, c == 7, [rwl, rxt], [rpl])
                            l_ = lin[r % 2]; rl_ = ("lin", r % 2)
                            s_ = sig[r]; rs_ = ("sig", r)
                            cl = ex * 16 + 8 + fb
                            S.add("vector", lambda e, l_=l_, pl=pl, cl=cl: e.tensor_scalar(out=l_, in0=pl, scalar1=b1sb[:, cl:cl + 1], scalar2=7.0, op0=ALU.add, op1=ALU.min), reads=[rpl, "b1sb"], writes=[rl_])
                            S.add("vector", lambda e, l_=l_: e.tensor_scalar(out=l_, in0=l_, scalar1=-7.0, scalar2=1.0, op0=ALU.max, op1=ALU.add), reads=[rl_], writes=[rl_])
                            S.add("vector", lambda e, fb=fb, s_=s_, l_=l_: e.scalar_tensor_tensor(out=actT[:, fb, :], in0=s_, scalar=1.0 / 1.702, in1=l_, op0=ALU.mult, op1=ALU.mult), reads=[rs_, rl_], writes=["actT"])
                    if ex + 1 < NE:
                        transposes(ex + 1)
                    ensure(ex * 6 + 4 + 5)
                    w2s = [pbuf.pop(ex * 6 + 4), pbuf.pop(ex * 6 + 5)]
                    for sb in range(3):
                        ys = Ysb[yk % 2]
                        rys = ("Ysb", yk % 2)
                        yk += 1
                        for dh in range(2):
                            w2, rw2 = w2s[dh]
                            py = psY2[dh]
                            rpy = ("psY2", dh)
                            for fc in range(8):
                                mm(py, actT[:, fc, sb * 128:(sb + 1) * 128], w2[:, fc, :], fc == 0, fc == 7, ["actT", rw2], [rpy])
                            S.add("vector", lambda e, ys=ys, py=py, dh=dh: e.tensor_tensor(out=ys[:, dh * 512:(dh + 1) * 512], in0=py, in1=b2rep[0][:, dh * 512:(dh + 1) * 512], op=ALU.add),
                                  reads=[rpy, ("b2rep", 0)], writes=[rys])
                        S.add("sync", lambda e, ys=ys, ex=ex, sb=sb: e.dma_start(out=Yv[ex, sb], in_=ys), reads=[rys], writes=["Y_scr"], dma=True)
                S.flush()

        if on("F"):
            with contextlib.ExitStack() as st2:
                A.seek(OFF_LOC2)
                g_rep = A.alloc([D], F32)
                b_rep = A.alloc([D], F32)
                Yk = [A.alloc([D], F32) for _ in range(8)]
                zb = [A.alloc([D], F32) for _ in range(2)]
                ob = [A.alloc([D], F32) for _ in range(2)]
                stats = A.alloc([2, 6], F32)
                mv = A.alloc([2], F32)
                rstd = A.alloc([1], F32)
                S.add("sync", lambda e: e.dma_start(out=g_rep, in_=lnrep[2]), writes=["lnp"], dma=True)
                S.add("sync", lambda e: e.dma_start(out=b_rep, in_=lnrep[3]), writes=["lnp"], dma=True)
                for m in range(NOWN):
                    z = zb[m % 2]
                    rz = ("z", m % 2)
                    S.add("vector", lambda e, z=z, m=m: e.tensor_scalar(out=z, in0=h1[:, m, :], scalar1=ALPHA, scalar2=None, op0=ALU.mult), reads=["h1"], writes=[rz])
                    for k in range(4):
                        yk_ = Yk[(4 * m + k) % 8]
                        ryk = ("Yk", (4 * m + k) % 8)
                        S.add("gpsimd", lambda e, yk_=yk_, m=m, k=k: e.indirect_dma_start(
                            out=yk_, out_offset=None, in_=Y_scr, in_offset=bass.IndirectOffsetOnAxis(ap=dest_i[:, m, k:k + 1], axis=0)),
                            reads=["Y_scr", "dest_i"], writes=[ryk], dma=True)
                        S.add("vector", lambda e, z=z, yk_=yk_, m=m, k=k: e.scalar_tensor_tensor(out=z, in0=yk_, scalar=gate4[:, m, k:k + 1], in1=z, op0=ALU.mult, op1=ALU.add), reads=[ryk, "gate4", rz], writes=[rz])
                    o_ = ob[m % 2]
                    ro = ("ob", m % 2)
                    layer_norm(z, rz, o_, ro, g_rep, b_rep, stats, mv, rstd)
                    S.add("sync", lambda e, o_=o_, m=m: e.dma_start(out=out_d[m * 128:(m + 1) * 128, :], in_=o_), reads=[ro], writes=["out"], dma=True)
                S.flush()
    return nc


def _t5_bucket(dist):
    dist = np.asarray(dist, dtype=np.int64)
    nf = np.maximum(dist, 16).astype(np.float32)
    large = 16 + (np.log(nf / np.float32(16)) / np.float32(math.log(2048 / 16)) * np.float32(16)).astype(np.int32)
    large = np.minimum(large, 31)
    return np.where(dist < 16, dist, large).astype(np.int64)


def _host_consts(core, b_fgate, b_router):
    n = 6 * 128 + 32 + NTS + 24 + 32
    c = np.zeros((128, n), np.float32)
    idx = np.arange(128)
    c[:, 0:128] = np.eye(128, dtype=np.float32)
    c[:, 128:256] = (idx[:, None] <= idx[None, :])
    c[:, 256:384] = (idx[:, None] < idx[None, :])
    c[:, 384:512] = 1.0
    c[64, 512:640] = 1.0
    c[:, 640:768] = np.where(idx[:, None] <= idx[None, :], 0.0, NEG)
    c[:, 768:800] = (np.arange(NE) * CAP)[None, :]
    s = np.arange(NTS)
    a = s - 7 + core
    c[:, 800:800 + NTS] = np.where((a >= 0) & (a < 128), 0.0, NEG)[None, :]
    c[:, 800 + NTS:800 + NTS + 24] = np.tile(np.asarray(b_fgate, np.float32).reshape(1, 6), (128, 4))
    c[:, 800 + NTS + 24:800 + NTS + 56] = np.asarray(b_router, np.float32).reshape(1, NE)
    return c


def _dil_bias(t5_bias):
    out = np.full((6, 128, 17, 128), NEG, np.float32)
    k = np.arange(128)[:, None]
    q = np.arange(128)[None, :]
    for hd in range(6):
        window, dil = DIL_CFG[hd // 2]
        for dl in range(DMAX[hd // 2] + 1):
            dist = 128 * dl + q - k
            valid = (dist >= 0) & (dist % dil == 0) & (dist <= window)
            b = _t5_bucket(np.clip(dist, 0, None))
            vals = t5_bias[b, hd]
            out[hd, :, dl, :] = np.where(valid, vals, np.float32(NEG))
    return out


_CACHE = {}


def prepare_inputs(inputs):
    x = np.asarray(inputs["x"], np.float32)[0]
    T = NTS * 128
    shared = {
        "memT": np.ascontiguousarray(np.asarray(inputs["mem"], np.float32)[0].T),
        "w_in": np.ascontiguousarray(np.asarray(inputs["w_in"], np.float32)[0]),
        "w_mem_kv": np.ascontiguousarray(np.asarray(inputs["w_mem_kv"], np.float32)[0]),
        "w_br": np.ascontiguousarray(np.concatenate([np.asarray(inputs["w_br_fox"], np.float32)[0], np.asarray(inputs["w_br_dil"], np.float32)[0],
                                                     np.asarray(inputs["w_br_mem"], np.float32)[0]], axis=0)),
        "w_out": np.ascontiguousarray(np.asarray(inputs["w_out"], np.float32)[0]),
        "w_router": np.ascontiguousarray(np.asarray(inputs["w_router"], np.float32)[0]),
        "w_exp_in": np.ascontiguousarray(np.asarray(inputs["w_exp_in"], np.float32)[0]),
        "w_exp_out": np.ascontiguousarray(np.asarray(inputs["w_exp_out"], np.float32)[0]),
        "b_exp_out": np.ascontiguousarray(np.asarray(inputs["b_exp_out"], np.float32)[0]),
        "b1T": np.ascontiguousarray(np.asarray(inputs["b_exp_in"], np.float32)[0].reshape(NE, 16, 128).transpose(2, 0, 1).reshape(128, NE * 16)),
        "lnrep": np.ascontiguousarray(np.stack([np.broadcast_to(np.asarray(inputs[k], np.float32)[0][None, :], (128, D)) for k in ("ln1_g", "ln1_b", "ln2_g", "ln2_b")])),
        "dilb": _dil_bias(np.asarray(inputs["t5_bias"], np.float32)),
    }
    xt = x.reshape(NOWN, NCORES, 128, D)
    in_maps = []
    for i in range(NCORES):
        xs = np.zeros((T, D), np.float32)
        xs[(7 - i) * 128:(7 - i) * 128 + S_LEN] = x
        mp = dict(shared)
        mp["xT"] = np.ascontiguousarray(xs.T)
        mp["x_own"] = np.ascontiguousarray(xt[:, i].reshape(NOWN * 128, D))
        mp["cst"] = _host_consts(i, inputs["b_fgate"], inputs["b_router"])
        a_tile = np.arange(NTS) - 7 + i
        mp["padrow"] = np.repeat(np.where((a_tile >= 0) & (a_tile < 128), 0.0, NEG).astype(np.float32), 128)[None, :]
        in_maps.append(mp)
    return in_maps


def assemble(results, key="out", width=D):
    out = np.zeros((NOWN, NCORES, 128, width), np.float32)
    for i in range(NCORES):
        out[:, i] = np.asarray(results[i][key], np.float32).reshape(NOWN, 128, width)
    return out.reshape(1, S_LEN, width)


def kernel(**inputs):
    if "nc" not in _CACHE:
        _CACHE["nc"] = build_program()
    in_maps = prepare_inputs(inputs)
    res = run_bass_kernel_spmd(_CACHE["nc"], in_maps, core_ids=list(range(NCORES)))
    return assemble(res.results)
```

```python
import contextlib
import math
import numpy as np
import concourse.bass as bass
import concourse.mybir as mybir
from concourse.bass_utils import run_bass_kernel_spmd

F32 = mybir.dt.float32
BF16 = mybir.dt.bfloat16
I32 = mybir.dt.int32
AF = mybir.ActivationFunctionType
ALU = mybir.AluOpType

NCORES = 8
D = 1024
S_LEN = 16384
NTS = 136
NOWN = 16
NE = 32
CAP = 384
ALPHA = 2.0 ** 0.25
NEG = -30000.0
DIL_CFG = ((128, 1), (512, 4), (2048, 16))
DMAX = (1, 4, 16)
ENGINES = ("tensor", "vector", "scalar", "gpsimd", "sync")
PHASES = ("A", "Q", "B", "C", "Dm", "M1", "M2", "R", "E", "F")


class Sched:
    NDS = 48

    def __init__(self, nc, stack):
        self.nc = nc
        self.esem = {e: stack.enter_context(nc.semaphore(f"s_{e}")) for e in ENGINES}
        self.dsem = [stack.enter_context(nc.semaphore(f"d_{i}")) for i in range(self.NDS)]
        self.ecount = {e: 0 for e in ENGINES}
        self.dcount = [0] * self.NDS
        self.dnext = {"sync": 0, "gpsimd": 0}
        self.reset()

    def reset(self):
        self.ops = []
        self.last_writer = {}
        self.readers = {}

    def add(self, engine, fn, reads=(), writes=(), dma=False):
        deps = set()
        for r in reads:
            if r in self.last_writer:
                deps.add(self.last_writer[r])
        for w in writes:
            if w in self.last_writer:
                deps.add(self.last_writer[w])
            for rd in self.readers.get(w, ()):
                deps.add(rd)
        oid = len(self.ops)
        self.ops.append((engine, fn, deps, dma))
        for r in reads:
            self.readers.setdefault(r, []).append(oid)
        for w in writes:
            self.last_writer[w] = oid
            self.readers[w] = []
        return oid

    def flush(self):
        nc = self.nc
        ops = self.ops
        n = len(ops)
        if n == 0:
            return
        needed = [False] * n
        for (_, _, deps, _) in ops:
            for d in deps:
                needed[d] = True
        last_on = {}
        for i, (e, _, _, dma) in enumerate(ops):
            if not dma:
                last_on[e] = i
        for i in last_on.values():
            needed[i] = True
        comp = [None] * n
        issue_wait = {}
        prev_on_sem = {}
        for i, (eng, fn, deps, dma) in enumerate(ops):
            if dma:
                half = self.NDS // 2
                s = self.dnext[eng] + (half if eng == "gpsimd" else 0)
                self.dnext[eng] = (self.dnext[eng] + 1) % half
                if self.dcount[s] > 0:
                    issue_wait[i] = (s, self.dcount[s])
                self.dcount[s] += 16
                comp[i] = ("d", s, self.dcount[s])
            elif needed[i]:
                self.ecount[eng] += 1
                comp[i] = ("e", eng, self.ecount[eng])
        final_e = dict(self.ecount)
        final_d = list(self.dcount)
        esem, dsem = self.esem, self.dsem

        def run_engine(ename):
            def body(eng):
                waited = {}

                def do_wait(key, semh, val):
                    if waited.get(key, -1) >= val:
                        return
                    eng.wait_ge(semh, val)
                    waited[key] = val

                for i, (e, fn, deps, dma) in enumerate(ops):
                    if e != ename:
                        continue
                    for d in sorted(deps):
                        c = comp[d]
                        if c is None:
                            continue
                        if c[0] == "e":
                            if c[1] == "tensor" and ename == "tensor":
                                continue
                            do_wait(("e", c[1]), esem[c[1]], c[2])
                        else:
                            do_wait(("d", c[1]), dsem[c[1]], c[2])
                    if i in issue_wait:
                        s, v = issue_wait[i]
                        do_wait(("d", s), dsem[s], v)
                    ins = fn(eng)
                    c = comp[i]
                    if c is not None:
                        if c[0] == "e":
                            ins.then_inc(esem[c[1]], 1)
                        else:
                            ins.then_inc(dsem[c[1]], 16)
                for e2 in ENGINES:
                    if final_e[e2] > 0:
                        do_wait(("e", e2), esem[e2], final_e[e2])
                for s in range(self.NDS):
                    if final_d[s] > 0:
                        do_wait(("d", s), dsem[s], final_d[s])
            return body

        with nc.Block() as block:
            block.tensor(run_engine("tensor"))
            block.vector(run_engine("vector"))
            block.scalar(run_engine("scalar"))
            block.gpsimd(run_engine("gpsimd"))
            block.sync(run_engine("sync"))
        self.reset()


class Arena:
    def __init__(self, nc, stack, nbytes):
        self.t = stack.enter_context(nc.sbuf_tensor("arena", [128, nbytes // 4], F32))
        self.nbytes = nbytes
        self.off = 0

    def seek(self, off):
        self.off = off

    def alloc(self, free_shape, dtype):
        n = int(np.prod(free_shape))
        esz = 4 if dtype in (F32, I32) else 2
        nb = (n * esz + 31) // 32 * 32
        assert self.off + nb <= self.nbytes, (self.off, nb, self.nbytes)
        ap = self.t[:, self.off // 4:(self.off + nb) // 4]
        self.off += nb
        if dtype != F32:
            ap = ap.bitcast(dtype)
        ap = ap[:, 0:n]
        if len(free_shape) == 2:
            ap = ap.rearrange("p (a b) -> p a b", b=free_shape[1])
        elif len(free_shape) == 3:
            ap = ap.rearrange("p (a b c) -> p a b c", b=free_shape[1], c=free_shape[2])
        elif len(free_shape) == 4:
            ap = ap.rearrange("p (a b c d) -> p a b c d", b=free_shape[1], c=free_shape[2], d=free_shape[3])
        return ap


def build_program(last_phase="F", debug=()):
    nc = bass.Bass("TRN2", target_bir_lowering=False)
    T = NTS * 128

    def din(name, shape, dt=F32):
        return nc.dram_tensor(name, list(shape), dt, kind="ExternalInput").ap()

    xT = din("xT", [D, T])
    x_own = din("x_own", [NOWN * 128, D])
    memT = din("memT", [D, 256])
    w_in = din("w_in", [D, 5638])
    w_mem_kv = din("w_mem_kv", [D, 512])
    w_br = din("w_br", [D, D])
    w_out = din("w_out", [D, D])
    w_router = din("w_router", [D, NE])
    if PHASES.index(last_phase) >= PHASES.index("E"):
        w_exp_in = din("w_exp_in", [NE, D, 2 * D])
        w_exp_out = din("w_exp_out", [NE, D, D])
    b_exp_out = din("b_exp_out", [NE, D])
    b1T = din("b1T", [128, NE * 16])
    lnrep = din("lnrep", [4, 128, D])
    cst = din("cst", [128, 6 * 128 + 32 + NTS + 24 + 32])
    dilb = din("dilb", [6, 128, 17, 128])
    padrow = din("padrow", [1, NTS * 128])
    out_d = nc.dram_tensor("out", [NOWN * 128, D], F32, kind="ExternalOutput").ap()
    dbg = {}
    for name, shape in debug:
        dbg[name] = nc.dram_tensor(name, list(shape), F32, kind="ExternalOutput").ap()

    KT_scr = nc.dram_tensor("KT_scr", [6, 128, T], BF16, kind="Internal").ap()
    V_scr = nc.dram_tensor("V_scr", [12, 128, NTS, 65], BF16, kind="Internal").ap()
    X_scr = nc.dram_tensor("X_scr", [NE * CAP, D], BF16, kind="Internal").ap()
    Y_scr = nc.dram_tensor("Y_scr", [NE * CAP, D], F32, kind="Internal").ap()
    Gd = nc.dram_tensor("Gd", [6, 3, NTS * 128], BF16, kind="Internal").ap()

    xTv = xT.rearrange("(c p) t -> p c t", p=128)
    xTown = xT.rearrange("(c p) (m j t) -> p c m j t", p=128, j=8, t=128)
    w_inv = w_in.rearrange("(c p) n -> p c n", p=128)

    li = PHASES.index(last_phase)

    def on(ph):
        return PHASES.index(ph) <= li

    with contextlib.ExitStack() as st:
        S = Sched(nc, st)
        A = Arena(nc, st, 206 * 1024)

        def psum(name, shape, dt=F32):
            return st2.enter_context(nc.psum_tensor(name, list(shape), dt))[:]

        A.seek(0)
        cst_sb = A.alloc([6 * 128 + 32 + NTS + 24 + 32], F32)
        ident_f = cst_sb[:, 0:128]
        tri_incl = cst_sb[:, 128:256]
        tri_strict_f = cst_sb[:, 256:384]
        ones_f = cst_sb[:, 384:512]
        e64 = cst_sb[:, 512:640]
        foxtri = cst_sb[:, 640:768]
        iotaC = cst_sb[:, 768:800]
        padb = cst_sb[:, 800:800 + NTS]
        bf_rep4 = cst_sb[:, 800 + NTS:800 + NTS + 24].rearrange("p (a b) -> p a b", b=6)
        br_rep = cst_sb[:, 800 + NTS + 24:800 + NTS + 56]
        ident_b = A.alloc([128], BF16)
        tri_strict_b = A.alloc([128], BF16)
        ones_b = A.alloc([128], BF16)
        foxtri_b = A.alloc([128], BF16)
        G_all = A.alloc([NTS, 6], F32)
        Gref = A.alloc([NOWN, 6], F32)
        dest_i = A.alloc([NOWN, 4], I32)
        gate4 = A.alloc([NOWN, 4], F32)
        assert A.off <= 12 * 1024, A.off
        OFF_QT = 12 * 1024
        OFF_O = OFF_QT + 44 * 1024
        OFF_LOC = OFF_O + 32 * 1024
        OFF_LOC2 = OFF_O + 64 * 1024
        A.seek(OFF_QT)
        QTf = A.alloc([6, NOWN * 128], BF16)
        QT = A.alloc([5, NOWN * 128], BF16)
        A.seek(OFF_QT)
        mergedT = A.alloc([8, NOWN * 128], BF16)
        A.seek(OFF_O)
        o_fox = A.alloc([NOWN, 384], BF16)
        o_dil = A.alloc([NOWN, 384], BF16)
        o_mem = A.alloc([NOWN, 256], BF16)
        A.seek(OFF_O)
        h1 = A.alloc([NOWN, D], F32)

        S.add("sync", lambda e: e.dma_start(out=cst_sb, in_=cst), writes=["cst"], dma=True)
        S.add("vector", lambda e: e.tensor_copy(out=ident_b, in_=ident_f), reads=["cst"], writes=["ident_b"])
        S.add("vector", lambda e: e.tensor_copy(out=tri_strict_b, in_=tri_strict_f), reads=["cst"], writes=["tsb"])
        S.add("vector", lambda e: e.tensor_copy(out=ones_b, in_=ones_f), reads=["cst"], writes=["ones_b"])
        S.add("vector", lambda e: e.tensor_copy(out=foxtri_b, in_=foxtri), reads=["cst"], writes=["foxtri_b"])
        S.flush()

        def evac(i, out, in_, reads, writes, scale=None):
            if i % 2 == 0:
                if scale is None:
                    S.add("scalar", lambda e: e.copy(out=out, in_=in_), reads, writes)
                else:
                    S.add("scalar", lambda e: e.mul(out=out, in_=in_, mul=scale), reads, writes)
            else:
                if scale is None:
                    S.add("vector", lambda e: e.tensor_copy(out=out, in_=in_), reads, writes)
                else:
                    S.add("vector", lambda e: e.tensor_scalar(out=out, in0=in_, scalar1=scale, scalar2=None, op0=ALU.mult), reads, writes)

        def mm(out, lhsT, rhs, start, stop, reads, writes):
            S.add("tensor", lambda e: e.matmul(out, lhsT, rhs, start=start, stop=stop), reads, writes)

        if on("A"):
            with contextlib.ExitStack() as st2:
                A.seek(OFF_LOC)
                Wk = A.alloc([8, 768], BF16)
                Wv = A.alloc([8, 768], BF16)
                Wf = A.alloc([8, 6], BF16)
                xbuf = [A.alloc([8, 512], BF16) for _ in range(2)]
                ktst = [A.alloc([6, 512], BF16) for _ in range(2)]
                vst = [A.alloc([12, 4, 65], BF16) for _ in range(2)]
                Z_all = A.alloc([NTS, 6], F32)
                SP_all = A.alloc([NTS, 6], F32)
                Wsb = A.alloc([NTS, 6], F32)
                Tsb = A.alloc([NTS, 6], F32)
                HA = A.alloc([NTS, 6], F32)
                HB = A.alloc([NTS, 6], F32)
                zero_t = A.alloc([8, D], BF16)
                Gp = [A.alloc([NTS, 6], BF16) for _ in range(3)]
                R1 = A.alloc([NTS, 6], F32)
                R2 = A.alloc([NTS, 6], F32)
                gst = [A.alloc([128], BF16) for _ in range(2)]
                psGT = psum("psGT", [128, 128], BF16)
                psK = [psum(f"psK{i}", [128, 512]) for i in range(2)]
                psV = [psum(f"psV{i}", [128, 384]) for i in range(2)]
                psF = psum("psF", [128, 4, 6])
                psW = [psum(f"psW{i}", [128, 408]) for i in range(2)]

                S.add("vector", lambda e: e.memset(zero_t, 0.0), writes=["zero_t"])
                Xz = X_scr.rearrange("(a b p) d -> a p b d", b=8, p=128)
                for a in range(NE * CAP // 1024):
                    S.add("sync", lambda e, a=a: e.dma_start(out=Xz[a], in_=zero_t), reads=["zero_t"], writes=[("X_scr", a)], dma=True)

                for (dst, lo) in ((Wk[:, :, 0:384], 384), (Wk[:, :, 384:768], 1542), (Wv[:, :, 0:384], 768), (Wv[:, :, 384:768], 1926)):
                    S.add("gpsimd", lambda e, dst=dst, lo=lo: e.dma_start(out=dst, in_=w_inv[:, :, lo:lo + 384]), writes=["WA"], dma=True)
                S.add("gpsimd", lambda e: e.dma_start(out=Wf, in_=w_inv[:, :, 1152:1158]), writes=["WA"], dma=True)
                for b in range(2):
                    S.add("vector", lambda e, b=b: e.memset(vst[b][:, :, :, 64:65], 1.0), writes=[("vst1", b)])

                KTv = KT_scr.rearrange("r p t -> p r t")
                Vv = V_scr.rearrange("h p t c -> p h t c")
                ev = 0
                for tb in range(NTS // 4):
                    xb = xbuf[tb % 2]
                    rx = ("xb", tb % 2)
                    S.add("gpsimd", lambda e, xb=xb, tb=tb: e.dma_start(out=xb, in_=xTv[:, :, tb * 512:(tb + 1) * 512]), writes=[rx], dma=True)
                    kt = ktst[tb % 2]
                    rk = ("ktst", tb % 2)
                    for pr in range(6):
                        ps = psK[pr % 2]
                        rp = ("psK", pr % 2)
                        for c in range(8):
                            mm(ps, Wk[:, c, pr * 128:(pr + 1) * 128], xb[:, c, :], c == 0, c == 7, [rx, "WA"], [rp])
                        evac(ev, kt[:, pr, :], ps, [rp], [rk]); ev += 1
                    S.add("sync", lambda e, kt=kt, tb=tb: e.dma_start(out=KTv[:, :, tb * 512:(tb + 1) * 512], in_=kt), reads=[rk], writes=[("KT_scr", tb)], dma=True)
                    vs = vst[tb % 2]
                    rv = ("vst", tb % 2)
                    for t4 in range(4):
                        for grp in range(2):
                            ps = psV[grp]
                            rp = ("psV", grp)
                            for c in range(8):
                                mm(ps, xb[:, c, t4 * 128:(t4 + 1) * 128], Wv[:, c, grp * 384:(grp + 1) * 384], c == 0, c == 7, [rx, "WA"], [rp])
                            evac(ev, vs[:, grp * 6:(grp + 1) * 6, t4, 0:64], ps.rearrange("p (h d) -> p h d", d=64), [rp], [rv, ("vst1", tb % 2)]); ev += 1
                    S.add("sync", lambda e, vs=vs, tb=tb: e.dma_start(out=Vv[:, :, tb * 4:(tb + 1) * 4, :], in_=vs), reads=[rv, ("vst1", tb % 2)], writes=[("V_scr", tb)], dma=True)
                    for t4 in range(4):
                        for c in range(8):
                            mm(psF[:, t4, :], xb[:, c, t4 * 128:(t4 + 1) * 128], Wf[:, c, :], c == 0, c == 7, [rx, "WA"], ["psF"])
                    S.add("vector", lambda e, tb=tb: e.tensor_tensor(out=Z_all[:, tb * 4:(tb + 1) * 4, :], in0=psF, in1=bf_rep4, op=ALU.add), reads=["psF", "cst"], writes=["Z_all"])

                Zf = Z_all.rearrange("p a b -> p (a b)")
                SPf = SP_all.rearrange("p a b -> p (a b)")
                S.add("scalar", lambda e: e.activation(out=SPf, in_=Zf, func=AF.Exp, scale=-1.0), reads=["Z_all"], writes=["SP"])
                S.add("scalar", lambda e: e.activation(out=SPf, in_=SPf, func=AF.Ln, bias=1.0, scale=1.0), reads=["SP"], writes=["SP"])
                Wf2 = Wsb.rearrange("p a b -> p (a b)")
                Tf2 = Tsb.rearrange("p a b -> p (a b)")
                for hf in range(2):
                    mm(psW[hf], tri_incl, SPf[:, hf * 408:(hf + 1) * 408], True, True, ["SP", "cst"], [("psW", hf)])
                    S.add("vector", lambda e, hf=hf: e.tensor_copy(out=Wf2[:, hf * 408:(hf + 1) * 408], in_=psW[hf]), reads=[("psW", hf)], writes=["Wsb"])
                for hf in range(2):
                    mm(psW[hf], ones_f, SPf[:, hf * 408:(hf + 1) * 408], True, True, ["SP", "cst"], [("psW", hf)])
                    S.add("vector", lambda e, hf=hf: e.tensor_copy(out=Tf2[:, hf * 408:(hf + 1) * 408], in_=psW[hf]), reads=[("psW", hf)], writes=["Tsb"])
                S.add("vector", lambda e: e.tensor_copy(out=HA, in_=Tsb), reads=["Tsb"], writes=["HA"])
                cur, nxt, rc, rn = HA, HB, "HA", "HB"
                sh = 1
                while sh < NTS:
                    S.add("vector", lambda e, cur=cur, nxt=nxt, sh=sh: e.tensor_copy(out=nxt[:, 0:sh, :], in_=cur[:, 0:sh, :]), reads=[rc], writes=[rn])
                    S.add("vector", lambda e, cur=cur, nxt=nxt, sh=sh: e.tensor_tensor(out=nxt[:, sh:NTS, :], in0=cur[:, sh:NTS, :], in1=cur[:, 0:NTS - sh, :], op=ALU.add), reads=[rc], writes=[rn])
                    cur, nxt, rc, rn = nxt, cur, rn, rc
                    sh *= 2
                S.add("vector", lambda e, cur=cur: e.tensor_tensor(out=G_all, in0=cur, in1=Tsb, op=ALU.subtract), reads=[rc, "Tsb"], writes=["G_all"])
                S.add("vector", lambda e: e.tensor_tensor(out=G_all, in0=G_all, in1=Wsb, op=ALU.add), reads=["G_all", "Wsb"], writes=["G_all"])
                Gown = G_all.rearrange("p (m j) h -> p m j h", j=8)[:, 0:NOWN, 7, :]
                S.add("vector", lambda e: e.tensor_copy(out=HA[:, 0:NOWN, :], in_=Gown), reads=["G_all"], writes=["HA", "HB"])
                mm(psW[0][:, 0:NOWN * 6], e64, HA[:, 0:NOWN, :].rearrange("p a b -> p (a b)"), True, True, ["HA", "cst"], [("psW", 0)])
                S.add("vector", lambda e: e.tensor_copy(out=Gref.rearrange("p a b -> p (a b)"), in_=psW[0][:, 0:NOWN * 6]), reads=[("psW", 0)], writes=["Gref"])
                S.add("vector", lambda e: e.tensor_copy(out=Gp[0], in_=G_all), reads=["G_all"], writes=["Gp0"])
                S.add("vector", lambda e: e.tensor_tensor(out=R1, in0=G_all, in1=Gp[0], op=ALU.subtract), reads=["G_all", "Gp0"], writes=["R1"])
                S.add("vector", lambda e: e.tensor_copy(out=Gp[1], in_=R1), reads=["R1"], writes=["Gp1"])
                S.add("vector", lambda e: e.tensor_tensor(out=R2, in0=R1, in1=Gp[1], op=ALU.subtract), reads=["R1", "Gp1"], writes=["R2"])
                S.add("vector", lambda e: e.tensor_copy(out=Gp[2], in_=R2), reads=["R2"], writes=["Gp2"])
                Gdv = Gd.rearrange("h r (s p) -> h r s p", p=128)
                kk = 0
                for h in range(6):
                    for part in range(3):
                        for (s0, ns) in ((0, 128), (128, NTS - 128)):
                            stg = gst[kk % 2]
                            rst = ("gst", kk % 2)
                            S.add("tensor", lambda e, h=h, part=part, s0=s0, ns=ns: e.transpose(psGT[0:ns, :], Gp[part][:, s0:s0 + ns, h], ident_b), reads=[f"Gp{part}", "ident_b"], writes=["psGT"])
                            evac(kk, stg[0:ns, :], psGT[0:ns, :], ["psGT"], [rst])
                            S.add("sync", lambda e, h=h, part=part, s0=s0, ns=ns, stg=stg: e.dma_start(out=Gdv[h, part, s0:s0 + ns, :], in_=stg[0:ns, :]), reads=[rst], writes=[("Gd", h, part, s0)], dma=True)
                            kk += 1
                if "G_all" in dbg:
                    S.add("sync", lambda e: e.dma_start(out=dbg["G_all"], in_=G_all.rearrange("p a b -> p (a b)")), reads=["G_all"], writes=["dbgG"], dma=True)
                S.flush()

        if on("Q"):
            with contextlib.ExitStack() as st2:
                A.seek(OFF_LOC)
                Wq = A.alloc([8, 1024], BF16)
                xo = A.alloc([8, NOWN, 128], BF16)
                psQ = [psum(f"psQ{i}", [128, 512]) for i in range(2)]
                for (lo_dst, lo, n) in ((0, 0, 384), (384, 1158, 384), (768, 2310, 256)):
                    S.add("gpsimd", lambda e, lo_dst=lo_dst, lo=lo, n=n: e.dma_start(out=Wq[:, :, lo_dst:lo_dst + n], in_=w_inv[:, :, lo:lo + n]), writes=["Wq"], dma=True)
                for c in range(8):
                    S.add("gpsimd", lambda e, c=c: e.dma_start(out=xo[:, c, :, :], in_=xTown[:, c, 0:NOWN, 7, :]), writes=["xo"], dma=True)
                xof = xo.rearrange("p c m t -> p c (m t)")
                k = 0
                for b4 in range(4):
                    for h in range(6):
                        ps = psQ[k % 2]
                        for c in range(8):
                            mm(ps[0:64, :], Wq[:, c, h * 64:(h + 1) * 64], xof[:, c, b4 * 512:(b4 + 1) * 512], c == 0, c == 7, ["Wq", "xo"], [("psQ", k % 2)])
                        evac(k, QTf[0:64, h, b4 * 512:(b4 + 1) * 512], ps[0:64, :], [("psQ", k % 2)], ["QTf"], scale=0.125)
                        k += 1
                    for pr in range(5):
                        ps = psQ[k % 2]
                        for c in range(8):
                            mm(ps, Wq[:, c, 384 + pr * 128:384 + (pr + 1) * 128], xof[:, c, b4 * 512:(b4 + 1) * 512], c == 0, c == 7, ["Wq", "xo"], [("psQ", k % 2)])
                        evac(k, QT[:, pr, b4 * 512:(b4 + 1) * 512], ps, [("psQ", k % 2)], ["QT"], scale=0.125)
                        k += 1
                S.add("vector", lambda e: e.memset(QTf[64:69, :, :], 1.0), writes=["QTfa"])
                for h in range(6):
                    for m in range(NOWN):
                        S.add("vector", lambda e, h=h, m=m: e.tensor_scalar(out=QTf[64:65, h, m * 128:(m + 1) * 128], in0=ones_f[64:65, :], scalar1=Gref[64:65, m, h:h + 1], scalar2=-1.0,
                                                                    op0=ALU.mult, op1=ALU.mult), reads=["Gref", "cst", "QTfa"], writes=["QTfa"])
                S.flush()

        def load_kv(KTb, Vb, pr, hp, vh, par, ntiles):
            rk, rv = ("KTb", par), ("Vb", par)
            q4 = (ntiles * 128) // 4
            for q in range(4):
                S.add("sync", lambda e, q=q: e.dma_start(out=KTb[hp:hp + 64, q * q4:(q + 1) * q4], in_=KT_scr[pr, hp:hp + 64, q * q4:(q + 1) * q4]), writes=[rk], dma=True)
            S.add("sync", lambda e: e.dma_start(out=Vb[:, 0:ntiles, :], in_=V_scr[vh, :, 0:ntiles, :]), writes=[rv], dma=True)
            return rk, rv

        class AttnPipe:
            LA = 2

            def __init__(self, psS, Pt, ssb, psO):
                self.psS, self.Pt, self.ssb, self.psO = psS, Pt, ssb, psO
                self.cnt = 0
                self.acc_i = 0
                self.pending = []

            def _drain(self, keep):
                while len(self.pending) > keep:
                    self.pending.pop(0)()

            def run(self, items, finish):
                acc = self.psO[self.acc_i % 2]
                racc = ("psO", self.acc_i % 2)
                self.acc_i += 1
                n = len(items)
                for idx, it in enumerate(items):
                    slot = self.cnt % 8
                    sslot = self.cnt % 4
                    self.cnt += 1
                    ps = self.psS[sslot]
                    rps = ("psS", sslot)
                    P = self.Pt[:, slot, :]
                    rP = ("P", slot)
                    mm(ps, it["lhsT"], it["rhs"], True, True, it["reads"], [rps])
                    src, rsrc = ps, rps
                    if it["mask"] is not None:
                        sb = self.ssb[:, slot % 4, :]
                        rsb = ("ssb", slot % 4)
                        S.add("vector", lambda e, sb=sb, ps=ps, m=it["mask"]: e.tensor_tensor(out=sb, in0=ps, in1=m, op=ALU.add), reads=[rps] + it["mreads"], writes=[rsb])
                        src, rsrc = sb, rsb
                    if it["bias"] is not None:
                        S.add("scalar", lambda e, P=P, src=src, b=it["bias"]: e.activation(out=P, in_=src, func=AF.Exp, bias=b, scale=1.0), reads=[rsrc] + it["breads"], writes=[rP])
                    else:
                        S.add("scalar", lambda e, P=P, src=src: e.activation(out=P, in_=src, func=AF.Exp), reads=[rsrc], writes=[rP])

                    def pv(P=P, rP=rP, it=it, first=(idx == 0), last=(idx == n - 1), acc=acc, racc=racc):
                        mm(acc, P, it["v"], first, last, [rP] + it["vreads"], [racc])
                        if last:
                            finish(acc, racc)
                    self.pending.append(pv)
                    self._drain(self.LA)

            def flush(self):
                self._drain(0)

        if on("B"):
            with contextlib.ExitStack() as st2:
                A.seek(OFF_LOC)
                KTb = [A.alloc([NTS * 128], BF16) for _ in range(2)]
                Vb = [A.alloc([NTS, 65], BF16) for _ in range(2)]
                NPB = 4
                Pt = [A.alloc([512], BF16) for _ in range(NPB)]
                rec = A.alloc([8], F32)
                psS = [psum(f"psS{i}", [128, 512]) for i in range(NPB)]
                psO = [psum(f"psO{i}", [128, 65]) for i in range(4)]
                fin_i = [0]
                if "o_fox" in dbg:
                    S.add("vector", lambda e: e.memset(o_fox, 0.0), writes=["o_attn"])
                pending = []
                gcnt = [0]
                acc_i = [0]

                def drain(keep):
                    while len(pending) > keep:
                        pending.pop(0)()

                def finish(acc, racc, dst):
                    r = rec[:, fin_i[0] % 8:fin_i[0] % 8 + 1]
                    rr = ("rec", fin_i[0] % 8)
                    fin_i[0] += 1
                    S.add("vector", lambda e: e.reciprocal(out=r, in_=acc[:, 64:65]), reads=[racc], writes=[rr])
                    S.add("vector", lambda e: e.tensor_scalar(out=dst, in0=acc[:, 0:64], scalar1=r, scalar2=None, op0=ALU.mult), reads=[racc, rr], writes=["o_attn"])

                for h in range(6):
                    par = h % 2
                    hp = (h % 2) * 64
                    pr = h // 2
                    KA = KTb[par]
                    rk, rv = ("KTb", par), ("Vb", par)
                    q4 = (NTS * 128) // 4
                    for q in range(4):
                        S.add("sync", lambda e, q=q, KA=KA, pr=pr, hp=hp: e.dma_start(out=KA[0:64, q * q4:(q + 1) * q4], in_=KT_scr[pr, hp:hp + 64, q * q4:(q + 1) * q4]), writes=[rk], dma=True)
                    S.add("vector", lambda e, KA=KA: e.memset(KA[64:65, :], 1.0), writes=[rk])
                    S.add("sync", lambda e, KA=KA, h=h: e.dma_start(out=KA[65:68, :], in_=Gd[h]), writes=[rk], dma=True)
                    S.add("gpsimd", lambda e, KA=KA: e.dma_start(out=KA[68:69, :], in_=padrow), writes=[rk], dma=True)
                    S.add("sync", lambda e, par=par, h=h: e.dma_start(out=Vb[par], in_=V_scr[h]), writes=[rv], dma=True)
                    for mp_ in range(NOWN // 2):
                        m0, m1 = 2 * mp_, 2 * mp_ + 1
                        J0, J1 = 8 * m0 + 8, 8 * m1 + 8
                        acc0 = psO[acc_i[0] % 4]; racc0 = ("psO", acc_i[0] % 4); acc_i[0] += 1
                        acc1 = psO[acc_i[0] % 4]; racc1 = ("psO", acc_i[0] % 4); acc_i[0] += 1
                        dst0 = o_fox[:, m0, h * 64:(h + 1) * 64]
                        dst1 = o_fox[:, m1, h * 64:(h + 1) * 64]
                        qrd = [rk, "QTf", "QTfa"]
                        for g in range(J0 // 2):
                            b = gcnt[0] % NPB
                            gcnt[0] += 1
                            ps = psS[b].rearrange("p (a q) -> p a q", q=256)
                            rps = ("psS", b)
                            P = Pt[b].rearrange("p (a q) -> p a q", q=256)
                            rP = ("P", b)
                            for jj in range(2):
                                j = 2 * g + jj
                                kt = KA[0:69, j * 128:(j + 1) * 128]
                                if j == J0 - 1:
                                    mm(ps[:, jj, 0:128], kt, QTf[0:69, h, m0 * 128:(m0 + 1) * 128], True, False, qrd, [rps])
                                    mm(ps[:, jj, 0:128], ident_b, foxtri_b, False, True, ["ident_b", "foxtri_b"], [rps])
                                    mm(ps[:, jj, 128:256], kt, QTf[0:69, h, m1 * 128:(m1 + 1) * 128], True, True, qrd, [rps])
                                else:
                                    mm(ps[:, jj, :], kt, QTf[0:69, h, m0 * 128:(m0 + 2) * 128], True, True, qrd, [rps])
                            S.add("scalar", lambda e, b=b: e.activation(out=Pt[b], in_=psS[b], func=AF.Exp), reads=[rps], writes=[rP])

                            def pv(g=g, P=P, rP=rP, acc0=acc0, racc0=racc0, acc1=acc1, racc1=racc1, J0=J0, par=par, rv=rv, dst0=dst0):
                                for jj in range(2):
                                    j = 2 * g + jj
                                    mm(acc0, P[:, jj, 0:128], Vb[par][:, j, :], j == 0, j == J0 - 1, [rP, rv], [racc0])
                                    mm(acc1, P[:, jj, 128:256], Vb[par][:, j, :], j == 0, False, [rP, rv], [racc1])
                                if 2 * g + 1 == J0 - 1:
                                    finish(acc0, racc0, dst0)
                            pending.append(pv)
                            drain(3)
                        for g in range(2):
                            b = gcnt[0] % NPB
                            gcnt[0] += 1
                            ps = psS[b].rearrange("p (a q) -> p a q", q=128)
                            rps = ("psS", b)
                            P = Pt[b].rearrange("p (a q) -> p a q", q=128)
                            rP = ("P", b)
                            for jj in range(4):
                                j = J0 + 4 * g + jj
                                diag = (j == J1 - 1)
                                mm(ps[:, jj, :], KA[0:69, j * 128:(j + 1) * 128], QTf[0:69, h, m1 * 128:(m1 + 1) * 128], True, not diag, qrd, [rps])
                                if diag:
                                    mm(ps[:, jj, :], ident_b, foxtri_b, False, True, ["ident_b", "foxtri_b"], [rps])
                            S.add("scalar", lambda e, b=b: e.activation(out=Pt[b], in_=psS[b], func=AF.Exp), reads=[rps], writes=[rP])

                            def pv2(g=g, P=P, rP=rP, acc1=acc1, racc1=racc1, J0=J0, J1=J1, par=par, rv=rv, dst1=dst1):
                                for jj in range(4):
                                    j = J0 + 4 * g + jj
                                    mm(acc1, P[:, jj, :], Vb[par][:, j, :], False, j == J1 - 1, [rP, rv], [racc1])
                                if g == 1:
                                    finish(acc1, racc1, dst1)
                            pending.append(pv2)
                            drain(3)
                drain(0)
                if "o_fox" in dbg:
                    A.seek(OFF_LOC)
                    tmp = A.alloc([NOWN, 384], F32)
                    S.add("vector", lambda e: e.tensor_copy(out=tmp, in_=o_fox), reads=["o_attn", ("KTb", 0), ("KTb", 1)], writes=[("KTb", 0)])
                    S.add("sync", lambda e: e.dma_start(out=dbg["o_fox"].rearrange("(m p) d -> p m d", p=128), in_=tmp), reads=[("KTb", 0)], writes=["dbgo"], dma=True)
                S.flush()

        if on("C"):
            with contextlib.ExitStack() as st2:
                A.seek(OFF_LOC)
                KTb = [A.alloc([NTS * 128], BF16)]
                Vb = [A.alloc([NTS, 65], BF16) for _ in range(2)]
                dbt = [A.alloc([17, 128], F32) for _ in range(2)]
                loc_save = A.off
                A.seek(OFF_QT)
                num = A.alloc([NOWN, 384], F32)
                A.seek(loc_save)
                den = A.alloc([NOWN, 6], F32)
                dsum = A.alloc([NOWN, 2], F32)
                NPB = 4
                Pt = [A.alloc([4, 128], BF16) for _ in range(NPB)]
                ssb = [A.alloc([4, 128], F32) for _ in range(NPB)]
                psS = [psum(f"psSc{i}", [128, 4, 128]) for i in range(NPB)]
                psO = [psum(f"psOc{i}", [128, 65]) for i in range(2)]
                pending = []
                gcnt = [0]
                acc_i = [0]

                def drain_c(keep):
                    while len(pending) > keep:
                        pending.pop(0)()

                def finish_c(acc, racc, m, hd):
                    S.add("vector", lambda e: e.tensor_copy(out=num[:, m, hd * 64:(hd + 1) * 64], in_=acc[:, 0:64]), reads=[racc], writes=["num"])
                    S.add("vector", lambda e: e.tensor_copy(out=den[:, m, hd:hd + 1], in_=acc[:, 64:65]), reads=[racc], writes=["den"])

                def load_v_c(hd_):
                    S.add("sync", lambda e, hd_=hd_: e.dma_start(out=Vb[hd_ % 2], in_=V_scr[6 + hd_]), writes=[("Vbc", hd_ % 2)], dma=True)
                    S.add("sync", lambda e, hd_=hd_: e.dma_start(out=dbt[hd_ % 2], in_=dilb[hd_]), writes=[("dbt", hd_ % 2)], dma=True)

                load_v_c(0)
                for hd in range(6):
                    par = 0
                    dpar = hd % 2
                    hp = (hd % 2) * 64
                    pr = 3 + hd // 2
                    prq = hd // 2
                    rk = ("KTb", 0)
                    q4 = (NTS * 128) // 4
                    for q in range(4):
                        S.add("sync", lambda e, q=q, pr=pr, hp=hp: e.dma_start(out=KTb[0][hp:hp + 64, q * q4:(q + 1) * q4], in_=KT_scr[pr, hp:hp + 64, q * q4:(q + 1) * q4]), writes=[rk], dma=True)
                    if hd + 1 < 6:
                        load_v_c(hd + 1)
                    rv = ("Vbc", dpar)
                    rd = ("dbt", dpar)
                    for m in range(NOWN):
                        sm = 8 * m + 7
                        dls = [dl for dl in range(DMAX[hd // 2] + 1) if sm - dl >= 0]
                        batches = [dls[i:i + 4] for i in range(0, len(dls), 4)]
                        acc = psO[acc_i[0] % 2]
                        racc = ("psO", acc_i[0] % 2)
                        acc_i[0] += 1
                        nbt = len(batches)
                        for bi, bt in enumerate(batches):
                            b = gcnt[0] % NPB
                            gcnt[0] += 1
                            ps, sb, P = psS[b], ssb[b], Pt[b]
                            rps, rsb, rP = ("psS", b), ("ssb", b), ("P", b)
                            nb = len(bt)
                            for t, dl in enumerate(bt):
                                j = sm - dl
                                mm(ps[:, t, :], KTb[par][hp:hp + 64, j * 128:(j + 1) * 128], QT[hp:hp + 64, prq, m * 128:(m + 1) * 128], True, True, [rk, "QT"], [rps])
                            S.add("vector", lambda e, sb=sb, ps=ps, nb=nb, d0=bt[0], dpar=dpar: e.tensor_tensor(out=sb[:, 0:nb, :], in0=ps[:, 0:nb, :], in1=dbt[dpar][:, d0:d0 + nb, :], op=ALU.add),
                                  reads=[rps, rd], writes=[rsb])
                            for t, dl in enumerate(bt):
                                j = sm - dl
                                if j <= 6:
                                    S.add("vector", lambda e, sb=sb, t=t, j=j: e.tensor_scalar(out=sb[:, t, :], in0=sb[:, t, :], scalar1=padb[:, j:j + 1], scalar2=None, op0=ALU.add), reads=[rsb, "cst"], writes=[rsb])
                            S.add("scalar", lambda e, P=P, sb=sb, nb=nb: e.activation(out=P[:, 0:nb, :], in_=sb[:, 0:nb, :], func=AF.Exp), reads=[rsb], writes=[rP])

                            def pv(bt=bt, P=P, rP=rP, acc=acc, racc=racc, bi=bi, nbt=nbt, sm=sm, par=par, dpar=dpar, rv=rv, m=m, hd=hd):
                                for t, dl in enumerate(bt):
                                    j = sm - dl
                                    mm(acc, P[:, t, :], Vb[dpar][:, j, :], (bi == 0 and t == 0), (bi == nbt - 1 and t == len(bt) - 1), [rP, rv], [racc])
                                if bi == nbt - 1:
                                    finish_c(acc, racc, m, hd)
                            pending.append(pv)
                            drain_c(2)
                    drain_c(0)
                drain_c(0)
                S.add("vector", lambda e: e.tensor_tensor(out=dsum, in0=den[:, :, 0:2], in1=den[:, :, 2:4], op=ALU.add), reads=["den"], writes=["dsum"])
                S.add("vector", lambda e: e.tensor_tensor(out=dsum, in0=dsum, in1=den[:, :, 4:6], op=ALU.add), reads=["den", "dsum"], writes=["dsum"])
                S.add("vector", lambda e: e.reciprocal(out=dsum, in_=dsum), reads=["dsum"], writes=["dsum"])
                for m in range(NOWN):
                    for hd in range(6):
                        S.add("vector", lambda e, m=m, hd=hd: e.tensor_scalar(out=o_dil[:, m, hd * 64:(hd + 1) * 64], in0=num[:, m, hd * 64:(hd + 1) * 64],
                                                                      scalar1=dsum[:, m, hd % 2:hd % 2 + 1], scalar2=None, op0=ALU.mult), reads=["num", "dsum"], writes=["o_attn"])
                S.flush()

        if on("Dm"):
            with contextlib.ExitStack() as st2:
                A.seek(OFF_LOC)
                Wkv = A.alloc([8, 512], BF16)
                mT = A.alloc([8, 256], BF16)
                KmT = A.alloc([2, 256], BF16)
                Vm = A.alloc([2, 4, 65], BF16)
                Pt = A.alloc([8, 128], BF16)
                ssb = A.alloc([4, 128], F32)
                rec = A.alloc([8], F32)
                psS = [psum(f"psSm{i}", [128, 128]) for i in range(4)]
                psO = [psum(f"psOm{i}", [128, 65]) for i in range(2)]
                psM = [psum(f"psM{i}", [128, 256]) for i in range(2)]
                S.add("gpsimd", lambda e: e.dma_start(out=Wkv, in_=w_mem_kv.rearrange("(c p) n -> p c n", p=128)), writes=["Wkv"], dma=True)
                S.add("gpsimd", lambda e: e.dma_start(out=mT, in_=memT.rearrange("(c p) n -> p c n", p=128)), writes=["mT"], dma=True)
                assert A.off <= OFF_LOC + 54 * 1024
                A.seek(OFF_LOC + 54 * 1024)
                Wg = A.alloc([8, 3072], BF16)
                Wbr = A.alloc([8, D], BF16)
                for b in range(3):
                    S.add("gpsimd", lambda e, b=b: e.dma_start(out=Wg[:, :, b * 1024:(b + 1) * 1024], in_=w_inv[:, :, 2566 + b * 1024:2566 + (b + 1) * 1024]), writes=["Wg"], dma=True)
                S.add("gpsimd", lambda e: e.dma_start(out=Wbr, in_=w_br.rearrange("(c p) n -> p c n", p=128)), writes=["Wbr"], dma=True)
                S.add("vector", lambda e: e.memset(Vm[:, :, :, 64:65], 1.0), writes=["Vm1"])
                for pr in range(2):
                    for c in range(8):
                        mm(psM[pr], Wkv[:, c, pr * 128:(pr + 1) * 128], mT[:, c, :], c == 0, c == 7, ["Wkv", "mT"], [("psM", pr)])
                    evac(pr, KmT[:, pr, :], psM[pr], [("psM", pr)], ["KmT"])
                for t2 in range(2):
                    for c in range(8):
                        mm(psM[t2], mT[:, c, t2 * 128:(t2 + 1) * 128], Wkv[:, c, 256:512], c == 0, c == 7, ["Wkv", "mT"], [("psM", t2)])
                    evac(t2, Vm[:, t2, :, 0:64], psM[t2].rearrange("p (h d) -> p h d", d=64), [("psM", t2)], ["Vm", "Vm1"])
                pipe = AttnPipe(psS, Pt, ssb, psO)
                fin_i = [0]

                def make_finish_m(dst):
                    def finish(acc, racc):
                        r = rec[:, fin_i[0] % 8:fin_i[0] % 8 + 1]
                        rr = ("rec", fin_i[0] % 8)
                        fin_i[0] += 1
                        S.add("vector", lambda e: e.reciprocal(out=r, in_=acc[:, 64:65]), reads=[racc], writes=[rr])
                        S.add("vector", lambda e: e.tensor_scalar(out=dst, in0=acc[:, 0:64], scalar1=r, scalar2=None, op0=ALU.mult), reads=[racc, rr], writes=["o_attn"])
                    return finish

                for hm in range(4):
                    hp = (hm % 2) * 64
                    pr = hm // 2
                    for m in range(NOWN):
                        items = []
                        for t2 in range(2):
                            items.append(dict(
                                lhsT=KmT[hp:hp + 64, pr, t2 * 128:(t2 + 1) * 128], rhs=QT[hp:hp + 64, 3 + pr, m * 128:(m + 1) * 128],
                                reads=["KmT", "QT"], mask=None, mreads=[], bias=None, breads=[], v=Vm[:, t2, hm, :], vreads=["Vm", "Vm1"]))
                        pipe.run(items, make_finish_m(o_mem[:, m, hm * 64:(hm + 1) * 64]))
                pipe.flush()
                S.flush()

        if on("M1"):
            with contextlib.ExitStack() as st2:
                A.seek(OFF_LOC + 54 * 1024)
                Wg = A.alloc([8, 3072], BF16)
                Wbr = A.alloc([8, D], BF16)
                A.seek(OFF_LOC2 + 4 * 1024)
                Wout = A.alloc([8, D], BF16)
                S.add("gpsimd", lambda e: e.dma_start(out=Wout, in_=w_out.rearrange("(c p) n -> p c n", p=128)), writes=["Wout"], dma=True)
                A.seek(OFF_LOC)
                xbb = [A.alloc([8, 4, 128], BF16) for _ in range(2)]
                oT = A.alloc([8, 512], BF16)
                sg = [A.alloc([512], F32) for _ in range(3)]
                t1 = A.alloc([512], F32)
                t2b = A.alloc([512], F32)
                assert A.off <= OFF_LOC2 + 4 * 1024, A.off
                psT = psum("psT", [128, 512], BF16)
                psB = [psum(f"psB{i}", [128, 512]) for i in range(3)]
                psG = [psum(f"psG{i}", [128, 512]) for i in range(2)]
                ev = 0
                gk = 0
                for b4 in range(4):
                    xb = xbb[b4 % 2]
                    rx = ("xbb", b4 % 2)
                    for c in range(8):
                        S.add("gpsimd", lambda e, xb=xb, b4=b4, c=c: e.dma_start(out=xb[:, c, :, :], in_=xTown[:, c, 4 * b4:4 * b4 + 4, 7, :]), writes=[rx], dma=True)
                    xbf = xb.rearrange("p c m t -> p c (m t)")
                    for ch in range(8):
                        for mm_ in range(4):
                            m = 4 * b4 + mm_
                            if ch < 3:
                                src = o_fox[:, m, ch * 128:(ch + 1) * 128]
                            elif ch < 6:
                                src = o_dil[:, m, (ch - 3) * 128:(ch - 2) * 128]
                            else:
                                src = o_mem[:, m, (ch - 6) * 128:(ch - 5) * 128]
                            S.add("tensor", lambda e, src=src, mm_=mm_: e.transpose(psT[:, mm_ * 128:(mm_ + 1) * 128], src, ident_b), reads=["o_attn", "ident_b"], writes=["psT"])
                        evac(ev, oT[:, ch, :], psT, ["psT"], ["oT"]); ev += 1
                    for dc in range(8):
                        for bi, chs in enumerate(((0, 1, 2), (3, 4, 5), (6, 7))):
                            for ci, ch in enumerate(chs):
                                mm(psB[bi], Wbr[:, ch, dc * 128:(dc + 1) * 128], oT[:, ch, :], ci == 0, ci == len(chs) - 1, ["Wbr", "oT"], [("psB", bi)])
                        for b in range(3):
                            pg = psG[gk % 2]
                            rg = ("psG", gk % 2)
                            gk += 1
                            for c in range(8):
                                mm(pg, Wg[:, c, b * 1024 + dc * 128:b * 1024 + (dc + 1) * 128], xbf[:, c, :], c == 0, c == 7, ["Wg", rx], [rg])
                            S.add("scalar", lambda e, b=b, pg=pg: e.activation(out=sg[b], in_=pg, func=AF.Sigmoid), reads=[rg], writes=[("sg", b)])
                        S.add("vector", lambda e: e.tensor_tensor(out=t1, in0=psB[0], in1=sg[0], op=ALU.mult), reads=[("psB", 0), ("sg", 0)], writes=["t1"])
                        S.add("vector", lambda e: e.tensor_tensor(out=t2b, in0=psB[1], in1=sg[1], op=ALU.mult), reads=[("psB", 1), ("sg", 1)], writes=["t2b"])
                        S.add("vector", lambda e: e.tensor_tensor(out=t1, in0=t1, in1=t2b, op=ALU.add), reads=["t1", "t2b"], writes=["t1"])
                        S.add("vector", lambda e: e.tensor_tensor(out=t2b, in0=psB[2], in1=sg[2], op=ALU.mult), reads=[("psB", 2), ("sg", 2)], writes=["t2b"])
                        S.add("vector", lambda e, dc=dc, b4=b4: e.tensor_tensor(out=mergedT[:, dc, b4 * 512:(b4 + 1) * 512], in0=t1, in1=t2b, op=ALU.add), reads=["t1", "t2b"], writes=["mergedT"])
                S.flush()

        def layer_norm(z, rz, dst, rdst, g_rep, b_rep, stats, mv, rstd):
            zz = z.rearrange("p (a b) -> p a b", b=512)
            for a in range(2):
                S.add("vector", lambda e, a=a: e.bn_stats(out=stats[:, a, :], in_=zz[:, a, :]), reads=[rz], writes=["stats"])
            S.add("vector", lambda e: e.bn_aggr(out=mv, in_=stats.rearrange("p a b -> p (a b)")), reads=["stats"], writes=["mv"])
            S.add("vector", lambda e: e.tensor_scalar(out=rstd, in0=mv[:, 1:2], scalar1=1e-5, scalar2=None, op0=ALU.add), reads=["mv"], writes=["rstd"])
            S.add("scalar", lambda e: e.activation(out=rstd, in_=rstd, func=AF.Sqrt), reads=["rstd"], writes=["rstd"])
            S.add("vector", lambda e: e.reciprocal(out=rstd, in_=rstd), reads=["rstd"], writes=["rstd"])
            S.add("vector", lambda e: e.tensor_scalar(out=z, in0=z, scalar1=mv[:, 0:1], scalar2=rstd, op0=ALU.subtract, op1=ALU.mult), reads=[rz, "mv", "rstd"], writes=[rz])
            S.add("gpsimd", lambda e: e.tensor_tensor(out=z, in0=z, in1=g_rep, op=ALU.mult), reads=[rz, "lnp"], writes=[rz])
            S.add("vector", lambda e: e.tensor_tensor(out=dst, in0=z, in1=b_rep, op=ALU.add), reads=[rz, "lnp"], writes=[rdst])

        if on("M2"):
            with contextlib.ExitStack() as st2:
                A.seek(OFF_LOC2 + 4 * 1024)
                Wout = A.alloc([8, D], BF16)
                g_rep = A.alloc([D], F32)
                b_rep = A.alloc([D], F32)
                xown = [A.alloc([D], F32) for _ in range(2)]
                zb = [A.alloc([D], F32) for _ in range(2)]
                stats = A.alloc([2, 6], F32)
                mv = A.alloc([2], F32)
                rstd = A.alloc([1], F32)
                psY = [psum(f"psY{i}", [128, 512]) for i in range(4)]
                S.add("sync", lambda e: e.dma_start(out=g_rep, in_=lnrep[0]), writes=["lnp"], dma=True)
                S.add("sync", lambda e: e.dma_start(out=b_rep, in_=lnrep[1]), writes=["lnp"], dma=True)
                for m in range(NOWN):
                    xo_ = xown[m % 2]
                    rxo = ("xown", m % 2)
                    z = zb[m % 2]
                    rz = ("z", m % 2)
                    S.add("sync", lambda e, xo_=xo_, m=m: e.dma_start(out=xo_, in_=x_own[m * 128:(m + 1) * 128, :]), writes=[rxo], dma=True)
                    for eh in range(2):
                        py = psY[(2 * m + eh) % 4]
                        rpy = ("psY", (2 * m + eh) % 4)
                        for dc in range(8):
                            mm(py, mergedT[:, dc, m * 128:(m + 1) * 128], Wout[:, dc, eh * 512:(eh + 1) * 512], dc == 0, dc == 7, ["mergedT", "Wout"], [rpy])
                        S.add("vector", lambda e, z=z, xo_=xo_, py=py, eh=eh: e.scalar_tensor_tensor(
                            out=z[:, eh * 512:(eh + 1) * 512], in0=xo_[:, eh * 512:(eh + 1) * 512], scalar=ALPHA, in1=py, op0=ALU.mult, op1=ALU.add),
                            reads=[rxo, rpy], writes=[rz])
                    layer_norm(z, rz, h1[:, m, :], "h1", g_rep, b_rep, stats, mv, rstd)
                if "h1" in dbg:
                    S.add("sync", lambda e: e.dma_start(out=dbg["h1"].rearrange("(m p) d -> p m d", p=128), in_=h1), reads=["h1"], writes=["dbgh"], dma=True)
                S.flush()

        if on("R"):
            with contextlib.ExitStack() as st2:
                A.seek(OFF_LOC2)
                Wr = A.alloc([8, NE], F32)
                hT = [A.alloc([8, 128], F32) for _ in range(2)]
                hbf = [A.alloc([D], BF16) for _ in range(2)]
                lg2 = [A.alloc([NE], F32) for _ in range(2)]
                mx82 = [A.alloc([8], F32) for _ in range(2)]
                negm2 = [A.alloc([1], F32) for _ in range(2)]
                ex42 = [A.alloc([4], F32) for _ in range(2)]
                esum2 = [A.alloc([1], F32) for _ in range(2)]
                maskall = A.alloc([NOWN, NE], BF16)
                destf2 = [A.alloc([NE], F32) for _ in range(2)]
                selk2 = [A.alloc([NE], F32) for _ in range(2)]
                junk2 = [A.alloc([NE], F32) for _ in range(2)]
                dk2 = [A.alloc([4], F32) for _ in range(2)]
                psTr = [psum(f"psTr{i}", [128, 4, 128]) for i in range(2)]
                psL2 = [psum(f"psL{i}", [128, NE]) for i in range(2)]
                psP2 = [psum(f"psP{i}", [128, NE]) for i in range(2)]
                S.add("sync", lambda e: e.dma_start(out=Wr, in_=w_router.rearrange("(c p) n -> p c n", p=128)), writes=["Wr"], dma=True)
                for m in range(NOWN):
                    p_ = m % 2
                    lg, mx8, negm, ex4, esum = lg2[p_], mx82[p_], negm2[p_], ex42[p_], esum2[p_]
                    destf, selk, junk, dk, psL, psP = destf2[p_], selk2[p_], junk2[p_], dk2[p_], psL2[p_], psP2[p_]
                    rlg, rmx, rng_, rex, res_ = ("lg", p_), ("mx8", p_), ("negm", p_), ("ex4", p_), ("esum", p_)
                    rdf, rsk, rjk, rdk, rpl_, rpp = ("destf", p_), ("selk", p_), ("junk", p_), ("dk", p_), ("psL", p_), ("psP", p_)
                    hTm = hT[m % 2]
                    rh = ("hT", m % 2)
                    for c in range(8):
                        S.add("tensor", lambda e, m=m, c=c: e.transpose(psTr[c // 4][:, c % 4, :], h1[:, m, c * 128:(c + 1) * 128], ident_f), reads=["h1", "cst"], writes=[("psTr", c // 4)])
                        if c % 4 == 3:
                            evac(c // 4, hTm[:, c - 3:c + 1, :], psTr[c // 4], [("psTr", c // 4)], [rh])
                    for c in range(8):
                        mm(psL, hTm[:, c, :], Wr[:, c, :], c == 0, c == 7, [rh, "Wr"], [rpl_])
                    S.add("vector", lambda e, lg=lg, psL=psL: e.tensor_tensor(out=lg, in0=psL, in1=br_rep, op=ALU.add), reads=[rpl_, "cst"], writes=[rlg])
                    S.add("vector", lambda e, lg=lg, mx8=mx8: e.max(out=mx8, in_=lg), reads=[rlg], writes=[rmx])
                    S.add("vector", lambda e, m=m, lg=lg, mx8=mx8: e.tensor_scalar(out=maskall[:, m, :], in0=lg, scalar1=mx8[:, 3:4], scalar2=None, op0=ALU.is_ge), reads=[rlg, rmx], writes=[("maskall", m)])
                    S.add("vector", lambda e, negm=negm, mx8=mx8: e.tensor_scalar(out=negm, in0=mx8[:, 0:1], scalar1=-1.0, scalar2=None, op0=ALU.mult), reads=[rmx], writes=[rng_])
                    S.add("scalar", lambda e, ex4=ex4, mx8=mx8, negm=negm: e.activation(out=ex4, in_=mx8[:, 0:4], func=AF.Exp, bias=negm, scale=1.0), reads=[rmx, rng_], writes=[rex])
                    S.add("vector", lambda e, esum=esum, ex4=ex4: e.tensor_reduce(out=esum, in_=ex4, axis=mybir.AxisListType.X, op=ALU.add), reads=[rex], writes=[res_])
                    S.add("vector", lambda e, esum=esum: e.reciprocal(out=esum, in_=esum), reads=[res_], writes=[res_])
                    S.add("vector", lambda e, m=m, ex4=ex4, esum=esum: e.tensor_scalar(out=gate4[:, m, :], in0=ex4, scalar1=esum, scalar2=None, op0=ALU.mult), reads=[rex, res_], writes=[("gate4", m)])
                    for m2 in range(m):
                        mm(psP, ones_b, maskall[:, m2, :], m2 == 0, False, [("maskall", m2), "ones_b"], [rpp])
                    mm(psP, tri_strict_b, maskall[:, m, :], m == 0, True, [("maskall", m), "tsb"], [rpp])
                    S.add("vector", lambda e, destf=destf, psP=psP: e.scalar_tensor_tensor(out=destf, in0=psP, scalar=float(CAP - 1), in1=iotaC, op0=ALU.min, op1=ALU.add), reads=[rpp, "cst"], writes=[rdf])
                    for k in range(4):
                        S.add("vector", lambda e, k=k, selk=selk, lg=lg, mx8=mx8: e.tensor_scalar(out=selk, in0=lg, scalar1=mx8[:, k:k + 1], scalar2=None, op0=ALU.is_equal), reads=[rlg, rmx], writes=[rsk])
                        S.add("vector", lambda e, k=k, junk=junk, selk=selk, destf=destf: e.tensor_tensor(out=junk, in0=selk, in1=destf, op=ALU.mult), reads=[rsk, rdf], writes=[rjk])
                        S.add("vector", lambda e, k=k, dk=dk, junk=junk: e.tensor_reduce(out=dk[:, k:k + 1], in_=junk, axis=mybir.AxisListType.X, op=ALU.add), reads=[rjk], writes=[rdk])
                    S.add("vector", lambda e, m=m, dk=dk: e.tensor_copy(out=dest_i[:, m, :], in_=dk), reads=[rdk], writes=[("dest_i", m)])
                    hb = hbf[m % 2]
                    rhb = ("hbf", m % 2)
                    S.add("scalar", lambda e, hb=hb, m=m: e.copy(out=hb, in_=h1[:, m, :]), reads=["h1"], writes=[rhb])
                    for k in range(4):
                        S.add("gpsimd", lambda e, hb=hb, m=m, k=k: e.indirect_dma_start(
                            out=X_scr, out_offset=bass.IndirectOffsetOnAxis(ap=dest_i[:, m, k:k + 1], axis=0), in_=hb, in_offset=None),
                            reads=[rhb, ("dest_i", m)], writes=[("X_scr", m, k)], dma=True)
                if "route" in dbg:
                    A.seek(OFF_LOC2 + 48 * 1024)
                    tmp = A.alloc([NOWN, 8], F32)
                    S.add("vector", lambda e: e.tensor_copy(out=tmp[:, :, 0:4], in_=dest_i), reads=[("dest_i", mm_) for mm_ in range(NOWN)], writes=["tmpr"])
                    S.add("vector", lambda e: e.tensor_copy(out=tmp[:, :, 4:8], in_=gate4), reads=[("gate4", mm_) for mm_ in range(NOWN)], writes=["tmpr"])
                    S.add("sync", lambda e: e.dma_start(out=dbg["route"].rearrange("(m p) d -> p m d", p=128), in_=tmp), reads=["tmpr"], writes=["dbgr"], dma=True)
                S.flush()

        if on("E"):
            with contextlib.ExitStack() as st2:
                A.seek(OFF_QT)
                W2h = [A.alloc([8, 512], BF16) for _ in range(3)]
                b1sb = A.alloc([NE * 16], F32)
                b2rep = [A.alloc([D], F32)]
                Ysb = [A.alloc([D], F32) for _ in range(2)]
                Xe = [A.alloc([3, D], BF16)]
                A.seek(OFF_LOC2)
                Xe.append(A.alloc([3, D], BF16))
                NQ = 5
                W1q = [A.alloc([8, 4, 128], BF16) for _ in range(NQ)]
                XT = [A.alloc([8, CAP], BF16) for _ in range(2)]
                actT = A.alloc([8, CAP], BF16)
                glu = [A.alloc([CAP], F32) for _ in range(2)]
                sig = [A.alloc([CAP], F32) for _ in range(4)]
                lin = [A.alloc([CAP], F32) for _ in range(2)]
                psX = psum("psX", [128, 512], BF16)
                psH = [psum(f"psH{i}", [128, CAP]) for i in range(4)]
                psY2 = [psum(f"psY2{i}", [128, 512]) for i in range(2)]
                S.add("sync", lambda e: e.dma_start(out=b1sb, in_=b1T), writes=["b1sb"], dma=True)
                Xv = X_scr.rearrange("(e sb p) d -> e p sb d", sb=3, p=128)
                Yv = Y_scr.rearrange("(e sb p) d -> e sb p d", sb=3, p=128)
                hk = 0
                yk = 0
                pieces = []
                for ex_ in range(NE):
                    pieces += [(ex_, "w1", q_) for q_ in range(4)] + [(ex_, "w2", d_) for d_ in range(2)]
                pbuf = {}
                pstate = {"next": 0, "qi": 0, "w2i": 0}

                def ensure(upto):
                    while pstate["next"] < min(upto, len(pieces)):
                        i = pstate["next"]
                        pstate["next"] += 1
                        ex_, kind, j = pieces[i]
                        if kind == "w1":
                            w1v_ = w_exp_in[ex_].rearrange("(c p) f -> p c f", p=128)
                            f0 = (0, 1024, 512, 1536)[j]
                            wq = W1q[pstate["qi"] % NQ]
                            rwq = ("W1q", pstate["qi"] % NQ)
                            pstate["qi"] += 1
                            S.add("gpsimd", lambda e, wq=wq, w1v_=w1v_, f0=f0: e.dma_start(out=wq.rearrange("p c r f -> p c (r f)"), in_=w1v_[:, :, f0:f0 + 512]), writes=[rwq], dma=True)
                            pbuf[i] = (wq, rwq)
                        else:
                            w2v_ = w_exp_out[ex_].rearrange("(c p) f -> p c f", p=128)
                            w2 = W2h[pstate["w2i"] % 3]
                            rw2 = ("W2h", pstate["w2i"] % 3)
                            pstate["w2i"] += 1
                            S.add("gpsimd", lambda e, w2=w2, w2v_=w2v_, j=j: e.dma_start(out=w2, in_=w2v_[:, :, j * 512:(j + 1) * 512]), writes=[rw2], dma=True)
                            pbuf[i] = (w2, rw2)

                def load_x(ex_):
                    S.add("sync", lambda e, ex_=ex_: e.dma_start(out=Xe[ex_ % 2], in_=Xv[ex_]), reads=["X_scr"], writes=[("Xe", ex_ % 2)], dma=True)

                def transposes(ex_):
                    xe = Xe[ex_ % 2]
                    rxe = ("Xe", ex_ % 2)
                    for c in range(8):
                        for sb in range(3):
                            S.add("tensor", lambda e, xe=xe, c=c, sb=sb: e.transpose(psX[:, sb * 128:(sb + 1) * 128], xe[:, sb, c * 128:(c + 1) * 128], ident_b), reads=[rxe, "ident_b"], writes=["psX"])
                        S.add("vector", lambda e, c=c, ex_=ex_: e.tensor_copy(out=XT[ex_ % 2][:, c, :], in_=psX[:, 0:CAP]), reads=["psX"], writes=[("XT", ex_ % 2)])

                load_x(0)
                ensure(5)
                transposes(0)
                for ex in range(NE):
                    xt = XT[ex % 2]
                    rxt = ("XT", ex % 2)
                    if ex + 1 < NE:
                        load_x(ex + 1)
                    S.add("sync", lambda e, ex=ex: e.dma_start(out=b2rep[0], in_=b_exp_out[ex:ex + 1, :].partition_broadcast(128)), writes=[("b2rep", 0)], dma=True)
                    for half in range(2):
                        pG = ex * 6 + 2 * half
                        ensure(pG + 5)
                        wq, rwq = pbuf.pop(pG)
                        for r in range(4):
                            fb = 4 * half + r
                            pg = psH[hk % 4]; rpg = ("psH", hk % 4); hk += 1
                            for c in range(8):
                                mm(pg, wq[:, c, r, :], xt[:, c, :], c == 0, c == 7, [rwq, rxt], [rpg])
                            g_ = glu[r % 2]; rg_ = ("glu", r % 2)
                            s_ = sig[r]; rs_ = ("sig", r)
                            cg = ex * 16 + fb
                            S.add("vector", lambda e, g_=g_, pg=pg, cg=cg: e.tensor_scalar(out=g_, in0=pg, scalar1=b1sb[:, cg:cg + 1], scalar2=7.0, op0=ALU.add, op1=ALU.min), reads=[rpg, "b1sb"], writes=[rg_])
                            S.add("scalar", lambda e, g_=g_, s_=s_: e.activation(out=s_, in_=g_, func=AF.Silu, scale=1.702), reads=[rg_], writes=[rs_])
                        ensure(pG + 6)
                        wl, rwl = pbuf.pop(pG + 1)
                        for r in range(4):
                            fb = 4 * half + r
                            pl = psH[hk % 4]; rpl = ("psH", hk % 4); hk += 1
                            for c in range(8):
                                mm(pl, wl[:, c, r, :], xt[:, c, :], c == 0, c == 7, [rwl, rxt], [rpl])
                            l_ = lin[r % 2]; rl_ = ("lin", r % 2)
                            s_ = sig[r]; rs_ = ("sig", r)
                            cl = ex * 16 + 8 + fb
                            S.add("vector", lambda e, l_=l_, pl=pl, cl=cl: e.tensor_scalar(out=l_, in0=pl, scalar1=b1sb[:, cl:cl + 1], scalar2=7.0, op0=ALU.add, op1=ALU.min), reads=[rpl, "b1sb"], writes=[rl_])
                            S.add("vector", lambda e, l_=l_: e.tensor_scalar(out=l_, in0=l_, scalar1=-7.0, scalar2=1.0, op0=ALU.max, op1=ALU.add), reads=[rl_], writes=[rl_])
                            S.add("vector", lambda e, fb=fb, s_=s_, l_=l_: e.scalar_tensor_tensor(out=actT[:, fb, :], in0=s_, scalar=1.0 / 1.702, in1=l_, op0=ALU.mult, op1=ALU.mult), reads=[rs_, rl_], writes=["actT"])
                    if ex + 1 < NE:
                        transposes(ex + 1)
                    ensure(ex * 6 + 4 + 5)
                    w2s = [pbuf.pop(ex * 6 + 4), pbuf.pop(ex * 6 + 5)]
                    for sb in range(3):
                        ys = Ysb[yk % 2]
                        rys = ("Ysb", yk % 2)
                        yk += 1
                        for dh in range(2):
                            w2, rw2 = w2s[dh]
                            py = psY2[dh]
                            rpy = ("psY2", dh)
                            for fc in range(8):
                                mm(py, actT[:, fc, sb * 128:(sb + 1) * 128], w2[:, fc, :], fc == 0, fc == 7, ["actT", rw2], [rpy])
                            S.add("vector", lambda e, ys=ys, py=py, dh=dh: e.tensor_tensor(out=ys[:, dh * 512:(dh + 1) * 512], in0=py, in1=b2rep[0][:, dh * 512:(dh + 1) * 512], op=ALU.add),
                                  reads=[rpy, ("b2rep", 0)], writes=[rys])
                        S.add("sync", lambda e, ys=ys, ex=ex, sb=sb: e.dma_start(out=Yv[ex, sb], in_=ys), reads=[rys], writes=["Y_scr"], dma=True)
                S.flush()

        if on("F"):
            with contextlib.ExitStack() as st2:
                A.seek(OFF_LOC2)
                g_rep = A.alloc([D], F32)
                b_rep = A.alloc([D], F32)
                Yk = [A.alloc([D], F32) for _ in range(8)]
                zb = [A.alloc([D], F32) for _ in range(2)]
                ob = [A.alloc([D], F32) for _ in range(2)]
                stats = A.alloc([2, 6], F32)
                mv = A.alloc([2], F32)
                rstd = A.alloc([1], F32)
                S.add("sync", lambda e: e.dma_start(out=g_rep, in_=lnrep[2]), writes=["lnp"], dma=True)
                S.add("sync", lambda e: e.dma_start(out=b_rep, in_=lnrep[3]), writes=["lnp"], dma=True)
                for m in range(NOWN):
                    z = zb[m % 2]
                    rz = ("z", m % 2)
                    S.add("vector", lambda e, z=z, m=m: e.tensor_scalar(out=z, in0=h1[:, m, :], scalar1=ALPHA, scalar2=None, op0=ALU.mult), reads=["h1"], writes=[rz])
                    for k in range(4):
                        yk_ = Yk[(4 * m + k) % 8]
                        ryk = ("Yk", (4 * m + k) % 8)
                        S.add("gpsimd", lambda e, yk_=yk_, m=m, k=k: e.indirect_dma_start(
                            out=yk_, out_offset=None, in_=Y_scr, in_offset=bass.IndirectOffsetOnAxis(ap=dest_i[:, m, k:k + 1], axis=0)),
                            reads=["Y_scr", "dest_i"], writes=[ryk], dma=True)
                        S.add("vector", lambda e, z=z, yk_=yk_, m=m, k=k: e.scalar_tensor_tensor(out=z, in0=yk_, scalar=gate4[:, m, k:k + 1], in1=z, op0=ALU.mult, op1=ALU.add), reads=[ryk, "gate4", rz], writes=[rz])
                    o_ = ob[m % 2]
                    ro = ("ob", m % 2)
                    layer_norm(z, rz, o_, ro, g_rep, b_rep, stats, mv, rstd)
                    S.add("sync", lambda e, o_=o_, m=m: e.dma_start(out=out_d[m * 128:(m + 1) * 128, :], in_=o_), reads=[ro], writes=["out"], dma=True)
                S.flush()
    return nc


def _t5_bucket(dist):
    dist = np.asarray(dist, dtype=np.int64)
    nf = np.maximum(dist, 16).astype(np.float32)
    large = 16 + (np.log(nf / np.float32(16)) / np.float32(math.log(2048 / 16)) * np.float32(16)).astype(np.int32)
    large = np.minimum(large, 31)
    return np.where(dist < 16, dist, large).astype(np.int64)


def _host_consts(core, b_fgate, b_router):
    n = 6 * 128 + 32 + NTS + 24 + 32
    c = np.zeros((128, n), np.float32)
    idx = np.arange(128)
    c[:, 0:128] = np.eye(128, dtype=np.float32)
    c[:, 128:256] = (idx[:, None] <= idx[None, :])
    c[:, 256:384] = (idx[:, None] < idx[None, :])
    c[:, 384:512] = 1.0
    c[64, 512:640] = 1.0
    c[:, 640:768] = np.where(idx[:, None] <= idx[None, :], 0.0, NEG)
    c[:, 768:800] = (np.arange(NE) * CAP)[None, :]
    s = np.arange(NTS)
    a = s - 7 + core
    c[:, 800:800 + NTS] = np.where((a >= 0) & (a < 128), 0.0, NEG)[None, :]
    c[:, 800 + NTS:800 + NTS + 24] = np.tile(np.asarray(b_fgate, np.float32).reshape(1, 6), (128, 4))
    c[:, 800 + NTS + 24:800 + NTS + 56] = np.asarray(b_router, np.float32).reshape(1, NE)
    return c


def _dil_bias(t5_bias):
    out = np.full((6, 128, 17, 128), NEG, np.float32)
    k = np.arange(128)[:, None]
    q = np.arange(128)[None, :]
    for hd in range(6):
        window, dil = DIL_CFG[hd // 2]
        for dl in range(DMAX[hd // 2] + 1):
            dist = 128 * dl + q - k
            valid = (dist >= 0) & (dist % dil == 0) & (dist <= window)
            b = _t5_bucket(np.clip(dist, 0, None))
            vals = t5_bias[b, hd]
            out[hd, :, dl, :] = np.where(valid, vals, np.float32(NEG))
    return out


_CACHE = {}


def prepare_inputs(inputs):
    x = np.asarray(inputs["x"], np.float32)[0]
    T = NTS * 128
    shared = {
        "memT": np.ascontiguousarray(np.asarray(inputs["mem"], np.float32)[0].T),
        "w_in": np.ascontiguousarray(np.asarray(inputs["w_in"], np.float32)[0]),
        "w_mem_kv": np.ascontiguousarray(np.asarray(inputs["w_mem_kv"], np.float32)[0]),
        "w_br": np.ascontiguousarray(np.concatenate([np.asarray(inputs["w_br_fox"], np.float32)[0], np.asarray(inputs["w_br_dil"], np.float32)[0],
                                                     np.asarray(inputs["w_br_mem"], np.float32)[0]], axis=0)),
        "w_out": np.ascontiguousarray(np.asarray(inputs["w_out"], np.float32)[0]),
        "w_router": np.ascontiguousarray(np.asarray(inputs["w_router"], np.float32)[0]),
        "w_exp_in": np.ascontiguousarray(np.asarray(inputs["w_exp_in"], np.float32)[0]),
        "w_exp_out": np.ascontiguousarray(np.asarray(inputs["w_exp_out"], np.float32)[0]),
        "b_exp_out": np.ascontiguousarray(np.asarray(inputs["b_exp_out"], np.float32)[0]),
        "b1T": np.ascontiguousarray(np.asarray(inputs["b_exp_in"], np.float32)[0].reshape(NE, 16, 128).transpose(2, 0, 1).reshape(128, NE * 16)),
        "lnrep": np.ascontiguousarray(np.stack([np.broadcast_to(np.asarray(inputs[k], np.float32)[0][None, :], (128, D)) for k in ("ln1_g", "ln1_b", "ln2_g", "ln2_b")])),
        "dilb": _dil_bias(np.asarray(inputs["t5_bias"], np.float32)),
    }
    xt = x.reshape(NOWN, NCORES, 128, D)
    in_maps = []
    for i in range(NCORES):
        xs = np.zeros((T, D), np.float32)
        xs[(7 - i) * 128:(7 - i) * 128 + S_LEN] = x
        mp = dict(shared)
        mp["xT"] = np.ascontiguousarray(xs.T)
        mp["x_own"] = np.ascontiguousarray(xt[:, i].reshape(NOWN * 128, D))
        mp["cst"] = _host_consts(i, inputs["b_fgate"], inputs["b_router"])
        a_tile = np.arange(NTS) - 7 + i
        mp["padrow"] = np.repeat(np.where((a_tile >= 0) & (a_tile < 128), 0.0, NEG).astype(np.float32), 128)[None, :]
        in_maps.append(mp)
    return in_maps


def assemble(results, key="out", width=D):
    out = np.zeros((NOWN, NCORES, 128, width), np.float32)
    for i in range(NCORES):
        out[:, i] = np.asarray(results[i][key], np.float32).reshape(NOWN, 128, width)
    return out.reshape(1, S_LEN, width)


def kernel(**inputs):
    if "nc" not in _CACHE:
        _CACHE["nc"] = build_program()
    in_maps = prepare_inputs(inputs)
    res = run_bass_kernel_spmd(_CACHE["nc"], in_maps, core_ids=list(range(NCORES)))
    return assemble(res.results)
```
